# Optimizing a Trainium2 kernel written in Bass

```python
import math
import jax, jax.numpy as jnp
from jax import lax
import numpy as np

D_MODEL = 1024
BATCH = 8
SEQ = 4096
DEPTH = 2

A_HEADS = 4
A_HEAD_DIM = D_MODEL // (2 * A_HEADS)
A_DIM = A_HEADS * A_HEAD_DIM
SHORT_CONV = 4
B_CHANNELS = D_MODEL // 2
B_CONV = 31
AB_IN = 4 * A_DIM + 2 * A_HEADS + 2 * B_CHANNELS
AB_OUT = A_DIM + B_CHANNELS
C_HEADS = 4
C_V_HEAD = D_MODEL // C_HEADS
C_QK_HEAD = C_V_HEAD // 2
C_QK = C_HEADS * C_QK_HEAD
C_V = C_HEADS * C_V_HEAD
C_IN = 2 * C_QK + 2 * C_V + 2 * C_HEADS
GATE_CAP = 15.0
CHUNK = 64
D_FF = 7 * D_MODEL // 2
N_EXPERTS = 8
TOP_K = 2
PLE_DIM = 256
N_EVEN = (DEPTH + 1) // 2
N_ODD = DEPTH // 2
DN_ALPHA = (2 * DEPTH) ** 0.25
DN_BETA = (8 * DEPTH) ** -0.25

kernel_name = "hybrid_deltanet_conformer_mlstm_moe_deepnorm"


def layer_norm(x, g, b, eps=1e-5):
    xf = x.astype(jnp.float32)
    mu = jnp.mean(xf, -1, keepdims=True)
    var = jnp.mean(jnp.square(xf - mu), -1, keepdims=True)
    return ((xf - mu) * lax.rsqrt(var + eps) * g + b).astype(x.dtype)


def rms_norm(x, g, eps=1e-6):
    xf = x.astype(jnp.float32)
    return (xf * lax.rsqrt(jnp.mean(jnp.square(xf), -1, keepdims=True) + eps) * g).astype(x.dtype)


def l2norm(t, eps=1e-6):
    return t * lax.rsqrt(jnp.sum(jnp.square(t), -1, keepdims=True) + eps)


def soft_cap(t):
    return GATE_CAP * jnp.tanh(t / GATE_CAP)


def causal_depthwise_conv(x, w):
    K, C = w.shape
    xp = jnp.pad(x, ((0, 0), (K - 1, 0), (0, 0)))
    return lax.conv_general_dilated(xp, w.astype(x.dtype)[:, None, :], window_strides=(1,),
                                    padding='VALID', dimension_numbers=('NWC', 'WIO', 'NWC'),
                                    feature_group_count=C)


def gated_delta_rule(q, k, v, beta, g):
    Bsz, H, S, dk = q.shape
    dv = v.shape[-1]
    n = S // CHUNK
    q = q.reshape(Bsz, H, n, CHUNK, dk)
    k = k.reshape(Bsz, H, n, CHUNK, dk)
    v = v.reshape(Bsz, H, n, CHUNK, dv)
    beta = beta.reshape(Bsz, H, n, CHUNK)
    gc = jnp.cumsum(g.reshape(Bsz, H, n, CHUNK), axis=-1)
    causal = jnp.tril(jnp.ones((CHUNK, CHUNK), dtype=bool))
    strict = jnp.tril(jnp.ones((CHUNK, CHUNK), dtype=bool), -1)
    decay = jnp.exp(jnp.where(causal, gc[..., :, None] - gc[..., None, :], -jnp.inf))
    k_beta = k * beta[..., None]
    lower = jnp.where(strict, jnp.einsum('bhnid,bhnjd->bhnij', k_beta, k) * decay, 0.0)
    tmat = jnp.eye(CHUNK, dtype=q.dtype) + lower
    u = lax.linalg.triangular_solve(tmat, v * beta[..., None], left_side=True, lower=True,
                                    unit_diagonal=True)
    w = lax.linalg.triangular_solve(tmat, k_beta * jnp.exp(gc)[..., None], left_side=True,
                                    lower=True, unit_diagonal=True)
    attn = jnp.einsum('bhnid,bhnjd->bhnij', q, k) * decay
    q_dec = q * jnp.exp(gc)[..., None]
    k_dec = k * jnp.exp(gc[..., -1:] - gc)[..., None]
    chunk_decay = jnp.exp(gc[..., -1])

    def step(state, xs):
        u_c, w_c, q_c, k_c, a_c, d_c = xs
        v_new = u_c - jnp.einsum('bhlk,bhkv->bhlv', w_c, state)
        o_c = jnp.einsum('bhlk,bhkv->bhlv', q_c, state) + jnp.einsum('bhls,bhsv->bhlv', a_c, v_new)
        state = d_c[..., None, None] * state + jnp.einsum('bhlk,bhlv->bhkv', k_c, v_new)
        return state, o_c

    s0 = jnp.zeros((Bsz, H, dk, dv), q.dtype)
    xs = tuple(jnp.moveaxis(t, 2, 0) for t in (u, w, q_dec, k_dec, attn, chunk_decay))
    _, o = lax.scan(step, s0, xs)
    return jnp.moveaxis(o, 0, 2).reshape(Bsz, H, S, dv)


def mlstm_chunked(q, k, v, log_i, log_f):
    Bsz, H, S, dk = q.shape
    dv = v.shape[-1]
    n = S // CHUNK
    q = q.reshape(Bsz, H, n, CHUNK, dk)
    k = k.reshape(Bsz, H, n, CHUNK, dk)
    v = v.reshape(Bsz, H, n, CHUNK, dv)
    log_i = log_i.reshape(Bsz, H, n, CHUNK)
    b = jnp.cumsum(log_f.reshape(Bsz, H, n, CHUNK), axis=-1)
    causal = jnp.tril(jnp.ones((CHUNK, CHUNK), dtype=bool))
    dmat = jnp.where(causal, b[..., :, None] - b[..., None, :] + log_i[..., None, :], -jnp.inf)
    m_intra = jnp.max(dmat, -1)
    pmat = jnp.exp(dmat - m_intra[..., None]) * jnp.einsum('bhnld,bhnsd->bhnls', q, k)
    num_intra = jnp.einsum('bhnls,bhnsv->bhnlv', pmat, v)
    den_intra = jnp.sum(pmat, -1)
    g_kv = b[..., -1:] - b + log_i
    m_kv = jnp.max(g_kv, -1)
    b_last = b[..., -1]

    def step(carry, xs):
        c_st, n_st, m_st = carry
        q_c, k_c, v_c, b_c, mi_c, num_c, den_c, g_c, mkv_c, bl_c = xs
        inter = b_c + m_st[..., None]
        m_t = jnp.maximum(inter, mi_c)
        s_inter = jnp.exp(inter - m_t)
        s_intra = jnp.exp(mi_c - m_t)
        num = s_inter[..., None] * jnp.einsum('bhlk,bhkv->bhlv', q_c, c_st) + s_intra[..., None] * num_c
        den = s_inter * jnp.einsum('bhlk,bhk->bhl', q_c, n_st) + s_intra * den_c
        h = num / jnp.maximum(jnp.abs(den), jnp.exp(-m_t))[..., None]
        m_new = jnp.maximum(bl_c + m_st, mkv_c)
        kw = k_c * jnp.exp(g_c - m_new[..., None])[..., None]
        dec = jnp.exp(bl_c + m_st - m_new)
        c_st = dec[..., None, None] * c_st + jnp.einsum('bhlk,bhlv->bhkv', kw, v_c)
        n_st = dec[..., None] * n_st + jnp.sum(kw, -2)
        return (c_st, n_st, m_new), h

    init = (jnp.zeros((Bsz, H, dk, dv), q.dtype), jnp.zeros((Bsz, H, dk), q.dtype),
            jnp.zeros((Bsz, H), q.dtype))
    xs = tuple(jnp.moveaxis(t, 2, 0) for t in
               (q, k, v, b, m_intra, num_intra, den_intra, g_kv, m_kv, b_last))
    _, h = lax.scan(step, init, xs)
    return jnp.moveaxis(h, 0, 2).reshape(Bsz, H, S, dv)


def deltanet_conformer_mixer(x, w_in, conv_qkv, a_log, dt_bias, o_norm_g, dw_w, dw_b, cn_g, cn_b, w_out):
    Bsz, S, _ = x.shape
    f32 = jnp.float32
    h = x @ w_in
    qkv, z, b_beta, a_dec, glu = jnp.split(
        h, [3 * A_DIM, 4 * A_DIM, 4 * A_DIM + A_HEADS, 4 * A_DIM + 2 * A_HEADS], axis=-1)
    qkv = jax.nn.silu(causal_depthwise_conv(qkv, conv_qkv))
    q, k, v = jnp.split(qkv, 3, axis=-1)
    to_heads = lambda t: t.reshape(Bsz, S, A_HEADS, A_HEAD_DIM).transpose(0, 2, 1, 3).astype(f32)
    q = l2norm(to_heads(q)) * (A_HEAD_DIM ** -0.5)
    k = l2norm(to_heads(k))
    v = to_heads(v)
    beta = jax.nn.sigmoid(b_beta.astype(f32)).transpose(0, 2, 1)
    g = (-jnp.exp(a_log.astype(f32)) * jax.nn.softplus(a_dec.astype(f32) + dt_bias.astype(f32))
         ).transpose(0, 2, 1)
    o = gated_delta_rule(q, k, v, beta, g).transpose(0, 2, 1, 3)
    o = rms_norm(o, o_norm_g.astype(f32)) * jax.nn.silu(
        z.reshape(Bsz, S, A_HEADS, A_HEAD_DIM).astype(f32))
    o_a = o.reshape(Bsz, S, A_DIM).astype(x.dtype)
    g_a, g_b = jnp.split(glu, 2, axis=-1)
    u = g_a * jax.nn.sigmoid(g_b)
    u = causal_depthwise_conv(u, dw_w) + dw_b
    u = jax.nn.silu(layer_norm(u, cn_g, cn_b))
    return jnp.concatenate([o_a, u], axis=-1) @ w_out


def mlstm_mixer(x, w_in, b_i, b_f, norm_g, w_out):
    Bsz, S, _ = x.shape
    f32 = jnp.float32
    h = x @ w_in
    q, k, v, o_pre, i_pre, f_pre = jnp.split(
        h, [C_QK, 2 * C_QK, 2 * C_QK + C_V, 2 * C_QK + 2 * C_V, 2 * C_QK + 2 * C_V + C_HEADS], axis=-1)
    heads = lambda t, d: t.reshape(Bsz, S, C_HEADS, d).transpose(0, 2, 1, 3).astype(f32)
    q = heads(q, C_QK_HEAD) * (C_QK_HEAD ** -0.5)
    k = heads(k, C_QK_HEAD)
    v = heads(v, C_V_HEAD)
    log_i = soft_cap(i_pre.astype(f32) + b_i.astype(f32)).transpose(0, 2, 1)
    log_f = jax.nn.log_sigmoid(soft_cap(f_pre.astype(f32) + b_f.astype(f32))).transpose(0, 2, 1)
    hh = mlstm_chunked(q, k, v, log_i, log_f).transpose(0, 2, 1, 3)
    hh = rms_norm(hh, norm_g.astype(f32).reshape(C_HEADS, C_V_HEAD)).reshape(Bsz, S, C_V)
    hh = (hh * jax.nn.sigmoid(o_pre.astype(f32))).astype(x.dtype)
    return hh @ w_out


def swiglu(x, w_gate, w_up, w_down):
    return (jax.nn.silu(x @ w_gate) * (x @ w_up)) @ w_down


def moe_swiglu(x, w_router, b_router, w_gate, w_up, w_down):
    logits = (x @ w_router).astype(jnp.float32) + b_router.astype(jnp.float32)
    top_val, top_idx = lax.top_k(logits, TOP_K)
    top_w = jax.nn.softmax(top_val, axis=-1)
    combine = jnp.sum(jax.nn.one_hot(top_idx, N_EXPERTS, dtype=jnp.float32) * top_w[..., None], axis=-2)
    combine = combine.astype(x.dtype)
    y = jnp.zeros_like(x)
    for e in range(N_EXPERTS):
        y = y + combine[..., e:e + 1] * swiglu(x, w_gate[e], w_up[e], w_down[e])
    return y


def setup_inputs(seed: int = 0) -> dict:
    key = jax.random.key(seed)
    k = jax.random.split(key, 32)
    f32 = jnp.float32
    nrm = lambda kk, shape, scale: jax.random.normal(kk, shape, f32) * scale
    x = nrm(k[0], (BATCH, SEQ, D_MODEL), 1.0)
    p = nrm(k[1], (DEPTH, BATCH, SEQ, PLE_DIM), 1.0)
    ab_w_in = nrm(k[2], (N_EVEN, D_MODEL, AB_IN), D_MODEL ** -0.5)
    ab_conv_qkv = nrm(k[3], (N_EVEN, SHORT_CONV, 3 * A_DIM), SHORT_CONV ** -0.5)
    ab_a_log = jnp.log(jax.random.uniform(k[4], (N_EVEN, A_HEADS), f32, 1.0, 16.0))
    dt = jnp.exp(jax.random.uniform(k[5], (N_EVEN, A_HEADS), f32, math.log(1e-3), math.log(1e-1)))
    ab_dt_bias = dt + jnp.log(-jnp.expm1(-dt))
    ab_o_norm_g = 1.0 + nrm(k[6], (N_EVEN, A_HEAD_DIM), 0.02)
    ab_dw_w = nrm(k[7], (N_EVEN, B_CONV, B_CHANNELS), B_CONV ** -0.5)
    ab_dw_b = nrm(k[8], (N_EVEN, B_CHANNELS), 0.02)
    ab_cn_g = 1.0 + nrm(k[9], (N_EVEN, B_CHANNELS), 0.02)
    ab_cn_b = nrm(k[10], (N_EVEN, B_CHANNELS), 0.02)
    ab_w_out = nrm(k[11], (N_EVEN, AB_OUT, D_MODEL), AB_OUT ** -0.5 * DN_BETA)
    ffn_w_gate = nrm(k[12], (N_EVEN, D_MODEL, D_FF), D_MODEL ** -0.5)
    ffn_w_up = nrm(k[13], (N_EVEN, D_MODEL, D_FF), D_MODEL ** -0.5)
    ffn_w_down = nrm(k[14], (N_EVEN, D_FF, D_MODEL), D_FF ** -0.5 * DN_BETA)
    c_w_in = nrm(k[15], (N_ODD, D_MODEL, C_IN), D_MODEL ** -0.5)
    c_b_i = -2.0 + nrm(k[16], (N_ODD, C_HEADS), 0.1)
    c_b_f = jnp.linspace(3.0, 6.0, C_HEADS, dtype=f32)[None, :] + nrm(k[17], (N_ODD, C_HEADS), 0.1)
    c_norm_g = 1.0 + nrm(k[18], (N_ODD, C_V), 0.02)
    c_w_out = nrm(k[19], (N_ODD, C_V, D_MODEL), C_V ** -0.5 * DN_BETA)
    moe_w_router = nrm(k[20], (N_ODD, D_MODEL, N_EXPERTS), D_MODEL ** -0.5)
    moe_b_router = nrm(k[21], (N_ODD, N_EXPERTS), 0.01)
    moe_w_gate = nrm(k[22], (N_ODD, N_EXPERTS, D_MODEL, D_FF), D_MODEL ** -0.5)
    moe_w_up = nrm(k[23], (N_ODD, N_EXPERTS, D_MODEL, D_FF), D_MODEL ** -0.5)
    moe_w_down = nrm(k[24], (N_ODD, N_EXPERTS, D_FF, D_MODEL), D_FF ** -0.5 * DN_BETA)
    ln_mix_g = 1.0 + nrm(k[25], (DEPTH, D_MODEL), 0.02)
    ln_mix_b = nrm(k[26], (DEPTH, D_MODEL), 0.02)
    ln_ffn_g = 1.0 + nrm(k[27], (DEPTH, D_MODEL), 0.02)
    ln_ffn_b = nrm(k[28], (DEPTH, D_MODEL), 0.02)
    ple_w_proj = nrm(k[29], (DEPTH, PLE_DIM, D_MODEL), PLE_DIM ** -0.5)
    ple_w_gate = nrm(k[30], (DEPTH, D_MODEL, D_MODEL), D_MODEL ** -0.5)
    return {"x": x, "p": p, "ab_w_in": ab_w_in, "ab_conv_qkv": ab_conv_qkv, "ab_a_log": ab_a_log,
            "ab_dt_bias": ab_dt_bias, "ab_o_norm_g": ab_o_norm_g, "ab_dw_w": ab_dw_w, "ab_dw_b": ab_dw_b,
            "ab_cn_g": ab_cn_g, "ab_cn_b": ab_cn_b, "ab_w_out": ab_w_out, "ffn_w_gate": ffn_w_gate,
            "ffn_w_up": ffn_w_up, "ffn_w_down": ffn_w_down, "c_w_in": c_w_in, "c_b_i": c_b_i,
            "c_b_f": c_b_f, "c_norm_g": c_norm_g, "c_w_out": c_w_out, "moe_w_router": moe_w_router,
            "moe_b_router": moe_b_router, "moe_w_gate": moe_w_gate, "moe_w_up": moe_w_up,
            "moe_w_down": moe_w_down, "ln_mix_g": ln_mix_g, "ln_mix_b": ln_mix_b, "ln_ffn_g": ln_ffn_g,
            "ln_ffn_b": ln_ffn_b, "ple_w_proj": ple_w_proj, "ple_w_gate": ple_w_gate}


def reference(x, p, ab_w_in, ab_conv_qkv, ab_a_log, ab_dt_bias, ab_o_norm_g, ab_dw_w, ab_dw_b,
              ab_cn_g, ab_cn_b, ab_w_out, ffn_w_gate, ffn_w_up, ffn_w_down, c_w_in, c_b_i, c_b_f,
              c_norm_g, c_w_out, moe_w_router, moe_b_router, moe_w_gate, moe_w_up, moe_w_down,
              ln_mix_g, ln_mix_b, ln_ffn_g, ln_ffn_b, ple_w_proj, ple_w_gate):
    for i in range(DEPTH):
        j = i // 2
        if i % 2 == 0:
            mix = deltanet_conformer_mixer(x, ab_w_in[j], ab_conv_qkv[j], ab_a_log[j], ab_dt_bias[j],
                                           ab_o_norm_g[j], ab_dw_w[j], ab_dw_b[j], ab_cn_g[j],
                                           ab_cn_b[j], ab_w_out[j])
            x = layer_norm(DN_ALPHA * x + mix, ln_mix_g[i], ln_mix_b[i])
            ff = swiglu(x, ffn_w_gate[j], ffn_w_up[j], ffn_w_down[j])
        else:
            mix = mlstm_mixer(x, c_w_in[j], c_b_i[j], c_b_f[j], c_norm_g[j], c_w_out[j])
            x = layer_norm(DN_ALPHA * x + mix, ln_mix_g[i], ln_mix_b[i])
            ff = moe_swiglu(x, moe_w_router[j], moe_b_router[j], moe_w_gate[j], moe_w_up[j],
                            moe_w_down[j])
        x = layer_norm(DN_ALPHA * x + ff, ln_ffn_g[i], ln_ffn_b[i])
        x = x + jax.nn.sigmoid(x @ ple_w_gate[i]) * (p[i] @ ple_w_proj[i])
    return x
```

```python
import numpy as np
import concourse.bass as bass
import concourse.mybir as mybir

F32 = mybir.dt.float32
BF16 = mybir.dt.bfloat16
AF = mybir.ActivationFunctionType
ALU = mybir.AluOpType
AX = mybir.AxisListType


class Trk:
    __slots__ = ("writer", "readers")

    def __init__(self):
        self.writer = None
        self.readers = {}


class View:
    __slots__ = ("ap", "trks")

    def __init__(self, ap, trks):
        self.ap = ap
        self.trks = trks


class TB:
    def __init__(self, handle):
        self.h = handle
        self.trk = {}

    def __call__(self, key, *idx):
        t = self.trk.get(key)
        if t is None:
            t = self.trk[key] = Trk()
        ap = self.h[idx] if idx else self.h[:]
        return View(ap, [t])

    def ap(self, key, ap):
        t = self.trk.get(key)
        if t is None:
            t = self.trk[key] = Trk()
        return View(ap, [t])

    def multi(self, keys, *idx):
        ts = []
        for key in keys:
            t = self.trk.get(key)
            if t is None:
                t = self.trk[key] = Trk()
            ts.append(t)
        ap = self.h[idx] if idx else self.h[:]
        return View(ap, ts)


class FW:
    NDMA = 48

    def __init__(self, nc):
        self.nc = nc
        self.eng = {"pe": nc.tensor, "dve": nc.vector, "act": nc.scalar,
                    "pool": nc.gpsimd, "sp": nc.sync}
        self.sem = {}
        self.cnt = {}
        for k in self.eng:
            self.sem[k] = nc.alloc_semaphore("s_" + k)
            self.cnt[k] = 0
        self.dsem = [nc.alloc_semaphore("d%d" % i) for i in range(self.NDMA)]
        self.dcnt = [0] * self.NDMA
        self.dnext = 0
        self.dnext_sw = 0
        self.waited = {k: {} for k in self.eng}
        self.ninst = 0
        self.nwait = 0

    def _sem(self, key):
        return self.sem[key] if isinstance(key, str) else self.dsem[key]

    def _need(self, eng, ev, needs):
        if ev is None:
            return
        key, val = ev
        if key == eng and eng == "pe":
            return
        if self.waited[eng].get(key, 0) >= val:
            return
        if needs.get(key, 0) < val:
            needs[key] = val

    def _deps(self, eng, outs, ins):
        needs = {}
        for v in ins:
            for t in v.trks:
                self._need(eng, t.writer, needs)
        for v in outs:
            for t in v.trks:
                self._need(eng, t.writer, needs)
                for key, ev in t.readers.items():
                    self._need(eng, ev, needs)
        e = self.eng[eng]
        for key, val in needs.items():
            e.wait_ge(self._sem(key), val)
            self.waited[eng][key] = val
            self.nwait += 1

    def _record(self, ev, outs, ins):
        for v in ins:
            for t in v.trks:
                old = t.readers.get(ev[0])
                if old is None or old[1] < ev[1]:
                    t.readers[ev[0]] = ev
        for v in outs:
            for t in v.trks:
                t.writer = ev
                t.readers = {}

    def op(self, eng, fn, outs, ins):
        self._deps(eng, outs, ins)
        inst = fn()
        self.cnt[eng] += 1
        inst.then_inc(self.sem[eng], 1)
        self.ninst += 1
        self._record((eng, self.cnt[eng]), outs, ins)
        return inst

    def dma(self, q, out, in_, **kw):
        half = self.NDMA // 2
        if q == "pool":
            i = half + self.dnext_sw
            self.dnext_sw = (self.dnext_sw + 1) % half
        else:
            i = self.dnext
            self.dnext = (self.dnext + 1) % half
        needs = {}
        if self.dcnt[i] > 0:
            self._need(q, (i, self.dcnt[i]), needs)
        for key, val in needs.items():
            self.eng[q].wait_ge(self._sem(key), val)
            self.waited[q][key] = val
        self._deps(q, [out], [in_])
        inst = self.eng[q].dma_start(out=out.ap, in_=in_.ap, **kw)
        self.dcnt[i] += 16
        inst.then_inc(self.dsem[i], 16)
        self.ninst += 1
        self._record((i, self.dcnt[i]), [out], [in_])
        return inst

    def barrier(self):
        for e in self.eng:
            for o in self.eng:
                if o != e and self.cnt[o] > self.waited[e].get(o, 0):
                    self.eng[e].wait_ge(self.sem[o], self.cnt[o])
                    self.waited[e][o] = self.cnt[o]
            for i in range(self.NDMA):
                if self.dcnt[i] > self.waited[e].get(i, 0):
                    self.eng[e].wait_ge(self.dsem[i], self.dcnt[i])
                    self.waited[e][i] = self.dcnt[i]

    def wait_all(self, eng, views):
        needs = {}
        for v in views:
            for t in v.trks:
                self._need(eng, t.writer, needs)
        for key, val in needs.items():
            self.eng[eng].wait_ge(self._sem(key), val)
            self.waited[eng][key] = val

    def mm(self, out, lhsT, rhs, start=True, stop=True, **kw):
        return self.op("pe", lambda: self.nc.tensor.matmul(out.ap, lhsT.ap, rhs.ap, start=start, stop=stop, **kw),
                       [out], [lhsT, rhs])

    def transpose(self, out, in_, ident):
        return self.op("pe", lambda: self.nc.tensor.matmul(out.ap, in_.ap, ident.ap, start=True, stop=True), [out], [in_, ident])

    def act(self, out, in_, func, bias=None, scale=None, eng="act", accum_out=None):
        ins = [in_]
        kw = {}
        if bias is not None:
            if isinstance(bias, View):
                ins.append(bias); kw["bias"] = bias.ap
            else:
                kw["bias"] = bias
        if scale is not None:
            if isinstance(scale, View):
                ins.append(scale); kw["scale"] = scale.ap
            else:
                kw["scale"] = scale
        outs = [out]
        if accum_out is not None:
            outs.append(accum_out); kw["accum_out"] = accum_out.ap
        return self.op("act", lambda: self.nc.scalar.activation(out.ap, in_.ap, func, **kw), outs, ins)

    def _ve(self, eng):
        return self.eng[eng]

    def tt(self, out, a, b, op, eng="dve"):
        return self.op(eng, lambda: self._ve(eng).tensor_tensor(out.ap, a.ap, b.ap, op), [out], [a, b])

    def ts(self, out, a, s1, op0, s2=None, op1=None, eng="dve", accum_out=None):
        ins = [a]
        s1a = s1
        if isinstance(s1, View):
            ins.append(s1); s1a = s1.ap
        s2a = s2
        if isinstance(s2, View):
            ins.append(s2); s2a = s2.ap
        kw = {}
        outs = [out]
        if op1 is not None:
            kw["op1"] = op1
        if accum_out is not None:
            kw["accum_out"] = accum_out.ap; outs.append(accum_out)
        return self.op(eng, lambda: self._ve(eng).tensor_scalar(out.ap, a.ap, s1a, s2a, op0, **kw), outs, ins)

    def stt(self, out, a, s, b, op0, op1, eng="dve"):
        ins = [a, b]
        sa = s
        if isinstance(s, View):
            ins.append(s); sa = s.ap
        return self.op(eng, lambda: self._ve(eng).scalar_tensor_tensor(out.ap, a.ap, sa, b.ap, op0, op1), [out], ins)

    def copy(self, out, in_, eng="dve"):
        if eng == "act":
            return self.op("act", lambda: self.nc.scalar.copy(out.ap, in_.ap), [out], [in_])
        return self.op(eng, lambda: self._ve(eng).tensor_copy(out.ap, in_.ap), [out], [in_])

    def memset(self, out, val, eng="dve"):
        return self.op(eng, lambda: self._ve(eng).memset(out.ap, val), [out], [])

    def reduce(self, out, in_, op, axis=AX.X, eng="dve"):
        return self.op(eng, lambda: self._ve(eng).tensor_reduce(out.ap, in_.ap, axis, op), [out], [in_])

import contextlib
import numpy as np
import concourse.bass as bass
import concourse.mybir as mybir

D = 1024
TT = 512
NEG = 1.0e4
ALPHA = 4.0 ** 0.25
C_ID, C_ONES, C_NBS, C_NBTI, C_SM01T, C_SEL4, C_RM, C_RB, C_END = 0, 128, 256, 384, 512, 640, 1152, 1664, 2176
C_SEL8 = 0


def make_consts():
    c = np.zeros((128, C_END), np.float32)
    idx = np.arange(128)
    same = (idx[:, None] // 64) == (idx[None, :] // 64)
    c[:, C_ID:C_ID + 128] = np.eye(128)
    c[:, C_ONES:C_ONES + 128] = 1.0
    c[:, C_NBS:C_NBS + 128] = np.where(same & (idx[None, :] < idx[:, None]), 0.0, NEG)
    c[:, C_NBTI:C_NBTI + 128] = np.where(same & (idx[None, :] >= idx[:, None]), 0.0, NEG)
    c[:, C_SM01T:C_SM01T + 128] = np.where(same & (idx[None, :] > idx[:, None]), 1.0, 0.0)
    for h in range(4):
        c[h, C_SEL4 + h * 128:C_SEL4 + (h + 1) * 128] = 1.0
    t = np.arange(512)
    c[0:4, C_RM:C_RM + 512] = np.where(t % 64 == 0, 0.0, 1.0)[None, :]
    c[0:4, C_RB:C_RB + 512] = np.where(t % 64 == 0, -1.0e30, 0.0)[None, :]
    return c


def make_sel8():
    c = np.zeros((8, 1024), np.float32)
    for e in range(8):
        c[e, e * 128:(e + 1) * 128] = 1.0
    return c


class Ctx:
    pass


class Rot:
    def __init__(self, bufs):
        self.bufs = bufs
        self.i = 0

    def get(self):
        b = self.bufs[self.i]
        self.i = (self.i + 1) % len(self.bufs)
        return b


def sl(a, n):
    return slice(a, a + n)


def setup(nc, S, dbg=False):
    cx = Ctx()
    cx.nc = nc
    cx.S = S
    cx.NT = S // TT
    fw = cx.fw = FW(nc)
    di = {}

    def din(name, shape):
        di[name] = TB(nc.dram_tensor(name, list(shape), F32, kind="ExternalInput"))

    din("x", (S, D)); din("p", (2, S, 256))
    din("ab_w_in", (D, 3080)); din("ab_conv_qkv", (4, 1536)); din("ab_a_log", (4,)); din("ab_dt_bias", (4,))
    din("ab_o_norm_g", (128,)); din("ab_dw_w", (31, 512)); din("ab_dw_b", (512,)); din("ab_cn_g", (512,))
    din("ab_cn_b", (512,)); din("ab_w_out", (D, D)); din("ffn_w_gate", (D, 3584)); din("ffn_w_up", (D, 3584))
    din("ffn_w_down", (3584, D)); din("c_w_in", (D, 3080)); din("c_b_i", (4,)); din("c_b_f", (4,))
    din("c_norm_g", (1024,)); din("c_w_out", (D, D)); din("moe_w_router", (D, 8)); din("moe_b_router", (8,))
    din("moe_w_gate", (8, D, 3584)); din("moe_w_up", (8, D, 3584)); din("moe_w_down", (8, 3584, D))
    din("ln_mix_g", (2, D)); din("ln_mix_b", (2, D)); din("ln_ffn_g", (2, D)); din("ln_ffn_b", (2, D))
    din("ple_w_proj", (2, 256, D)); din("ple_w_gate", (2, D, D)); din("cst", (128, C_END)); din("sel8", (8, 1024))
    cx.di = di
    cx.out = TB(nc.dram_tensor("out", [S, D], F32, kind="ExternalOutput"))
    kd = "ExternalOutput" if dbg else "Internal"
    cx.x1 = TB(nc.dram_tensor("x1s", [D, S], F32, kind=kd))
    cx.x2 = TB(nc.dram_tensor("x2s", [D, S], F32, kind=kd))
    cx.x3 = TB(nc.dram_tensor("x3s", [D, S], F32, kind=kd))
    cx.psb = [TB(nc.alloc_psum_tensor("ps%d" % i, [128, 512], F32)) for i in range(8)]
    cx.ps = Rot(cx.psb[0:6])
    cx.psl = Rot(cx.psb[6:8])
    cx.cst = TB(nc.alloc_sbuf_tensor("cst_sb", [128, C_END], F32))
    fw.dma("sp", cx.cst("c"), di["cst"]("c"))
    cx.ident = cx.cst("c", slice(0, 128), sl(C_ID, 128))
    cx.ones = cx.cst("c", slice(0, 128), sl(C_ONES, 128))
    return cx


def C(cx, off, n, rows=128):
    return cx.cst("c", slice(0, rows), sl(off, n))


class Phase:
    uid = 0

    def __init__(self, cx):
        self.cx = cx
        self.es = contextlib.ExitStack()
        self.n = 0

    def sb(self, shape, dtype=F32, name=None):
        self.n += 1
        Phase.uid += 1
        nm = name or ("t%d" % Phase.uid)
        return TB(self.es.enter_context(self.cx.nc.sbuf_tensor(nm, list(shape), dtype)))

    def rot(self, n, shape, dtype=F32):
        return Rot([self.sb(shape, dtype) for _ in range(n)])

    def close(self):
        self.cx.fw.barrier()
        self.es.close()


def bcast_rows(cx, dst, src_rows, sel_off, h, nrows, n=512):
    fw = cx.fw
    ps = cx.ps.get()
    fw.mm(ps("a", slice(0, 128), slice(0, n)), C(cx, sel_off + h * 128, 128, nrows), src_rows)
    fw.copy(dst, ps("a", slice(0, 128), slice(0, n)), eng="act")


def rstd_from(cx, out, in_, scale, eps, tmp):
    fw = cx.fw
    fw.ts(tmp, in_, scale, ALU.mult, eps, ALU.add)
    fw.act(tmp, tmp, AF.Ln)
    fw.act(out, tmp, AF.Exp, scale=-0.5)


def ln_feature_major(cx, ph, y, nchunk, gcol, bcol, outs, eps=1e-5, func=AF.Identity, sq_rot=None, stat=None,
                     TW=512, out_rot=None, out_cb=None):
    fw = cx.fw
    n = float(nchunk * 128)
    A = slice(0, 128); F = slice(0, TW)
    ps1 = cx.ps.get(); ps2 = cx.ps.get()
    for c in range(nchunk):
        fw.mm(ps1("a", A, F), cx.ones, y[c], start=(c == 0), stop=(c == nchunk - 1))
    for c in range(nchunk):
        sq = sq_rot.get()
        fw.act(sq("a"), y[c], AF.Square)
        fw.mm(ps2("a", A, F), cx.ones, sq("a"), start=(c == 0), stop=(c == nchunk - 1))
    mean, msq, rstd, tmp = stat
    fw.act(mean("a"), ps1("a", A, F), AF.Copy, scale=1.0 / n)
    fw.tt(msq("a"), mean("a"), mean("a"), ALU.mult)
    fw.stt(tmp("a"), ps2("a", A, F), 1.0 / n, msq("a"), ALU.mult, ALU.subtract)
    fw.ts(tmp("a"), tmp("a"), eps, ALU.add)
    fw.act(tmp("a"), tmp("a"), AF.Ln)
    fw.act(rstd("a"), tmp("a"), AF.Exp, scale=-0.5)
    for c in range(nchunk):
        t = sq_rot.get()
        fw.tt(t("a"), y[c], mean("a"), ALU.subtract)
        fw.tt(t("a"), t("a"), rstd("a"), ALU.mult, eng="pool")
        if outs is not None:
            fw.act(outs[c], t("a"), func, scale=gcol[c], bias=bcol[c])
        else:
            o = out_rot.get()
            fw.act(o("a"), t("a"), func, scale=gcol[c], bias=bcol[c])
            out_cb(c, o("a"))


def load_cols(cx, ph, dram_ap_rearranged, shape, q="sp"):
    t = ph.sb(shape)
    cx.fw.dma(q, t("a"), View(dram_ap_rearranged, []), allow_slow_non_contiguous=True)
    return t


def load_kcp(cx, ph, w2d, K, Cn):
    t = ph.sb([128, Cn, K])
    for kk in range(K):
        cx.fw.dma("sp", t.ap("a", t.h[:, :, kk]), View(w2d[kk, :].rearrange("(c p) -> p c", p=128), []),
                  allow_slow_non_contiguous=True)
    return t


class StopBuild(Exception):
    pass


def ck(cx, name):
    if getattr(cx, "stop_at", None) == name:
        raise StopBuild(name)


def mark(cx, name, t=None):
    pt = getattr(cx, "prof_tile", None)
    cur = getattr(cx, "_scope", None)
    if cur is not None:
        cx.nc.leave_named_scope(cur) if hasattr(cx.nc, "leave_named_scope") else None
        cx._scope = None
    if pt is not None and t == pt and name is not None:
        cx.nc.enter_named_scope(name)
        cx._scope = name


def load_w_bf16(cx, ph, wap, kchunks, ncols, q="pool"):
    t = ph.sb([128, kchunks, ncols], BF16)
    for kc in range(kchunks):
        cx.fw.dma(q, t("w", slice(0, 128), kc, slice(0, ncols)), View(wap[kc * 128:(kc + 1) * 128, :], []))
    return t


def load_x_tile_T(cx, src_ap, t0, TW, xT, xTb, xin_rot, nchunks=8):
    fw = cx.fw
    for g in range(TW // 128):
        b = xin_rot.get()
        fw.dma("sp", b("a", slice(0, 128), slice(0, nchunks * 128)), View(src_ap[t0 + g * 128:t0 + (g + 1) * 128, :], []))
        ck(cx, "xdma")
        for k0 in range(0, nchunks, 4):
            nk = min(4, nchunks - k0)
            ps = cx.ps.get()
            for k in range(nk):
                fw.transpose(ps("a", slice(0, 128), sl(k * 128, 128)), b("a", slice(0, 128), sl((k0 + k) * 128, 128)), cx.ident)
            ck(cx, "xtr")
            src = ps.ap("a", ps.h[:, 0:nk * 128].rearrange("p (k t) -> p k t", t=128))
            if xT is not None:
                fw.copy(xT.ap("a", xT.h[:, k0:k0 + nk, g * 128:(g + 1) * 128]), src, eng="act")
            ck(cx, "xcp1")
            if xTb is not None and xT is not None:
                fw.copy(xTb.ap("a", xTb.h[:, k0:k0 + nk, g * 128:(g + 1) * 128]), xT.ap("a", xT.h[:, k0:k0 + nk, g * 128:(g + 1) * 128]), eng="dve")
            elif xTb is not None:
                fw.copy(xTb.ap("a", xTb.h[:, k0:k0 + nk, g * 128:(g + 1) * 128]), src, eng="act")


def phaseA(cx, TW=256):
    fw, nc, di = cx.fw, cx.nc, cx.di
    ph = Phase(cx)
    NTW = cx.S // TW
    NG = TW // 128
    NCH = TW // 64
    Wb = load_w_bf16(cx, ph, di["ab_w_in"].h, 8, 3080)
    Wg32 = ph.sb([128, 8, 8])
    for kc in range(8):
        fw.dma("sp", Wg32("a", slice(0, 128), kc, slice(0, 8)), View(di["ab_w_in"].h[kc * 128:(kc + 1) * 128, 2048:2056], []))
    cw = load_kcp(cx, ph, di["ab_conv_qkv"].h, 4, 12)
    dww = load_kcp(cx, ph, di["ab_dw_w"].h, 31, 4)
    dwb = load_cols(cx, ph, di["ab_dw_b"].h[:].rearrange("(c p) -> p c", p=128), [128, 4])
    cng = load_cols(cx, ph, di["ab_cn_g"].h[:].rearrange("(c p) -> p c", p=128), [128, 4])
    cnb = load_cols(cx, ph, di["ab_cn_b"].h[:].rearrange("(c p) -> p c", p=128), [128, 4])
    ong = load_cols(cx, ph, di["ab_o_norm_g"].h[:].rearrange("(c p) -> p c", p=128), [128, 1])
    lng = load_cols(cx, ph, di["ln_mix_g"].h[0, :].rearrange("(c p) -> p c", p=128), [128, 8])
    lnb = load_cols(cx, ph, di["ln_mix_b"].h[0, :].rearrange("(c p) -> p c", p=128), [128, 8])
    hp = ph.sb([4, 4])
    fw.dma("sp", hp("a", slice(0, 4), slice(0, 1)), View(di["ab_a_log"].h[:].rearrange("(p o) -> p o", o=1), []), allow_slow_non_contiguous=True)
    fw.dma("sp", hp("a", slice(0, 4), slice(1, 2)), View(di["ab_dt_bias"].h[:].rearrange("(p o) -> p o", o=1), []), allow_slow_non_contiguous=True)
    fw.act(hp("a", slice(0, 4), slice(2, 3)), hp("a", slice(0, 4), slice(0, 1)), AF.Exp)
    fw.ts(hp("a", slice(0, 4), slice(2, 3)), hp("a", slice(0, 4), slice(2, 3)), -1.0, ALU.mult)
    ck(cx, "w")
    halo_q = ph.sb([128, 12, 3]); fw.memset(halo_q("a"), 0.0)
    ucv = [ph.sb([128, 30 + TW]) for _ in range(4)]
    for c in range(4):
        fw.memset(ucv[c]("a", slice(0, 128), slice(0, 30)), 0.0)
    Sst = [ph.sb([128, 128]) for _ in range(4)]
    for h in range(4):
        fw.memset(Sst[h]("a"), 0.0)
    ck(cx, "mem")
    xin_rot = ph.rot(2, [128, 1024])
    xT = ph.sb([128, 8, TW]); xTb = ph.sb([128, 8, TW], BF16)
    cvr = ph.rot(2, [128, 3 + TW])
    qkv = ph.sb([128, 12, TW])
    zs = ph.sb([128, 4, TW], BF16)
    tmpr = ph.rot(6, [128, TW])
    GF = ph.sb([4, 8, TW])
    TM = [ph.sb([128, 5, 4]) for _ in range(NG)]
    BIG1 = ph.sb([128, 8, TW]); BIG2 = ph.sb([128, 8, TW])
    gcB = lambda h, cols: BIG1(("c", h), slice(0, 128), h, cols)
    nbB = lambda h, cols: BIG1(("c", 4 + h), slice(0, 128), 4 + h, cols)
    egB = lambda h, cols: BIG2(("c", h), slice(0, 128), h, cols)
    acc = lambda c: BIG2(("c", 4 + c), slice(0, 128), 4 + c, slice(0, TW))
    yT = lambda c: BIG1(("c", c), slice(0, 128), c, slice(0, TW))
    cdec = ph.sb([128, 4, NCH])
    usb = ph.sb([128, 4, TW], BF16)
    oab = ph.sb([128, 4, TW], BF16)
    oT = ph.sb([128, 4, TW])
    stat = [ph.sb([128, TW]) for _ in range(4)]
    x1o = ph.rot(3, [128, TW])
    mk4 = lambda shape=(128, 128): [ph.sb(list(shape)) for _ in range(4)]
    U_nk = mk4(); U_kd = mk4(); U_vb = mk4(); U_ds = mk4(); U_dt = mk4(); U_t1 = mk4(); U_at = mk4()
    U_nw = mk4(); U_qd = mk4(); U_vn = mk4()
    U_PQ = [[ph.sb([128, 256]) for _ in range(2)] for _ in range(4)]
    U_X = [[ph.sb([128, 128]) for _ in range(2)] for _ in range(4)]
    worot = ph.rot(2, [128, 8, 128], BF16)
    A = slice(0, 128)
    F = slice(0, TW)
    G4 = slice(0, 4)

    for t in range(NTW):
        t0 = t * TW
        load_x_tile_T(cx, di["x"].h, t0, TW, xT, xTb, xin_rot)
        ck(cx, "x")
        def inproj(c0):
            ps = cx.ps.get()
            for kc in range(8):
                fw.mm(ps("a", A, F), Wb("w", A, kc, sl(c0, 128)), xTb("a", A, kc, F), start=(kc == 0), stop=(kc == 7))
            return ps
        def gate_gen():
            psb_ = cx.ps.get(); psd_ = cx.ps.get()
            for kc in range(8):
                fw.mm(psb_("a", G4, F), Wg32("a", A, kc, slice(0, 4)), xT("a", A, kc, F), start=(kc == 0), stop=(kc == 7))
            for kc in range(8):
                fw.mm(psd_("a", G4, F), Wg32("a", A, kc, slice(4, 8)), xT("a", A, kc, F), start=(kc == 0), stop=(kc == 7))
            gf = lambda q: GF("a", G4, q, F)
            fw.act(gf(0), psb_("a", G4, F), AF.Sigmoid)
            fw.ts(gf(3), gf(0), -1.0, ALU.mult)
            fw.act(gf(7), psd_("a", G4, F), AF.Identity, bias=hp("a", G4, slice(1, 2)))
            yield
            fw.ts(gf(6), gf(7), 0.0, ALU.max)
            fw.stt(gf(5), gf(6), -2.0, gf(7), ALU.mult, ALU.add)
            fw.act(gf(5), gf(5), AF.Exp)
            fw.ts(gf(5), gf(5), 1.0, ALU.add)
            fw.act(gf(5), gf(5), AF.Ln)
            yield
            fw.tt(gf(5), gf(5), gf(6), ALU.add)
            fw.ts(gf(1), gf(5), hp("a", G4, slice(2, 3)), ALU.mult)
            rm = C(cx, C_RM, TW, 4)
            fw.op("dve", lambda: nc.vector.tensor_tensor_scan(gf(2).ap, rm.ap, gf(1).ap, 0.0, ALU.mult, ALU.add),
                  [gf(2)], [rm, gf(1)])
            fw.act(gf(4), gf(2), AF.Exp)
            fw.tt(gf(5), gf(3), gf(4), ALU.mult)
            gc3 = GF.ap("a", GF.h[0:4, 2, :].rearrange("p (c l) -> p c l", l=64))
            gcl = GF.ap("a", GF.h[0:4, 2, :].rearrange("p (c l) -> p c l", l=64)[:, :, 63:64].to_broadcast([4, NCH, 64]))
            kds3 = GF.ap("a", GF.h[0:4, 6, :].rearrange("p (c l) -> p c l", l=64))
            fw.tt(kds3, gcl, gc3, ALU.subtract)
            yield
            fw.act(gf(6), gf(6), AF.Exp)
            for g in range(NG):
                ps = cx.ps.get()
                for qi, q in enumerate((2, 3, 5, 0, 6)):
                    fw.mm(ps("a", A, sl(qi * 4, 4)), GF("a", G4, q, sl(g * 128, 128)), C(cx, C_ID, 4, 4))
                fw.copy(TM[g]("a"), ps.ap("a", ps.h[:, 0:20].rearrange("p (q h) -> p q h", h=4)))
                yield
            for h in range(4):
                bcast_rows(cx, gcB(h, F), gf(2), C_SEL4, h, 4, TW)
                bcast_rows(cx, nbB(h, F), gf(3), C_SEL4, h, 4, TW)
                bcast_rows(cx, egB(h, F), gf(4), C_SEL4, h, 4, TW)
                yield
            ps = cx.ps.get()
            for h in range(4):
                fw.mm(ps("a", A, sl(h * NCH, NCH)), C(cx, C_SEL4 + h * 128, 128, 4), GF.ap("a", GF.h[0:4, 4, 63::64]))
            fw.copy(cdec("a"), ps.ap("a", ps.h[:, 0:4 * NCH].rearrange("p (h c) -> p h c", c=NCH)))
            yield
        gg = gate_gen()
        for c in range(12):
            next(gg, None); next(gg, None)
            ps = inproj(c * 128)
            cv = cvr.get()
            fw.copy(cv("a", A, slice(0, 3)), halo_q("a", A, c, slice(0, 3)), eng="pool")
            fw.copy(cv("a", A, slice(3, 3 + TW)), ps("a", A, F), eng="act")
            fw.copy(halo_q("a", A, c, slice(0, 3)), cv("a", A, slice(TW, TW + 3)), eng="pool")
            tq = tmpr.get()
            fw.ts(tq("a"), cv("a", A, slice(0, TW)), cw("a", A, c, slice(0, 1)), ALU.mult)
            for k in range(1, 4):
                fw.stt(tq("a"), cv("a", A, slice(k, k + TW)), cw("a", A, c, slice(k, k + 1)), tq("a"), ALU.mult, ALU.add)
            fw.act(qkv("a", A, c, F), tq("a"), AF.Silu)
        for _ in gg:
            pass
        ck(cx, "conv")
        for c in range(8):
            sq = tmpr.get()
            fw.act(sq("a"), qkv("a", A, c, F), AF.Square)
            ps = cx.ps.get()
            fw.mm(ps("a", A, F), cx.ones, sq("a"))
            rn = tmpr.get()
            rstd_from(cx, rn("a"), ps("a", A, F), 1.0, 1e-6, sq("a"))
            scale = (128.0 ** -0.5) if c < 4 else 1.0
            fw.stt(qkv("a", A, c, F), qkv("a", A, c, F), scale, rn("a"), ALU.mult, ALU.mult)
        for c in range(4):
            ps = inproj(1536 + c * 128)
            fw.act(zs("a", A, c, F), ps("a", A, F), AF.Silu)
        ck(cx, "l2")
        ck(cx, "tm")
        def conf_gen():
            for c in range(4):
                psa = inproj(2056 + c * 128)
                psg = inproj(2568 + c * 128)
                sg = tmpr.get()
                fw.act(sg("a"), psg("a", A, F), AF.Sigmoid)
                fw.tt(ucv[c]("a", A, slice(30, 30 + TW)), psa("a", A, F), sg("a"), ALU.mult)
                yield
                fw.ts(acc(c), ucv[c]("a", A, slice(0, TW)), dww("a", A, c, slice(0, 1)), ALU.mult, dwb("a", A, slice(c, c + 1)), ALU.add)
                for k in range(1, 31):
                    fw.stt(acc(c), ucv[c]("a", A, slice(k, k + TW)), dww("a", A, c, slice(k, k + 1)), acc(c), ALU.mult, ALU.add)
                    if k % 3 == 0:
                        yield
                fw.copy(ucv[c]("a", A, slice(0, 30)), ucv[c]("a", A, slice(TW, TW + 30)), eng="pool")
                yield
            ln_feature_major(cx, ph, [acc(c) for c in range(4)], 4,
                             [cng("a", A, slice(c, c + 1)) for c in range(4)], [cnb("a", A, slice(c, c + 1)) for c in range(4)],
                             [usb("a", A, c, F) for c in range(4)], func=AF.Silu, sq_rot=tmpr, stat=stat, TW=TW)
            yield
        cg = conf_gen()
        pump = lambda n=1: [next(cg, None) for _ in range(n)]
        ck(cx, "conf")
        H4 = range(4)
        for g in range(NG):
            gs = sl(g * 128, 128)
            pso = cx.psl.get()
            qn = [qkv("a", A, h, gs) for h in H4]; kn = [qkv("a", A, 4 + h, gs) for h in H4]; vv = [qkv("a", A, 8 + h, gs) for h in H4]
            gcT = [TM[g]("a", A, 0, slice(h, h + 1)) for h in H4]; nbT = [TM[g]("a", A, 1, slice(h, h + 1)) for h in H4]
            nbegT = [TM[g]("a", A, 2, slice(h, h + 1)) for h in H4]; betaT = [TM[g]("a", A, 3, slice(h, h + 1)) for h in H4]
            kdsT = [TM[g]("a", A, 4, slice(h, h + 1)) for h in H4]
            Pv = lambda bb: bb("a", A, slice(0, 128))
            Qv = lambda bb: bb("a", A, slice(128, 256))
            pst = [None] * 4
            for h in H4:
                ps = pst[h] = cx.ps.get()
                fw.transpose(ps("a", A, slice(0, 128)), kn[h], cx.ident)
                fw.transpose(ps("a", A, slice(128, 256)), vv[h], cx.ident)
            for h in H4:
                ps = pst[h]
                fw.act(U_nk[h]("a"), ps("a", A, slice(0, 128)), AF.Identity, scale=nbegT[h])
                fw.act(U_kd[h]("a"), ps("a", A, slice(0, 128)), AF.Identity, scale=kdsT[h])
                fw.act(U_vb[h]("a"), ps("a", A, slice(128, 256)), AF.Identity, scale=betaT[h])
            for h in H4:
                fw.stt(U_ds[h]("a"), gcB(h, gs), gcT[h], C(cx, C_NBS, 128), ALU.subtract, ALU.add)
                fw.stt(U_dt[h]("a"), gcB(h, gs), gcT[h], C(cx, C_NBTI, 128), ALU.subtract, ALU.subtract)
            pump(2)
            for h in H4:
                fw.act(U_ds[h]("a"), U_ds[h]("a"), AF.Exp, scale=-1.0)
                fw.act(U_dt[h]("a"), U_dt[h]("a"), AF.Exp)
            pump(2)
            for h in H4:
                fw.tt(U_t1[h]("a"), U_dt[h]("a"), C(cx, C_SM01T, 128), ALU.mult, eng="pool")
                fw.tt(U_t1[h]("a"), U_t1[h]("a"), nbB(h, gs), ALU.mult, eng="pool")
                fw.tt(U_qd[h]("a"), qn[h], egB(h, gs), ALU.mult, eng="pool")
            psA = [None] * 4
            for h in H4:
                psA[h] = cx.ps.get()
                fw.mm(psA[h]("a", A, slice(0, 128)), kn[h], kn[h])
                fw.mm(psA[h]("a", A, slice(128, 256)), kn[h], qn[h])
            cur = [0] * 4
            for h in H4:
                PQ = U_PQ[h][0]
                fw.stt(Pv(PQ), psA[h]("a", A, slice(0, 128)), nbT[h], U_ds[h]("a"), ALU.mult, ALU.mult)
                fw.tt(Qv(PQ), psA[h]("a", A, slice(0, 128)), U_t1[h]("a"), ALU.mult)
                fw.tt(U_at[h]("a"), psA[h]("a", A, slice(128, 256)), U_dt[h]("a"), ALU.mult)
            for h in H4:
                fw.tt(U_X[h][0]("a"), Qv(U_PQ[h][0]), cx.ident, ALU.add, eng="pool")
            pump(2)
            for lvl in range(1, 6):
                ps1 = [None] * 4; ps2 = [None] * 4
                for h in H4:
                    PQ = U_PQ[h][cur[h]]
                    ps1[h] = cx.ps.get()
                    fw.mm(ps1[h]("a", A, slice(0, 128)), Qv(PQ), Pv(PQ))
                    if lvl < 5:
                        fw.mm(ps1[h]("a", A, slice(128, 256)), Pv(PQ), Qv(PQ))
                for h in H4:
                    PQn = U_PQ[h][1 - cur[h]]
                    if lvl < 5:
                        fw.copy(PQn("a"), ps1[h]("a", A, slice(0, 256)), eng="act")
                    else:
                        fw.copy(Pv(PQn), ps1[h]("a", A, slice(0, 128)), eng="act")
                for h in H4:
                    PQn = U_PQ[h][1 - cur[h]]
                    ps2[h] = cx.ps.get()
                    fw.mm(ps2[h]("a", A, slice(0, 128)), Pv(PQn), U_X[h][cur[h]]("a"))
                for h in H4:
                    fw.tt(U_X[h][1 - cur[h]]("a"), ps2[h]("a", A, slice(0, 128)), U_X[h][cur[h]]("a"), ALU.add)
                    cur[h] = 1 - cur[h]
                pump(2)
            Xt = [U_X[h][cur[h]]("a") for h in H4]
            psn = [None] * 4
            for h in H4:
                psn[h] = cx.ps.get()
                fw.mm(psn[h]("a", A, slice(0, 128)), U_nk[h]("a"), Xt[h])
            for h in H4:
                fw.copy(U_nw[h]("a"), psn[h]("a", A, slice(0, 128)), eng="act")
            pump(2)
            for c in range(2):
                cs = sl(c * 64, 64)
                ci = g * 2 + c
                psC = [None] * 4; psS = [None] * 4
                for h in H4:
                    psC[h] = cx.ps.get()
                    fw.mm(psC[h]("a", A, slice(0, 128)), Xt[h], U_vb[h]("a"), start=True, stop=False)
                    fw.mm(psC[h]("a", A, slice(0, 128)), U_nw[h]("a"), Sst[h]("a"), start=False, stop=True)
                for h in H4:
                    fw.copy(U_vn[h]("a", cs, slice(0, 128)), psC[h]("a", cs, slice(0, 128)), eng="act")
                for h in H4:
                    ocols = sl(h * 128 + c * 64, 64)
                    fw.mm(pso("a", A, ocols), Sst[h]("a"), U_qd[h]("a", A, cs), start=True, stop=False)
                    fw.mm(pso("a", A, ocols), U_vn[h]("a", cs, slice(0, 128)), U_at[h]("a", cs, cs), start=False, stop=True)
                for h in H4:
                    psS[h] = cx.ps.get()
                    fw.mm(psS[h]("a", A, slice(0, 128)), U_kd[h]("a", cs, slice(0, 128)), U_vn[h]("a", cs, slice(0, 128)))
                for h in H4:
                    fw.stt(Sst[h]("a"), Sst[h]("a"), cdec("a", A, h, slice(ci, ci + 1)), psS[h]("a", A, slice(0, 128)), ALU.mult, ALU.add)
                pump(2)
            fw.copy(oT.ap("a", oT.h[:, :, g * 128:(g + 1) * 128]), pso.ap("a", pso.h[:, :].rearrange("p (h t) -> p h t", t=128)), eng="act")
        for _ in cg:
            pass
        ck(cx, "dn")
        for h in range(4):
            sq = tmpr.get()
            fw.act(sq("a"), oT("a", A, h, F), AF.Square)
            ps = cx.ps.get()
            fw.mm(ps("a", A, F), cx.ones, sq("a"))
            rn = tmpr.get()
            rstd_from(cx, rn("a"), ps("a", A, F), 1.0 / 128.0, 1e-6, sq("a"))
            fw.tt(rn("a"), rn("a"), oT("a", A, h, F), ALU.mult)
            fw.tt(rn("a"), rn("a"), zs("a", A, h, F), ALU.mult, eng="pool")
            fw.act(oab("a", A, h, F), rn("a"), AF.Identity, scale=ong("a", A, slice(0, 1)))
        ck(cx, "rms")
        for dc in range(8):
            wo = worot.get()
            fw.dma("pool", wo("a"), View(di["ab_w_out"].h[:, dc * 128:(dc + 1) * 128].rearrange("(k p) c -> p k c", p=128), []))
            ps = cx.ps.get()
            for kc in range(8):
                rhs = oab("a", A, kc, F) if kc < 4 else usb("a", A, kc - 4, F)
                fw.mm(ps("a", A, F), wo("a", A, kc, slice(0, 128)), rhs, start=(kc == 0), stop=(kc == 7))
            fw.stt(yT(dc), xT("a", A, dc, F), ALPHA, ps("a", A, F), ALU.mult, ALU.add)
        xo = [x1o.get() if c < 0 else None for c in range(8)]
        outv = []
        def ln_out_cb(c):
            b = x1o.get()
            return b
        bufs = []
        ln_feature_major(cx, ph, [yT(c) for c in range(8)], 8,
                         [lng("a", A, slice(c, c + 1)) for c in range(8)], [lnb("a", A, slice(c, c + 1)) for c in range(8)],
                         None, sq_rot=tmpr, stat=stat, TW=TW, out_rot=x1o,
                         out_cb=lambda c, v: fw.dma("sp", cx.x1((t, c), sl(c * 128, 128), sl(t0, TW)), v))
    ph.close()


def phaseC(cx, TW=256):
    fw, nc, di = cx.fw, cx.nc, cx.di
    ph = Phase(cx)
    NTW = cx.S // TW; NG = TW // 128; NCH = TW // 64
    A = slice(0, 128); F = slice(0, TW); G4 = slice(0, 4)
    Wb = load_w_bf16(cx, ph, di["c_w_in"].h, 8, 3080)
    Wo = load_w_bf16(cx, ph, di["c_w_out"].h, 8, 1024)
    Wg32 = ph.sb([128, 8, 8])
    for kc in range(8):
        fw.dma("sp", Wg32("a", A, kc, slice(0, 8)), View(di["c_w_in"].h[kc * 128:(kc + 1) * 128, 3072:3080], []))
    ng = load_cols(cx, ph, di["c_norm_g"].h[0:1024].rearrange("(c p) -> p c", p=128), [128, 8])
    lng = load_cols(cx, ph, di["ln_mix_g"].h[1, :].rearrange("(c p) -> p c", p=128), [128, 8])
    lnb = load_cols(cx, ph, di["ln_mix_b"].h[1, :].rearrange("(c p) -> p c", p=128), [128, 8])
    hp = ph.sb([4, 4])
    fw.dma("sp", hp("a", G4, slice(0, 1)), View(di["c_b_i"].h[:].rearrange("(p o) -> p o", o=1), []), allow_slow_non_contiguous=True)
    fw.dma("sp", hp("a", G4, slice(1, 2)), View(di["c_b_f"].h[:].rearrange("(p o) -> p o", o=1), []), allow_slow_non_contiguous=True)
    fw.ts(hp("a", G4, slice(2, 4)), hp("a", G4, slice(0, 2)), 1.0 / 15.0, ALU.mult)
    Caug = [ph.sb([128, 384]) for _ in range(4)]
    for h in range(4):
        fw.memset(Caug[h]("a"), 0.0)
    mcar = ph.sb([4, 1]); fw.memset(mcar("a"), 0.0)
    vaug = ph.sb([128, NG, 4, 384])
    fw.memset(vaug("a"), 1.0)
    xT = ph.sb([128, 8, TW]); xTb = ph.sb([128, 8, TW], BF16)
    qT = ph.sb([128, 4, TW]); kT = ph.sb([128, 4, TW])
    ktok = ph.sb([128, NG, 512])
    og = ph.sb([128, 8, TW], BF16)
    GF = ph.sb([4, 12, TW])
    SM = ph.sb([4, 8, NCH])
    TMc = [ph.sb([128, 2, 4]) for _ in range(NG)]
    B1 = ph.sb([128, 8, TW]); B2 = ph.sb([128, 8, TW])
    cmB = lambda h, cols: B1("a", A, h, cols)
    siB = lambda h, cols: B1("a", A, 4 + h, cols)
    saB = lambda h, cols: B2("a", A, h, cols)
    emB = lambda h, cols: B2("a", A, 4 + h, cols)
    decB = ph.sb([128, 4, NCH])
    hT = ph.sb([128, 8, TW])
    hb = ph.sb([128, 8, TW], BF16)
    tmpr = ph.rot(6, [128, TW])
    stat = [ph.sb([128, TW]) for _ in range(4)]
    x3o = ph.rot(3, [128, TW])
    mk4 = lambda shape=(128, 128): [ph.sb(list(shape)) for _ in range(4)]
    U_E = mk4(); U_kw = mk4(); U_pm = mk4(); U_dn = mk4()
    U_t1 = mk4((128, 384)); U_t2 = mk4((128, 384))
    psR = [cx.psb[6], cx.psb[7], cx.psb[4], cx.psb[5]]
    cx.ps = Rot(cx.psb[0:4])
    siB3 = lambda h, cols: B1.ap("a", B1.h[:, 4 + h:5 + h, cols].to_broadcast([128, 3, 128]))
    saB3 = lambda h, cols: B2.ap("a", B2.h[:, h:h + 1, cols].to_broadcast([128, 3, 128]))
    yv = lambda c: B1("a", A, c, F)

    for t in range(NTW):
        t0 = t * TW
        for c in range(8):
            fw.dma("sp", xT("a", A, c, F), cx.x2((t, c), sl(c * 128, 128), sl(t0, TW)))
        fw.copy(xTb("a"), xT("a"), eng="act")

        def inproj(c0):
            ps = cx.ps.get()
            for kc in range(8):
                fw.mm(ps("a", A, F), Wb("w", A, kc, sl(c0, 128)), xTb("a", A, kc, F), start=(kc == 0), stop=(kc == 7))
            return ps
        def gate_gen():
            psi_ = cx.ps.get(); psf_ = cx.ps.get()
            for kc in range(8):
                fw.mm(psi_("a", G4, F), Wg32("a", A, kc, slice(0, 4)), xT("a", A, kc, F), start=(kc == 0), stop=(kc == 7))
            for kc in range(8):
                fw.mm(psf_("a", G4, F), Wg32("a", A, kc, slice(4, 8)), xT("a", A, kc, F), start=(kc == 0), stop=(kc == 7))
            gf = lambda q: GF("a", G4, q, F)
            g3 = lambda q: GF.ap("a", GF.h[0:4, q, :].rearrange("p (c l) -> p c l", l=64))
            smv = lambda q, a=0, n=None: SM("a", G4, q, slice(a, NCH if n is None else a + n))
            smb = lambda q: SM.ap("a", SM.h[0:4, q, :].rearrange("p (c o) -> p c o", o=1).to_broadcast([4, NCH, 64]))
            fw.act(gf(0), psi_("a", G4, F), AF.Tanh, scale=1.0 / 15.0, bias=hp("a", G4, slice(2, 3)))
            fw.ts(gf(0), gf(0), 15.0, ALU.mult)
            fw.act(gf(1), psf_("a", G4, F), AF.Tanh, scale=1.0 / 15.0, bias=hp("a", G4, slice(3, 4)))
            fw.act(gf(1), gf(1), AF.Exp, scale=-15.0)
            yield
            fw.ts(gf(1), gf(1), 1.0, ALU.add)
            fw.act(gf(1), gf(1), AF.Ln)
            fw.ts(gf(1), gf(1), -1.0, ALU.mult)
            rm = C(cx, C_RM, TW, 4); rb = C(cx, C_RB, TW, 4)
            fw.op("dve", lambda: nc.vector.tensor_tensor_scan(gf(2).ap, rm.ap, gf(1).ap, 0.0, ALU.mult, ALU.add), [gf(2)], [rm, gf(1)])
            yield
            fw.tt(gf(3), gf(0), gf(2), ALU.subtract)
            fw.op("dve", lambda: nc.vector.tensor_tensor_scan(gf(4).ap, rb.ap, gf(3).ap, 0.0, ALU.add, ALU.max), [gf(4)], [rb, gf(3)])
            fw.tt(gf(5), gf(2), gf(4), ALU.add)
            fw.copy(smv(0), GF.ap("a", GF.h[0:4, 2, 63::64]))
            fw.tt(smv(1), smv(0), GF.ap("a", GF.h[0:4, 4, 63::64]), ALU.add)
            fw.op("dve", lambda: nc.vector.tensor_tensor_scan(smv(2).ap, smv(0).ap, smv(1).ap, mcar("a").ap, ALU.add, ALU.max),
                  [smv(2)], [smv(0), smv(1), mcar("a")])
            fw.copy(smv(3, 0, 1), mcar("a"))
            if NCH > 1:
                fw.copy(smv(3, 1, NCH - 1), smv(2, 0, NCH - 1))
            fw.copy(mcar("a"), smv(2, NCH - 1, 1))
            yield
            fw.tt(g3(6), g3(2), smb(3), ALU.add)
            fw.tt(gf(7), gf(6), gf(5), ALU.max)
            fw.tt(gf(8), gf(6), gf(7), ALU.subtract); fw.act(gf(8), gf(8), AF.Exp)
            fw.tt(gf(9), gf(5), gf(7), ALU.subtract); fw.act(gf(9), gf(9), AF.Exp)
            fw.act(gf(10), gf(7), AF.Exp, scale=-1.0)
            fw.tt(smv(5), smv(0), smv(2), ALU.subtract)
            fw.tt(g3(11), g3(3), smb(5), ALU.add); fw.act(gf(11), gf(11), AF.Exp)
            fw.tt(smv(4), smv(5), smv(3), ALU.add); fw.act(smv(4), smv(4), AF.Exp)
            yield
            for g in range(NG):
                ps = cx.ps.get()
                for qi, q in enumerate((3, 11)):
                    fw.mm(ps("a", A, sl(qi * 4, 4)), GF("a", G4, q, sl(g * 128, 128)), C(cx, C_ID, 4, 4))
                fw.copy(TMc[g]("a"), ps.ap("a", ps.h[:, 0:8].rearrange("p (q h) -> p q h", h=4)))
            for h in range(4):
                bcast_rows(cx, cmB(h, F), gf(4), C_SEL4, h, 4, TW)
                bcast_rows(cx, siB(h, F), gf(8), C_SEL4, h, 4, TW)
                bcast_rows(cx, saB(h, F), gf(9), C_SEL4, h, 4, TW)
                bcast_rows(cx, emB(h, F), gf(10), C_SEL4, h, 4, TW)
                yield
            ps = cx.ps.get()
            for h in range(4):
                fw.mm(ps("a", A, sl(h * NCH, NCH)), C(cx, C_SEL4 + h * 128, 128, 4), smv(4))
            fw.copy(decB("a"), ps.ap("a", ps.h[:, 0:4 * NCH].rearrange("p (h c) -> p h c", c=NCH)))
            yield
        gg = gate_gen()
        for h in range(4):
            next(gg, None); next(gg, None)
            ps = inproj(h * 128)
            fw.act(qT("a", A, h, F), ps("a", A, F), AF.Copy, scale=128.0 ** -0.5)
            ps = inproj(512 + h * 128)
            fw.copy(kT("a", A, h, F), ps("a", A, F), eng="act")
        for c in range(8):
            next(gg, None); next(gg, None)
            ps = inproj(2048 + c * 128)
            fw.act(og("a", A, c, F), ps("a", A, F), AF.Sigmoid)
        for g in range(NG):
            gs = sl(g * 128, 128)
            next(gg, None); next(gg, None)
            ps = cx.ps.get()
            for kc in range(8):
                fw.mm(ps("a"), xTb("a", A, kc, gs), Wb("w", A, kc, slice(512, 1024)), start=(kc == 0), stop=(kc == 7))
            fw.copy(ktok("a", A, g, slice(0, 512)), ps("a"), eng="act")
            for half in range(2):
                ps = cx.ps.get()
                for kc in range(8):
                    fw.mm(ps("a"), xTb("a", A, kc, gs), Wb("w", A, kc, sl(1024 + half * 512, 512)), start=(kc == 0), stop=(kc == 7))
                fw.copy(vaug.ap("a", vaug.h[:, g, 2 * half:2 * half + 2, 0:256]),
                        ps.ap("a", ps.h[:, :].rearrange("p (h d) -> p h d", d=256)), eng="act")
        for _ in gg:
            pass
        H4 = range(4)
        for g in range(NG):
            gs = sl(g * 128, 128)
            aT = [TMc[g]("a", A, 0, slice(h, h + 1)) for h in H4]; ekT = [TMc[g]("a", A, 1, slice(h, h + 1)) for h in H4]
            for h in H4:
                fw.stt(U_E[h]("a"), cmB(h, gs), aT[h], C(cx, C_NBTI, 128), ALU.subtract, ALU.add)
            for h in H4:
                fw.act(U_E[h]("a"), U_E[h]("a"), AF.Exp, scale=-1.0)
                fw.act(U_kw[h]("a"), ktok("a", A, g, sl(h * 128, 128)), AF.Identity, scale=ekT[h])
            psQ = [None] * 4
            for h in H4:
                psQ[h] = cx.ps.get()
                fw.mm(psQ[h]("a", A, slice(0, 128)), kT("a", A, h, gs), qT("a", A, h, gs))
            for h in H4:
                fw.tt(U_pm[h]("a"), psQ[h]("a", A, slice(0, 128)), U_E[h]("a"), ALU.mult)
            for c in range(2):
                cs = sl(c * 64, 64)
                ci = g * 2 + c
                for h in H4:
                    for j in range(3):
                        fw.mm(psR[h]("a", A, sl(j * 128 + c * 64, 64)), Caug[h]("a", A, sl(j * 128, 128)),
                              qT("a", A, h, sl(g * 128 + c * 64, 64)))
                psS = [None] * 4
                for h in H4:
                    psS[h] = cx.ps.get()
                    fw.mm(psS[h]("a", A, slice(0, 384)), U_kw[h]("a", cs, slice(0, 128)), vaug("a", cs, g, h, slice(0, 384)))
                for h in H4:
                    fw.stt(Caug[h]("a"), Caug[h]("a"), decB("a", A, h, slice(ci, ci + 1)), psS[h]("a", A, slice(0, 384)), ALU.mult, ALU.add)
            for h in H4:
                psI = cx.ps.get()
                for j in range(3):
                    fw.mm(psI("a", A, sl(j * 128, 128)), vaug("a", A, g, h, sl(j * 128, 128)), U_pm[h]("a"))
                t1 = U_t1[h]; t2 = U_t2[h]
                fw.tt(t1("a"), psR[h]("a", A, slice(0, 384)), siB3(h, gs), ALU.mult)
                fw.tt(t2("a"), psI("a", A, slice(0, 384)), saB3(h, gs), ALU.mult)
                fw.tt(t1("a"), t1("a"), t2("a"), ALU.add, eng="pool")
            for h in H4:
                t1 = U_t1[h]; dn = U_dn[h]
                c2 = t1("a", A, slice(256, 384))
                fw.ts(dn("a"), c2, -1.0, ALU.mult, eng="pool")
                fw.tt(dn("a"), dn("a"), c2, ALU.max)
                fw.tt(dn("a"), dn("a"), emB(h, gs), ALU.max)
                fw.op("dve", lambda: nc.vector.reciprocal(dn("a").ap, dn("a").ap), [dn("a")], [dn("a")])
            for h in H4:
                t1 = U_t1[h]; dn = U_dn[h]
                for j in range(2):
                    fw.tt(hT("a", A, h * 2 + j, gs), t1("a", A, sl(j * 128, 128)), dn("a"), ALU.mult)
        for h in range(4):
            ps = cx.ps.get()
            for j in range(2):
                sq = tmpr.get()
                fw.act(sq("a"), hT("a", A, h * 2 + j, F), AF.Square)
                fw.mm(ps("a", A, F), cx.ones, sq("a"), start=(j == 0), stop=(j == 1))
            rn = tmpr.get(); tq = tmpr.get()
            rstd_from(cx, rn("a"), ps("a", A, F), 1.0 / 256.0, 1e-6, tq("a"))
            for j in range(2):
                c = h * 2 + j
                t1 = tmpr.get()
                fw.tt(t1("a"), hT("a", A, c, F), rn("a"), ALU.mult)
                fw.tt(t1("a"), t1("a"), og("a", A, c, F), ALU.mult, eng="pool")
                fw.act(hb("a", A, c, F), t1("a"), AF.Identity, scale=ng("a", A, slice(c, c + 1)))
        for dc in range(8):
            ps = cx.ps.get()
            for kc in range(8):
                fw.mm(ps("a", A, F), Wo("w", A, kc, sl(dc * 128, 128)), hb("a", A, kc, F), start=(kc == 0), stop=(kc == 7))
            fw.stt(yv(dc), xT("a", A, dc, F), ALPHA, ps("a", A, F), ALU.mult, ALU.add)
        ln_feature_major(cx, ph, [yv(c) for c in range(8)], 8,
                         [lng("a", A, slice(c, c + 1)) for c in range(8)], [lnb("a", A, slice(c, c + 1)) for c in range(8)],
                         None, sq_rot=tmpr, stat=stat, TW=TW, out_rot=x3o,
                         out_cb=lambda c, v: fw.dma("sp", cx.x3((t, c), sl(c * 128, 128), sl(t0, TW)), v))
    ph.close()
    cx.ps = Rot(cx.psb[0:6])


def phaseF(cx, layer, xin, xout_scratch):
    fw, nc, di = cx.fw, cx.nc, cx.di
    ph = Phase(cx)
    cx.ps = Rot(cx.psb[0:8])
    TW = 512
    NTW = cx.S // TW
    NTS = 2 if NTW % 2 == 0 else 1
    SW = NTS * TW
    A = slice(0, 128); F = slice(0, TW)
    moe = (layer == 1)
    NE = 8 if moe else 1
    if moe:
        wg_ap = lambda e: di["moe_w_gate"].h[e]; wu_ap = lambda e: di["moe_w_up"].h[e]; wd_ap = lambda e: di["moe_w_down"].h[e]
    else:
        wg_ap = lambda e: di["ffn_w_gate"].h; wu_ap = lambda e: di["ffn_w_up"].h; wd_ap = lambda e: di["ffn_w_down"].h
    lng = load_cols(cx, ph, di["ln_ffn_g"].h[layer, :].rearrange("(c p) -> p c", p=128), [128, 8])
    lnb = load_cols(cx, ph, di["ln_ffn_b"].h[layer, :].rearrange("(c p) -> p c", p=128), [128, 8])
    Wpp = load_w_bf16(cx, ph, di["ple_w_proj"].h[layer], 2, 1024)
    if moe:
        Wr = ph.sb([128, 8, 8])
        for kc in range(8):
            fw.dma("sp", Wr("a", A, kc, slice(0, 8)), View(di["moe_w_router"].h[kc * 128:(kc + 1) * 128, :], []))
        brB = ph.sb([128, 8])
        fw.dma("sp", brB("a"), View(di["moe_b_router"].h[:].partition_broadcast(128), []), allow_slow_non_contiguous=True)
        sel8 = ph.sb([8, 1024])
        fw.dma("sp", sel8("a"), di["sel8"]("a"))
        combT = [ph.sb([8, TW]) for _ in range(NTS)]
        combB = [ph.sb([128, TW]) for _ in range(NTS)]
        rt = ph.rot(2, [128, 176])
    xf = [ph.sb([128, 8, TW]) for _ in range(NTS)]
    xb = [ph.sb([128, 8, TW], BF16) for _ in range(NTS)]
    y = [ph.sb([128, 8, TW]) for _ in range(NTS)]
    wgb = ph.rot(2, [128, 8, 512], BF16); wub = ph.rot(2, [128, 8, 512], BF16); wdb = ph.rot(2, [128, 4, 1024], BF16)
    hb = ph.rot(8 * NTS, [128, TW], BF16)
    tmpr = ph.rot(4, [128, TW])
    stat = [ph.sb([128, TW]) for _ in range(4)]
    xin_rot = ph.rot(2, [128, 256])
    pTb = ph.sb([128, 2, TW], BF16)
    otok = ph.rot(2, [128, 1024])
    oT = ph.rot(3, [128, TW]) if xout_scratch is not None else None

    for st in range(NTW // NTS):
        for ti in range(NTS):
            t = st * NTS + ti; t0 = t * TW
            for c in range(8):
                fw.dma("sp", xf[ti]("a", A, c, F), xin((t, c), sl(c * 128, 128), sl(t0, TW)))
            fw.copy(xb[ti]("a"), xf[ti]("a"), eng="act")
            if moe:
                ps = cx.ps.get()
                for g in range(4):
                    gs = sl(g * 128, 128)
                    for kc in range(8):
                        fw.mm(ps("a", A, sl(g * 8, 8)), xf[ti]("a", A, kc, gs), Wr("a", A, kc, slice(0, 8)), start=(kc == 0), stop=(kc == 7))
                r = rt.get()
                R3 = lambda a: r.ap("a", r.h[:, a:a + 32].rearrange("p (g e) -> p g e", e=8))
                R1 = lambda a: r("a", A, sl(a, 4))
                RB = lambda a: r.ap("a", r.h[:, a:a + 4].rearrange("p (g o) -> p g o", o=1).to_broadcast([128, 4, 8]))
                lg = R3(0); eq = R3(32); l2 = R3(64); ex = R3(96); cb = R3(128)
                m1 = R1(160); m2 = R1(164); dd = R1(168)
                fw.tt(lg, ps.ap("a", ps.h[:, 0:32].rearrange("p (g e) -> p g e", e=8)),
                      brB.ap("a", brB.h[:, :].rearrange("p (o e) -> p o e", o=1).to_broadcast([128, 4, 8])), ALU.add)
                fw.reduce(m1, lg, ALU.max)
                fw.tt(eq, lg, RB(160), ALU.is_equal)
                fw.stt(l2, eq, -1.0e30, lg, ALU.mult, ALU.add)
                fw.reduce(m2, l2, ALU.max)
                fw.tt(eq, lg, RB(164), ALU.is_ge)
                fw.tt(ex, lg, RB(160), ALU.subtract)
                fw.act(ex, ex, AF.Exp)
                fw.tt(dd, m2, m1, ALU.subtract)
                fw.act(dd, dd, AF.Exp)
                fw.ts(dd, dd, 1.0, ALU.add)
                fw.op("dve", lambda: nc.vector.reciprocal(dd.ap, dd.ap), [dd], [dd])
                fw.tt(cb, ex, RB(168), ALU.mult)
                fw.tt(cb, cb, eq, ALU.mult)
                ps2 = cx.ps.get()
                for g in range(4):
                    fw.mm(ps2("a", slice(0, 8), sl(g * 128, 128)), r("a", A, sl(128 + g * 8, 8)), cx.ident)
                fw.copy(combT[ti]("a"), ps2("a", slice(0, 8), F), eng="act")
        for e in range(NE):
            if moe:
                for ti in range(NTS):
                    ps = cx.ps.get()
                    fw.mm(ps("a", A, F), sel8("a", slice(0, 8), sl(e * 128, 128)), combT[ti]("a"))
                    fw.copy(combB[ti]("a"), ps("a", A, F), eng="act")
            for f in range(7):
                wg = wgb.get(); wu = wub.get(); wd = wdb.get()
                fw.dma("pool", wg("a"), View(wg_ap(e)[:, f * 512:(f + 1) * 512].rearrange("(k p) c -> p k c", p=128), []))
                fw.dma("pool", wu("a"), View(wu_ap(e)[:, f * 512:(f + 1) * 512].rearrange("(k p) c -> p k c", p=128), []))
                fw.dma("pool", wd("a"), View(wd_ap(e)[f * 512:(f + 1) * 512, :].rearrange("(k p) c -> p k c", p=128), []))
                hs = [[None] * 4 for _ in range(NTS)]
                for fc in range(4):
                    for ti in range(NTS):
                        pg = cx.ps.get(); pu = cx.ps.get()
                        for kc in range(8):
                            fw.mm(pg("a", A, F), wg("a", A, kc, sl(fc * 128, 128)), xb[ti]("a", A, kc, F), start=(kc == 0), stop=(kc == 7))
                        for kc in range(8):
                            fw.mm(pu("a", A, F), wu("a", A, kc, sl(fc * 128, 128)), xb[ti]("a", A, kc, F), start=(kc == 0), stop=(kc == 7))
                        sg = tmpr.get()
                        fw.act(sg("a"), pg("a", A, F), AF.Silu)
                        h = hb.get()
                        if moe:
                            fw.tt(sg("a"), sg("a"), pu("a", A, F), ALU.mult)
                            fw.tt(h("a"), sg("a"), combB[ti]("a"), ALU.mult)
                        else:
                            fw.tt(h("a"), sg("a"), pu("a", A, F), ALU.mult)
                        hs[ti][fc] = h
                for dc in range(8):
                    for ti in range(NTS):
                        pd = cx.ps.get()
                        for fc in range(4):
                            fw.mm(pd("a", A, F), wd("a", A, fc, sl(dc * 128, 128)), hs[ti][fc]("a"), start=(fc == 0), stop=(fc == 3))
                        if e == 0 and f == 0:
                            fw.stt(y[ti]("a", A, dc, F), xf[ti]("a", A, dc, F), ALPHA, pd("a", A, F), ALU.mult, ALU.add)
                        else:
                            fw.tt(y[ti]("a", A, dc, F), pd("a", A, F), y[ti]("a", A, dc, F), ALU.add)
        for ti in range(NTS):
            t = st * NTS + ti; t0 = t * TW
            yv = y[ti]; xx = xf[ti]; xxb = xb[ti]
            ln_feature_major(cx, ph, [yv("a", A, c, F) for c in range(8)], 8,
                             [lng("a", A, slice(c, c + 1)) for c in range(8)], [lnb("a", A, slice(c, c + 1)) for c in range(8)],
                             [xx("a", A, c, F) for c in range(8)], sq_rot=tmpr, stat=stat, TW=TW)
            fw.copy(xxb("a"), xx("a"), eng="act")
            load_x_tile_T(cx, di["p"].h[layer], t0, TW, None, pTb, xin_rot, nchunks=2)
            for half in range(2):
                wpg = wgb.get()
                fw.dma("pool", wpg("a"), View(di["ple_w_gate"].h[layer][:, half * 512:(half + 1) * 512].rearrange("(k p) c -> p k c", p=128), []))
                for d4 in range(4):
                    dc = half * 4 + d4
                    pg = cx.ps.get(); pp = cx.ps.get()
                    for kc in range(8):
                        fw.mm(pg("a", A, F), wpg("a", A, kc, sl(d4 * 128, 128)), xxb("a", A, kc, F), start=(kc == 0), stop=(kc == 7))
                    for kc in range(2):
                        fw.mm(pp("a", A, F), Wpp("w", A, kc, sl(dc * 128, 128)), pTb("a", A, kc, F), start=(kc == 0), stop=(kc == 1))
                    sg = tmpr.get()
                    fw.act(sg("a"), pg("a", A, F), AF.Sigmoid)
                    fw.tt(sg("a"), sg("a"), pp("a", A, F), ALU.mult)
                    if xout_scratch is not None:
                        o = oT.get()
                        fw.tt(o("a"), sg("a"), xx("a", A, dc, F), ALU.add, eng="pool")
                        fw.dma("sp", xout_scratch((t, dc), sl(dc * 128, 128), sl(t0, TW)), o("a"))
                    else:
                        fw.tt(yv("a", A, dc, F), sg("a"), xx("a", A, dc, F), ALU.add, eng="pool")
            if xout_scratch is None:
                for g in range(4):
                    ot = otok.get()
                    for d0 in (0, 4):
                        ps = cx.ps.get()
                        for d4 in range(4):
                            fw.transpose(ps("a", A, sl(d4 * 128, 128)), yv("a", A, d0 + d4, sl(g * 128, 128)), cx.ident)
                        fw.copy(ot("a", A, sl(d0 * 128, 512)), ps("a"), eng="act")
                    fw.dma("sp", cx.out((t, g), sl(t0 + g * 128, 128), slice(0, 1024)), ot("a"))
    ph.close()
    cx.ps = Rot(cx.psb[0:6])


from concourse.bass_utils import run_bass_kernel_spmd

SEQ = 4096
NCORES = 8
_SQUEEZE = ("ab_w_in", "ab_conv_qkv", "ab_a_log", "ab_dt_bias", "ab_o_norm_g", "ab_dw_w", "ab_dw_b", "ab_cn_g",
            "ab_cn_b", "ab_w_out", "ffn_w_gate", "ffn_w_up", "ffn_w_down", "c_w_in", "c_b_i", "c_b_f", "c_norm_g",
            "c_w_out", "moe_w_router", "moe_b_router", "moe_w_gate", "moe_w_up", "moe_w_down")


def build_program(S):
    nc = bass.Bass("TRN2", target_bir_lowering=False)
    cx = setup(nc, S)
    phaseA(cx)
    phaseF(cx, 0, cx.x1, cx.x2)
    phaseC(cx)
    phaseF(cx, 1, cx.x3, None)
    cx.fw.barrier()
    return nc


def kernel(**inputs):
    f32 = lambda a: np.ascontiguousarray(np.asarray(a), dtype=np.float32)
    x = f32(inputs["x"]); p = f32(inputs["p"])
    B, S, _ = x.shape
    shared = {}
    for k, v in inputs.items():
        if k in ("x", "p"):
            continue
        a = f32(v)
        if k in _SQUEEZE:
            a = np.ascontiguousarray(a[0])
        shared[k] = a
    shared["cst"] = make_consts()
    shared["sel8"] = make_sel8()
    nc = build_program(S)
    in_maps = []
    for b in range(B):
        m = dict(shared)
        m["x"] = np.ascontiguousarray(x[b])
        m["p"] = np.ascontiguousarray(p[:, b])
        in_maps.append(m)
    res = run_bass_kernel_spmd(nc, in_maps, core_ids=list(range(B)))
    return np.stack([np.asarray(r["out"], dtype=np.float32) for r in res.results], axis=0)
```

```python
import numpy as np
import concourse.bass as bass
import concourse.mybir as mybir

F32 = mybir.dt.float32
BF16 = mybir.dt.bfloat16
AF = mybir.ActivationFunctionType
ALU = mybir.AluOpType
AX = mybir.AxisListType


class Trk:
    __slots__ = ("writer", "readers")

    def __init__(self):
        self.writer = None
        self.readers = {}


class View:
    __slots__ = ("ap", "trks")

    def __init__(self, ap, trks):
        self.ap = ap
        self.trks = trks


class TB:
    def __init__(self, handle):
        self.h = handle
        self.trk = {}

    def __call__(self, key, *idx):
        t = self.trk.get(key)
        if t is None:
            t = self.trk[key] = Trk()
        ap = self.h[idx] if idx else self.h[:]
        return View(ap, [t])

    def ap(self, key, ap):
        t = self.trk.get(key)
        if t is None:
            t = self.trk[key] = Trk()
        return View(ap, [t])

    def multi(self, keys, *idx):
        ts = []
        for key in keys:
            t = self.trk.get(key)
            if t is None:
                t = self.trk[key] = Trk()
            ts.append(t)
        ap = self.h[idx] if idx else self.h[:]
        return View(ap, ts)


class FW:
    NDMA = 48

    def __init__(self, nc):
        self.nc = nc
        self.eng = {"pe": nc.tensor, "dve": nc.vector, "act": nc.scalar,
                    "pool": nc.gpsimd, "sp": nc.sync}
        self.sem = {}
        self.cnt = {}
        for k in self.eng:
            self.sem[k] = nc.alloc_semaphore("s_" + k)
            self.cnt[k] = 0
        self.dsem = [nc.alloc_semaphore("d%d" % i) for i in range(self.NDMA)]
        self.dcnt = [0] * self.NDMA
        self.dnext = 0
        self.dnext_sw = 0
        self.waited = {k: {} for k in self.eng}
        self.ninst = 0
        self.nwait = 0

    def _sem(self, key):
        return self.sem[key] if isinstance(key, str) else self.dsem[key]

    def _need(self, eng, ev, needs):
        if ev is None:
            return
        key, val = ev
        if key == eng and eng == "pe":
            return
        if self.waited[eng].get(key, 0) >= val:
            return
        if needs.get(key, 0) < val:
            needs[key] = val

    def _collect(self, eng, outs, ins):
        needs = {}
        for v in ins:
            for t in v.trks:
                self._need(eng, t.writer, needs)
        for v in outs:
            for t in v.trks:
                self._need(eng, t.writer, needs)
                for key, ev in t.readers.items():
                    self._need(eng, ev, needs)
        return needs

    def _emit_waits(self, eng, items):
        e = self.eng[eng]
        for key, val in items:
            e.wait_ge(self._sem(key), val)
            self.waited[eng][key] = val
            self.nwait += 1

    def _deps(self, eng, outs, ins):
        self._emit_waits(eng, list(self._collect(eng, outs, ins).items()))

    def _record(self, ev, outs, ins):
        for v in ins:
            for t in v.trks:
                old = t.readers.get(ev[0])
                if old is None or old[1] < ev[1]:
                    t.readers[ev[0]] = ev
        for v in outs:
            for t in v.trks:
                t.writer = ev
                t.readers = {}

    def op(self, eng, fn, outs, ins):
        items = list(self._collect(eng, outs, ins).items())
        self._emit_waits(eng, items[:-1])
        inst = fn()
        if items:
            key, val = items[-1]
            inst._wait_ge(self._sem(key), val)
            self.waited[eng][key] = val
        self.cnt[eng] += 1
        inst.then_inc(self.sem[eng], 1)
        self.ninst += 1
        self._record((eng, self.cnt[eng]), outs, ins)
        return inst

    def dma(self, q, out, in_, **kw):
        half = self.NDMA // 2
        if q == "pool":
            i = half + self.dnext_sw
            self.dnext_sw = (self.dnext_sw + 1) % half
        else:
            i = self.dnext
            self.dnext = (self.dnext + 1) % half
        needs = {}
        if self.dcnt[i] > 0:
            self._need(q, (i, self.dcnt[i]), needs)
        for key, val in needs.items():
            self.eng[q].wait_ge(self._sem(key), val)
            self.waited[q][key] = val
        self._deps(q, [out], [in_])
        inst = self.eng[q].dma_start(out=out.ap, in_=in_.ap, **kw)
        self.dcnt[i] += 16
        inst.then_inc(self.dsem[i], 16)
        self.ninst += 1
        self._record((i, self.dcnt[i]), [out], [in_])
        return inst

    def barrier(self):
        for e in self.eng:
            for o in self.eng:
                if o != e and self.cnt[o] > self.waited[e].get(o, 0):
                    self.eng[e].wait_ge(self.sem[o], self.cnt[o])
                    self.waited[e][o] = self.cnt[o]
            for i in range(self.NDMA):
                if self.dcnt[i] > self.waited[e].get(i, 0):
                    self.eng[e].wait_ge(self.dsem[i], self.dcnt[i])
                    self.waited[e][i] = self.dcnt[i]

    def wait_all(self, eng, views):
        needs = {}
        for v in views:
            for t in v.trks:
                self._need(eng, t.writer, needs)
        for key, val in needs.items():
            self.eng[eng].wait_ge(self._sem(key), val)
            self.waited[eng][key] = val

    def mm(self, out, lhsT, rhs, start=True, stop=True, **kw):
        return self.op("pe", lambda: self.nc.tensor.matmul(out.ap, lhsT.ap, rhs.ap, start=start, stop=stop, **kw),
                       [out], [lhsT, rhs])

    def transpose(self, out, in_, ident):
        return self.op("pe", lambda: self.nc.tensor.matmul(out.ap, in_.ap, ident.ap, start=True, stop=True), [out], [in_, ident])

    def act(self, out, in_, func, bias=None, scale=None, eng="act", accum_out=None):
        ins = [in_]
        kw = {}
        if bias is not None:
            if isinstance(bias, View):
                ins.append(bias); kw["bias"] = bias.ap
            else:
                kw["bias"] = bias
        if scale is not None:
            if isinstance(scale, View):
                ins.append(scale); kw["scale"] = scale.ap
            else:
                kw["scale"] = scale
        outs = [out]
        if accum_out is not None:
            outs.append(accum_out); kw["accum_out"] = accum_out.ap
        return self.op("act", lambda: self.nc.scalar.activation(out.ap, in_.ap, func, **kw), outs, ins)

    def _ve(self, eng):
        return self.eng[eng]

    def tt(self, out, a, b, op, eng="dve"):
        return self.op(eng, lambda: self._ve(eng).tensor_tensor(out.ap, a.ap, b.ap, op), [out], [a, b])

    def ts(self, out, a, s1, op0, s2=None, op1=None, eng="dve", accum_out=None):
        ins = [a]
        s1a = s1
        if isinstance(s1, View):
            ins.append(s1); s1a = s1.ap
        s2a = s2
        if isinstance(s2, View):
            ins.append(s2); s2a = s2.ap
        kw = {}
        outs = [out]
        if op1 is not None:
            kw["op1"] = op1
        if accum_out is not None:
            kw["accum_out"] = accum_out.ap; outs.append(accum_out)
        return self.op(eng, lambda: self._ve(eng).tensor_scalar(out.ap, a.ap, s1a, s2a, op0, **kw), outs, ins)

    def stt(self, out, a, s, b, op0, op1, eng="dve"):
        ins = [a, b]
        sa = s
        if isinstance(s, View):
            ins.append(s); sa = s.ap
        return self.op(eng, lambda: self._ve(eng).scalar_tensor_tensor(out.ap, a.ap, sa, b.ap, op0, op1), [out], ins)

    def copy(self, out, in_, eng="dve"):
        if eng == "act":
            return self.op("act", lambda: self.nc.scalar.copy(out.ap, in_.ap), [out], [in_])
        return self.op(eng, lambda: self._ve(eng).tensor_copy(out.ap, in_.ap), [out], [in_])

    def memset(self, out, val, eng="dve"):
        return self.op(eng, lambda: self._ve(eng).memset(out.ap, val), [out], [])

    def reduce(self, out, in_, op, axis=AX.X, eng="dve"):
        return self.op(eng, lambda: self._ve(eng).tensor_reduce(out.ap, in_.ap, axis, op), [out], [in_])

import contextlib
import numpy as np
import concourse.bass as bass
import concourse.mybir as mybir

D = 1024
TT = 512
NEG = 1.0e4
ALPHA = 4.0 ** 0.25
C_ID, C_ONES, C_NBS, C_NBTI, C_SM01T, C_SEL4, C_RM, C_RB, C_END = 0, 128, 256, 384, 512, 640, 1152, 1664, 2176
C_SEL8 = 0


def make_consts():
    c = np.zeros((128, C_END), np.float32)
    idx = np.arange(128)
    same = (idx[:, None] // 64) == (idx[None, :] // 64)
    c[:, C_ID:C_ID + 128] = np.eye(128)
    c[:, C_ONES:C_ONES + 128] = 1.0
    c[:, C_NBS:C_NBS + 128] = np.where(same & (idx[None, :] < idx[:, None]), 0.0, NEG)
    c[:, C_NBTI:C_NBTI + 128] = np.where(same & (idx[None, :] >= idx[:, None]), 0.0, NEG)
    c[:, C_SM01T:C_SM01T + 128] = np.where(same & (idx[None, :] > idx[:, None]), 1.0, 0.0)
    for h in range(4):
        c[h, C_SEL4 + h * 128:C_SEL4 + (h + 1) * 128] = 1.0
    t = np.arange(512)
    c[0:4, C_RM:C_RM + 512] = np.where(t % 64 == 0, 0.0, 1.0)[None, :]
    c[0:4, C_RB:C_RB + 512] = np.where(t % 64 == 0, -1.0e30, 0.0)[None, :]
    return c


def make_sel8():
    c = np.zeros((8, 1024), np.float32)
    for e in range(8):
        c[e, e * 128:(e + 1) * 128] = 1.0
    return c


class Ctx:
    pass


class Rot:
    def __init__(self, bufs):
        self.bufs = bufs
        self.i = 0

    def get(self):
        b = self.bufs[self.i]
        self.i = (self.i + 1) % len(self.bufs)
        return b


def sl(a, n):
    return slice(a, a + n)


def setup(nc, S, dbg=False):
    cx = Ctx()
    cx.nc = nc
    cx.S = S
    cx.NT = S // TT
    fw = cx.fw = FW(nc)
    di = {}

    def din(name, shape):
        di[name] = TB(nc.dram_tensor(name, list(shape), F32, kind="ExternalInput"))

    din("x", (S, D)); din("p", (2, S, 256))
    din("ab_w_in", (D, 3080)); din("ab_conv_qkv", (4, 1536)); din("ab_a_log", (4,)); din("ab_dt_bias", (4,))
    din("ab_o_norm_g", (128,)); din("ab_dw_w", (31, 512)); din("ab_dw_b", (512,)); din("ab_cn_g", (512,))
    din("ab_cn_b", (512,)); din("ab_w_out", (D, D)); din("ffn_w_gate", (D, 3584)); din("ffn_w_up", (D, 3584))
    din("ffn_w_down", (3584, D)); din("c_w_in", (D, 3080)); din("c_b_i", (4,)); din("c_b_f", (4,))
    din("c_norm_g", (1024,)); din("c_w_out", (D, D)); din("moe_w_router", (D, 8)); din("moe_b_router", (8,))
    din("moe_w_gate", (8, D, 3584)); din("moe_w_up", (8, D, 3584)); din("moe_w_down", (8, 3584, D))
    din("ln_mix_g", (2, D)); din("ln_mix_b", (2, D)); din("ln_ffn_g", (2, D)); din("ln_ffn_b", (2, D))
    din("ple_w_proj", (2, 256, D)); din("ple_w_gate", (2, D, D)); din("cst", (128, C_END)); din("sel8", (8, 1024))
    cx.di = di
    cx.out = TB(nc.dram_tensor("out", [S, D], F32, kind="ExternalOutput"))
    kd = "ExternalOutput" if dbg else "Internal"
    cx.x1 = TB(nc.dram_tensor("x1s", [D, S], F32, kind=kd))
    cx.x2 = TB(nc.dram_tensor("x2s", [D, S], F32, kind=kd))
    cx.x3 = TB(nc.dram_tensor("x3s", [D, S], F32, kind=kd))
    cx.psb = [TB(nc.alloc_psum_tensor("ps%d" % i, [128, 512], F32)) for i in range(8)]
    cx.ps = Rot(cx.psb[0:6])
    cx.psl = Rot(cx.psb[6:8])
    cx.cst = TB(nc.alloc_sbuf_tensor("cst_sb", [128, C_END], F32))
    fw.dma("sp", cx.cst("c"), di["cst"]("c"))
    cx.ident = cx.cst("c", slice(0, 128), sl(C_ID, 128))
    cx.ones = cx.cst("c", slice(0, 128), sl(C_ONES, 128))
    return cx


def C(cx, off, n, rows=128):
    return cx.cst("c", slice(0, rows), sl(off, n))


class Phase:
    uid = 0

    def __init__(self, cx):
        self.cx = cx
        self.es = contextlib.ExitStack()
        self.n = 0

    def sb(self, shape, dtype=F32, name=None):
        self.n += 1
        Phase.uid += 1
        nm = name or ("t%d" % Phase.uid)
        return TB(self.es.enter_context(self.cx.nc.sbuf_tensor(nm, list(shape), dtype)))

    def rot(self, n, shape, dtype=F32):
        return Rot([self.sb(shape, dtype) for _ in range(n)])

    def close(self):
        self.cx.fw.barrier()
        self.es.close()


def bcast_rows(cx, dst, src_rows, sel_off, h, nrows, n=512):
    fw = cx.fw
    ps = cx.ps.get()
    fw.mm(ps("a", slice(0, 128), slice(0, n)), C(cx, sel_off + h * 128, 128, nrows), src_rows)
    fw.copy(dst, ps("a", slice(0, 128), slice(0, n)), eng="act")


def rstd_from(cx, out, in_, scale, eps, tmp):
    fw = cx.fw
    fw.ts(tmp, in_, scale, ALU.mult, eps, ALU.add)
    fw.act(tmp, tmp, AF.Ln)
    fw.act(out, tmp, AF.Exp, scale=-0.5)


def ln_feature_major(cx, ph, y, nchunk, gcol, bcol, outs, eps=1e-5, func=AF.Identity, sq_rot=None, stat=None,
                     TW=512, out_rot=None, out_cb=None):
    fw = cx.fw
    n = float(nchunk * 128)
    A = slice(0, 128); F = slice(0, TW)
    ps1 = cx.ps.get(); ps2 = cx.ps.get()
    for c in range(nchunk):
        fw.mm(ps1("a", A, F), cx.ones, y[c], start=(c == 0), stop=(c == nchunk - 1))
    for c in range(nchunk):
        sq = sq_rot.get()
        fw.act(sq("a"), y[c], AF.Square)
        fw.mm(ps2("a", A, F), cx.ones, sq("a"), start=(c == 0), stop=(c == nchunk - 1))
    mean, msq, rstd, tmp = stat
    fw.act(mean("a"), ps1("a", A, F), AF.Copy, scale=1.0 / n)
    fw.tt(msq("a"), mean("a"), mean("a"), ALU.mult)
    fw.stt(tmp("a"), ps2("a", A, F), 1.0 / n, msq("a"), ALU.mult, ALU.subtract)
    fw.ts(tmp("a"), tmp("a"), eps, ALU.add)
    fw.act(tmp("a"), tmp("a"), AF.Ln)
    fw.act(rstd("a"), tmp("a"), AF.Exp, scale=-0.5)
    for c in range(nchunk):
        t = sq_rot.get()
        fw.tt(t("a"), y[c], mean("a"), ALU.subtract)
        fw.tt(t("a"), t("a"), rstd("a"), ALU.mult, eng="pool")
        if outs is not None:
            fw.act(outs[c], t("a"), func, scale=gcol[c], bias=bcol[c])
        else:
            o = out_rot.get()
            fw.act(o("a"), t("a"), func, scale=gcol[c], bias=bcol[c])
            out_cb(c, o("a"))


def load_cols(cx, ph, dram_ap_rearranged, shape, q="sp"):
    t = ph.sb(shape)
    cx.fw.dma(q, t("a"), View(dram_ap_rearranged, []), allow_slow_non_contiguous=True)
    return t


def load_kcp(cx, ph, w2d, K, Cn):
    t = ph.sb([128, Cn, K])
    for kk in range(K):
        cx.fw.dma("sp", t.ap("a", t.h[:, :, kk]), View(w2d[kk, :].rearrange("(c p) -> p c", p=128), []),
                  allow_slow_non_contiguous=True)
    return t


class StopBuild(Exception):
    pass


def ck(cx, name):
    if getattr(cx, "stop_at", None) == name:
        raise StopBuild(name)


def mark(cx, name, t=None):
    pt = getattr(cx, "prof_tile", None)
    cur = getattr(cx, "_scope", None)
    if cur is not None:
        cx.nc.leave_named_scope(cur) if hasattr(cx.nc, "leave_named_scope") else None
        cx._scope = None
    if pt is not None and t == pt and name is not None:
        cx.nc.enter_named_scope(name)
        cx._scope = name


def load_w_bf16(cx, ph, wap, kchunks, ncols, q="pool"):
    t = ph.sb([128, kchunks, ncols], BF16)
    for kc in range(kchunks):
        cx.fw.dma(q, t("w", slice(0, 128), kc, slice(0, ncols)), View(wap[kc * 128:(kc + 1) * 128, :], []))
    return t


def load_x_tile_T(cx, src_ap, t0, TW, xT, xTb, xin_rot, nchunks=8):
    fw = cx.fw
    for g in range(TW // 128):
        b = xin_rot.get()
        fw.dma("sp", b("a", slice(0, 128), slice(0, nchunks * 128)), View(src_ap[t0 + g * 128:t0 + (g + 1) * 128, :], []))
        ck(cx, "xdma")
        for k0 in range(0, nchunks, 4):
            nk = min(4, nchunks - k0)
            ps = cx.ps.get()
            for k in range(nk):
                fw.transpose(ps("a", slice(0, 128), sl(k * 128, 128)), b("a", slice(0, 128), sl((k0 + k) * 128, 128)), cx.ident)
            ck(cx, "xtr")
            src = ps.ap("a", ps.h[:, 0:nk * 128].rearrange("p (k t) -> p k t", t=128))
            if xT is not None:
                fw.copy(xT.ap("a", xT.h[:, k0:k0 + nk, g * 128:(g + 1) * 128]), src, eng="act")
            ck(cx, "xcp1")
            if xTb is not None and xT is not None:
                fw.copy(xTb.ap("a", xTb.h[:, k0:k0 + nk, g * 128:(g + 1) * 128]), xT.ap("a", xT.h[:, k0:k0 + nk, g * 128:(g + 1) * 128]), eng="dve")
            elif xTb is not None:
                fw.copy(xTb.ap("a", xTb.h[:, k0:k0 + nk, g * 128:(g + 1) * 128]), src, eng="act")


def phaseA(cx, TW=256):
    fw, nc, di = cx.fw, cx.nc, cx.di
    ph = Phase(cx)
    NTW = cx.S // TW
    NG = TW // 128
    NCH = TW // 64
    Wb = load_w_bf16(cx, ph, di["ab_w_in"].h, 8, 3080)
    Wg32 = ph.sb([128, 8, 8])
    for kc in range(8):
        fw.dma("sp", Wg32("a", slice(0, 128), kc, slice(0, 8)), View(di["ab_w_in"].h[kc * 128:(kc + 1) * 128, 2048:2056], []))
    cw = load_kcp(cx, ph, di["ab_conv_qkv"].h, 4, 12)
    dww = load_kcp(cx, ph, di["ab_dw_w"].h, 31, 4)
    dwb = load_cols(cx, ph, di["ab_dw_b"].h[:].rearrange("(c p) -> p c", p=128), [128, 4])
    cng = load_cols(cx, ph, di["ab_cn_g"].h[:].rearrange("(c p) -> p c", p=128), [128, 4])
    cnb = load_cols(cx, ph, di["ab_cn_b"].h[:].rearrange("(c p) -> p c", p=128), [128, 4])
    ong = load_cols(cx, ph, di["ab_o_norm_g"].h[:].rearrange("(c p) -> p c", p=128), [128, 1])
    lng = load_cols(cx, ph, di["ln_mix_g"].h[0, :].rearrange("(c p) -> p c", p=128), [128, 8])
    lnb = load_cols(cx, ph, di["ln_mix_b"].h[0, :].rearrange("(c p) -> p c", p=128), [128, 8])
    hp = ph.sb([4, 4])
    fw.dma("sp", hp("a", slice(0, 4), slice(0, 1)), View(di["ab_a_log"].h[:].rearrange("(p o) -> p o", o=1), []), allow_slow_non_contiguous=True)
    fw.dma("sp", hp("a", slice(0, 4), slice(1, 2)), View(di["ab_dt_bias"].h[:].rearrange("(p o) -> p o", o=1), []), allow_slow_non_contiguous=True)
    fw.act(hp("a", slice(0, 4), slice(2, 3)), hp("a", slice(0, 4), slice(0, 1)), AF.Exp)
    fw.ts(hp("a", slice(0, 4), slice(2, 3)), hp("a", slice(0, 4), slice(2, 3)), -1.0, ALU.mult)
    ck(cx, "w")
    halo_q = ph.sb([128, 12, 3]); fw.memset(halo_q("a"), 0.0)
    ucv = [ph.sb([128, 30 + TW]) for _ in range(4)]
    for c in range(4):
        fw.memset(ucv[c]("a", slice(0, 128), slice(0, 30)), 0.0)
    Sst = [ph.sb([128, 128]) for _ in range(4)]
    for h in range(4):
        fw.memset(Sst[h]("a"), 0.0)
    ck(cx, "mem")
    xin_rot = ph.rot(2, [128, 1024])
    xT = ph.sb([128, 8, TW]); xTb = ph.sb([128, 8, TW], BF16)
    cvr = ph.rot(2, [128, 3 + TW])
    qkv = ph.sb([128, 12, TW])
    zs = ph.sb([128, 4, TW], BF16)
    tmpr = ph.rot(6, [128, TW])
    GF = ph.sb([4, 8, TW])
    TM = [ph.sb([128, 5, 4]) for _ in range(NG)]
    BIG1 = ph.sb([128, 8, TW]); BIG2 = ph.sb([128, 8, TW])
    gcB = lambda h, cols: BIG1(("c", h), slice(0, 128), h, cols)
    nbB = lambda h, cols: BIG1(("c", 4 + h), slice(0, 128), 4 + h, cols)
    egB = lambda h, cols: BIG2(("c", h), slice(0, 128), h, cols)
    acc = lambda c: BIG2(("c", 4 + c), slice(0, 128), 4 + c, slice(0, TW))
    yT = lambda c: BIG1(("c", c), slice(0, 128), c, slice(0, TW))
    cdec = ph.sb([128, 4, NCH])
    usb = ph.sb([128, 4, TW], BF16)
    oab = ph.sb([128, 4, TW], BF16)
    oT = ph.sb([128, 4, TW])
    stat = [ph.sb([128, TW]) for _ in range(4)]
    x1o = ph.rot(3, [128, TW])
    mk4 = lambda shape=(128, 128): [ph.sb(list(shape)) for _ in range(4)]
    U_nk = mk4(); U_kd = mk4(); U_vb = mk4(); U_ds = mk4(); U_dt = mk4(); U_t1 = mk4(); U_at = mk4()
    U_nw = mk4(); U_qd = mk4(); U_vn = mk4()
    U_PQ = [[ph.sb([128, 256]) for _ in range(2)] for _ in range(4)]
    U_X = [[ph.sb([128, 128]) for _ in range(2)] for _ in range(4)]
    worot = ph.rot(2, [128, 8, 128], BF16)
    A = slice(0, 128)
    F = slice(0, TW)
    G4 = slice(0, 4)

    for t in range(NTW):
        t0 = t * TW
        load_x_tile_T(cx, di["x"].h, t0, TW, xT, xTb, xin_rot)
        ck(cx, "x")
        def inproj(c0):
            ps = cx.ps.get()
            for kc in range(8):
                fw.mm(ps("a", A, F), Wb("w", A, kc, sl(c0, 128)), xTb("a", A, kc, F), start=(kc == 0), stop=(kc == 7))
            return ps
        def gate_gen():
            psb_ = cx.ps.get(); psd_ = cx.ps.get()
            for kc in range(8):
                fw.mm(psb_("a", G4, F), Wg32("a", A, kc, slice(0, 4)), xT("a", A, kc, F), start=(kc == 0), stop=(kc == 7))
            for kc in range(8):
                fw.mm(psd_("a", G4, F), Wg32("a", A, kc, slice(4, 8)), xT("a", A, kc, F), start=(kc == 0), stop=(kc == 7))
            gf = lambda q: GF("a", G4, q, F)
            fw.act(gf(0), psb_("a", G4, F), AF.Sigmoid)
            fw.ts(gf(3), gf(0), -1.0, ALU.mult)
            fw.act(gf(7), psd_("a", G4, F), AF.Identity, bias=hp("a", G4, slice(1, 2)))
            yield
            fw.ts(gf(6), gf(7), 0.0, ALU.max)
            fw.stt(gf(5), gf(6), -2.0, gf(7), ALU.mult, ALU.add)
            fw.act(gf(5), gf(5), AF.Exp)
            fw.ts(gf(5), gf(5), 1.0, ALU.add)
            fw.act(gf(5), gf(5), AF.Ln)
            yield
            fw.tt(gf(5), gf(5), gf(6), ALU.add)
            fw.ts(gf(1), gf(5), hp("a", G4, slice(2, 3)), ALU.mult)
            rm = C(cx, C_RM, TW, 4)
            fw.op("dve", lambda: nc.vector.tensor_tensor_scan(gf(2).ap, rm.ap, gf(1).ap, 0.0, ALU.mult, ALU.add),
                  [gf(2)], [rm, gf(1)])
            fw.act(gf(4), gf(2), AF.Exp)
            fw.tt(gf(5), gf(3), gf(4), ALU.mult)
            gc3 = GF.ap("a", GF.h[0:4, 2, :].rearrange("p (c l) -> p c l", l=64))
            gcl = GF.ap("a", GF.h[0:4, 2, :].rearrange("p (c l) -> p c l", l=64)[:, :, 63:64].to_broadcast([4, NCH, 64]))
            kds3 = GF.ap("a", GF.h[0:4, 6, :].rearrange("p (c l) -> p c l", l=64))
            fw.tt(kds3, gcl, gc3, ALU.subtract)
            yield
            fw.act(gf(6), gf(6), AF.Exp)
            for g in range(NG):
                ps = cx.ps.get()
                for qi, q in enumerate((2, 3, 5, 0, 6)):
                    fw.mm(ps("a", A, sl(qi * 4, 4)), GF("a", G4, q, sl(g * 128, 128)), C(cx, C_ID, 4, 4))
                fw.copy(TM[g]("a"), ps.ap("a", ps.h[:, 0:20].rearrange("p (q h) -> p q h", h=4)))
                yield
            for h in range(4):
                bcast_rows(cx, gcB(h, F), gf(2), C_SEL4, h, 4, TW)
                bcast_rows(cx, nbB(h, F), gf(3), C_SEL4, h, 4, TW)
                bcast_rows(cx, egB(h, F), gf(4), C_SEL4, h, 4, TW)
                yield
            ps = cx.ps.get()
            for h in range(4):
                fw.mm(ps("a", A, sl(h * NCH, NCH)), C(cx, C_SEL4 + h * 128, 128, 4), GF.ap("a", GF.h[0:4, 4, 63::64]))
            fw.copy(cdec("a"), ps.ap("a", ps.h[:, 0:4 * NCH].rearrange("p (h c) -> p h c", c=NCH)))
            yield
        gg = gate_gen()
        for c in range(12):
            next(gg, None); next(gg, None)
            ps = inproj(c * 128)
            cv = cvr.get()
            fw.copy(cv("a", A, slice(0, 3)), halo_q("a", A, c, slice(0, 3)), eng="pool")
            fw.copy(cv("a", A, slice(3, 3 + TW)), ps("a", A, F), eng="act")
            fw.copy(halo_q("a", A, c, slice(0, 3)), cv("a", A, slice(TW, TW + 3)), eng="pool")
            tq = tmpr.get()
            fw.ts(tq("a"), cv("a", A, slice(0, TW)), cw("a", A, c, slice(0, 1)), ALU.mult)
            for k in range(1, 4):
                fw.stt(tq("a"), cv("a", A, slice(k, k + TW)), cw("a", A, c, slice(k, k + 1)), tq("a"), ALU.mult, ALU.add)
            fw.act(qkv("a", A, c, F), tq("a"), AF.Silu)
        for _ in gg:
            pass
        ck(cx, "conv")
        for c in range(8):
            sq = tmpr.get()
            fw.act(sq("a"), qkv("a", A, c, F), AF.Square)
            ps = cx.ps.get()
            fw.mm(ps("a", A, F), cx.ones, sq("a"))
            rn = tmpr.get()
            rstd_from(cx, rn("a"), ps("a", A, F), 1.0, 1e-6, sq("a"))
            scale = (128.0 ** -0.5) if c < 4 else 1.0
            fw.stt(qkv("a", A, c, F), qkv("a", A, c, F), scale, rn("a"), ALU.mult, ALU.mult)
        for c in range(4):
            ps = inproj(1536 + c * 128)
            fw.act(zs("a", A, c, F), ps("a", A, F), AF.Silu)
        ck(cx, "l2")
        ck(cx, "tm")
        def conf_gen():
            for c in range(4):
                psa = inproj(2056 + c * 128)
                psg = inproj(2568 + c * 128)
                sg = tmpr.get()
                fw.act(sg("a"), psg("a", A, F), AF.Sigmoid)
                fw.tt(ucv[c]("a", A, slice(30, 30 + TW)), psa("a", A, F), sg("a"), ALU.mult)
                yield
                fw.ts(acc(c), ucv[c]("a", A, slice(0, TW)), dww("a", A, c, slice(0, 1)), ALU.mult, dwb("a", A, slice(c, c + 1)), ALU.add)
                for k in range(1, 31):
                    fw.stt(acc(c), ucv[c]("a", A, slice(k, k + TW)), dww("a", A, c, slice(k, k + 1)), acc(c), ALU.mult, ALU.add)
                    if k % 3 == 0:
                        yield
                fw.copy(ucv[c]("a", A, slice(0, 30)), ucv[c]("a", A, slice(TW, TW + 30)), eng="pool")
                yield
            ln_feature_major(cx, ph, [acc(c) for c in range(4)], 4,
                             [cng("a", A, slice(c, c + 1)) for c in range(4)], [cnb("a", A, slice(c, c + 1)) for c in range(4)],
                             [usb("a", A, c, F) for c in range(4)], func=AF.Silu, sq_rot=tmpr, stat=stat, TW=TW)
            yield
        cg = conf_gen()
        pump = lambda n=1: [next(cg, None) for _ in range(n)]
        ck(cx, "conf")
        H4 = range(4)
        for g in range(NG):
            gs = sl(g * 128, 128)
            pso = cx.psl.get()
            qn = [qkv("a", A, h, gs) for h in H4]; kn = [qkv("a", A, 4 + h, gs) for h in H4]; vv = [qkv("a", A, 8 + h, gs) for h in H4]
            gcT = [TM[g]("a", A, 0, slice(h, h + 1)) for h in H4]; nbT = [TM[g]("a", A, 1, slice(h, h + 1)) for h in H4]
            nbegT = [TM[g]("a", A, 2, slice(h, h + 1)) for h in H4]; betaT = [TM[g]("a", A, 3, slice(h, h + 1)) for h in H4]
            kdsT = [TM[g]("a", A, 4, slice(h, h + 1)) for h in H4]
            Pv = lambda bb: bb("a", A, slice(0, 128))
            Qv = lambda bb: bb("a", A, slice(128, 256))
            pst = [None] * 4
            for h in H4:
                ps = pst[h] = cx.ps.get()
                fw.transpose(ps("a", A, slice(0, 128)), kn[h], cx.ident)
                fw.transpose(ps("a", A, slice(128, 256)), vv[h], cx.ident)
            for h in H4:
                ps = pst[h]
                fw.act(U_nk[h]("a"), ps("a", A, slice(0, 128)), AF.Identity, scale=nbegT[h])
                fw.act(U_kd[h]("a"), ps("a", A, slice(0, 128)), AF.Identity, scale=kdsT[h])
                fw.act(U_vb[h]("a"), ps("a", A, slice(128, 256)), AF.Identity, scale=betaT[h])
            for h in H4:
                fw.stt(U_ds[h]("a"), gcB(h, gs), gcT[h], C(cx, C_NBS, 128), ALU.subtract, ALU.add)
                fw.stt(U_dt[h]("a"), gcB(h, gs), gcT[h], C(cx, C_NBTI, 128), ALU.subtract, ALU.subtract)
            pump(2)
            for h in H4:
                fw.act(U_ds[h]("a"), U_ds[h]("a"), AF.Exp, scale=-1.0)
                fw.act(U_dt[h]("a"), U_dt[h]("a"), AF.Exp)
            pump(2)
            for h in H4:
                fw.tt(U_t1[h]("a"), U_dt[h]("a"), C(cx, C_SM01T, 128), ALU.mult, eng="pool")
                fw.tt(U_t1[h]("a"), U_t1[h]("a"), nbB(h, gs), ALU.mult, eng="pool")
                fw.tt(U_qd[h]("a"), qn[h], egB(h, gs), ALU.mult, eng="pool")
            psA = [None] * 4
            for h in H4:
                psA[h] = cx.ps.get()
                fw.mm(psA[h]("a", A, slice(0, 128)), kn[h], kn[h])
                fw.mm(psA[h]("a", A, slice(128, 256)), kn[h], qn[h])
            cur = [0] * 4
            for h in H4:
                PQ = U_PQ[h][0]
                fw.stt(Pv(PQ), psA[h]("a", A, slice(0, 128)), nbT[h], U_ds[h]("a"), ALU.mult, ALU.mult)
                fw.tt(Qv(PQ), psA[h]("a", A, slice(0, 128)), U_t1[h]("a"), ALU.mult)
                fw.tt(U_at[h]("a"), psA[h]("a", A, slice(128, 256)), U_dt[h]("a"), ALU.mult)
            for h in H4:
                fw.tt(U_X[h][0]("a"), Qv(U_PQ[h][0]), cx.ident, ALU.add, eng="pool")
            pump(2)
            for lvl in range(1, 6):
                ps1 = [None] * 4; ps2 = [None] * 4
                for h in H4:
                    PQ = U_PQ[h][cur[h]]
                    ps1[h] = cx.ps.get()
                    fw.mm(ps1[h]("a", A, slice(0, 128)), Qv(PQ), Pv(PQ))
                    if lvl < 5:
                        fw.mm(ps1[h]("a", A, slice(128, 256)), Pv(PQ), Qv(PQ))
                for h in H4:
                    PQn = U_PQ[h][1 - cur[h]]
                    if lvl < 5:
                        fw.copy(PQn("a"), ps1[h]("a", A, slice(0, 256)), eng="act")
                    else:
                        fw.copy(Pv(PQn), ps1[h]("a", A, slice(0, 128)), eng="act")
                for h in H4:
                    PQn = U_PQ[h][1 - cur[h]]
                    ps2[h] = cx.ps.get()
                    fw.mm(ps2[h]("a", A, slice(0, 128)), Pv(PQn), U_X[h][cur[h]]("a"))
                for h in H4:
                    fw.tt(U_X[h][1 - cur[h]]("a"), ps2[h]("a", A, slice(0, 128)), U_X[h][cur[h]]("a"), ALU.add)
                    cur[h] = 1 - cur[h]
                pump(2)
            Xt = [U_X[h][cur[h]]("a") for h in H4]
            psn = [None] * 4
            for h in H4:
                psn[h] = cx.ps.get()
                fw.mm(psn[h]("a", A, slice(0, 128)), U_nk[h]("a"), Xt[h])
            for h in H4:
                fw.copy(U_nw[h]("a"), psn[h]("a", A, slice(0, 128)), eng="act")
            pump(2)
            for c in range(2):
                cs = sl(c * 64, 64)
                ci = g * 2 + c
                psC = [None] * 4; psS = [None] * 4
                for h in H4:
                    psC[h] = cx.ps.get()
                    fw.mm(psC[h]("a", A, slice(0, 128)), Xt[h], U_vb[h]("a"), start=True, stop=False)
                    fw.mm(psC[h]("a", A, slice(0, 128)), U_nw[h]("a"), Sst[h]("a"), start=False, stop=True)
                for h in H4:
                    fw.copy(U_vn[h]("a", cs, slice(0, 128)), psC[h]("a", cs, slice(0, 128)), eng="act")
                for h in H4:
                    ocols = sl(h * 128 + c * 64, 64)
                    fw.mm(pso("a", A, ocols), Sst[h]("a"), U_qd[h]("a", A, cs), start=True, stop=False)
                    fw.mm(pso("a", A, ocols), U_vn[h]("a", cs, slice(0, 128)), U_at[h]("a", cs, cs), start=False, stop=True)
                for h in H4:
                    psS[h] = cx.ps.get()
                    fw.mm(psS[h]("a", A, slice(0, 128)), U_kd[h]("a", cs, slice(0, 128)), U_vn[h]("a", cs, slice(0, 128)))
                for h in H4:
                    fw.stt(Sst[h]("a"), Sst[h]("a"), cdec("a", A, h, slice(ci, ci + 1)), psS[h]("a", A, slice(0, 128)), ALU.mult, ALU.add)
                pump(2)
            fw.copy(oT.ap("a", oT.h[:, :, g * 128:(g + 1) * 128]), pso.ap("a", pso.h[:, :].rearrange("p (h t) -> p h t", t=128)), eng="act")
        for _ in cg:
            pass
        ck(cx, "dn")
        for h in range(4):
            sq = tmpr.get()
            fw.act(sq("a"), oT("a", A, h, F), AF.Square)
            ps = cx.ps.get()
            fw.mm(ps("a", A, F), cx.ones, sq("a"))
            rn = tmpr.get()
            rstd_from(cx, rn("a"), ps("a", A, F), 1.0 / 128.0, 1e-6, sq("a"))
            fw.tt(rn("a"), rn("a"), oT("a", A, h, F), ALU.mult)
            fw.tt(rn("a"), rn("a"), zs("a", A, h, F), ALU.mult, eng="pool")
            fw.act(oab("a", A, h, F), rn("a"), AF.Identity, scale=ong("a", A, slice(0, 1)))
        ck(cx, "rms")
        for dc in range(8):
            wo = worot.get()
            fw.dma("pool", wo("a"), View(di["ab_w_out"].h[:, dc * 128:(dc + 1) * 128].rearrange("(k p) c -> p k c", p=128), []))
            ps = cx.ps.get()
            for kc in range(8):
                rhs = oab("a", A, kc, F) if kc < 4 else usb("a", A, kc - 4, F)
                fw.mm(ps("a", A, F), wo("a", A, kc, slice(0, 128)), rhs, start=(kc == 0), stop=(kc == 7))
            fw.stt(yT(dc), xT("a", A, dc, F), ALPHA, ps("a", A, F), ALU.mult, ALU.add)
        xo = [x1o.get() if c < 0 else None for c in range(8)]
        outv = []
        def ln_out_cb(c):
            b = x1o.get()
            return b
        bufs = []
        ln_feature_major(cx, ph, [yT(c) for c in range(8)], 8,
                         [lng("a", A, slice(c, c + 1)) for c in range(8)], [lnb("a", A, slice(c, c + 1)) for c in range(8)],
                         None, sq_rot=tmpr, stat=stat, TW=TW, out_rot=x1o,
                         out_cb=lambda c, v: fw.dma("sp", cx.x1((t, c), sl(c * 128, 128), sl(t0, TW)), v))
    ph.close()


def phaseC(cx, TW=256):
    fw, nc, di = cx.fw, cx.nc, cx.di
    ph = Phase(cx)
    NTW = cx.S // TW; NG = TW // 128; NCH = TW // 64
    A = slice(0, 128); F = slice(0, TW); G4 = slice(0, 4)
    Wb = load_w_bf16(cx, ph, di["c_w_in"].h, 8, 3080)
    Wo = load_w_bf16(cx, ph, di["c_w_out"].h, 8, 1024)
    Wg32 = ph.sb([128, 8, 8])
    for kc in range(8):
        fw.dma("sp", Wg32("a", A, kc, slice(0, 8)), View(di["c_w_in"].h[kc * 128:(kc + 1) * 128, 3072:3080], []))
    ng = load_cols(cx, ph, di["c_norm_g"].h[0:1024].rearrange("(c p) -> p c", p=128), [128, 8])
    lng = load_cols(cx, ph, di["ln_mix_g"].h[1, :].rearrange("(c p) -> p c", p=128), [128, 8])
    lnb = load_cols(cx, ph, di["ln_mix_b"].h[1, :].rearrange("(c p) -> p c", p=128), [128, 8])
    hp = ph.sb([4, 4])
    fw.dma("sp", hp("a", G4, slice(0, 1)), View(di["c_b_i"].h[:].rearrange("(p o) -> p o", o=1), []), allow_slow_non_contiguous=True)
    fw.dma("sp", hp("a", G4, slice(1, 2)), View(di["c_b_f"].h[:].rearrange("(p o) -> p o", o=1), []), allow_slow_non_contiguous=True)
    fw.ts(hp("a", G4, slice(2, 4)), hp("a", G4, slice(0, 2)), 1.0 / 15.0, ALU.mult)
    Caug = [ph.sb([128, 384]) for _ in range(4)]
    for h in range(4):
        fw.memset(Caug[h]("a"), 0.0)
    mcar = ph.sb([4, 1]); fw.memset(mcar("a"), 0.0)
    vaug = ph.sb([128, NG, 4, 384])
    fw.memset(vaug("a"), 1.0)
    xT = ph.sb([128, 8, TW]); xTb = ph.sb([128, 8, TW], BF16)
    qT = ph.sb([128, 4, TW]); kT = ph.sb([128, 4, TW])
    ktok = ph.sb([128, NG, 512])
    og = ph.sb([128, 8, TW], BF16)
    GF = ph.sb([4, 12, TW])
    SM = ph.sb([4, 8, NCH])
    TMc = [ph.sb([128, 2, 4]) for _ in range(NG)]
    B1 = ph.sb([128, 8, TW]); B2 = ph.sb([128, 8, TW])
    cmB = lambda h, cols: B1("a", A, h, cols)
    siB = lambda h, cols: B1("a", A, 4 + h, cols)
    saB = lambda h, cols: B2("a", A, h, cols)
    emB = lambda h, cols: B2("a", A, 4 + h, cols)
    decB = ph.sb([128, 4, NCH])
    hT = ph.sb([128, 8, TW])
    hb = ph.sb([128, 8, TW], BF16)
    tmpr = ph.rot(6, [128, TW])
    stat = [ph.sb([128, TW]) for _ in range(4)]
    x3o = ph.rot(3, [128, TW])
    mk4 = lambda shape=(128, 128): [ph.sb(list(shape)) for _ in range(4)]
    U_E = mk4(); U_kw = mk4(); U_pm = mk4(); U_dn = mk4()
    U_t1 = mk4((128, 384)); U_t2 = mk4((128, 384))
    psR = [cx.psb[6], cx.psb[7], cx.psb[4], cx.psb[5]]
    cx.ps = Rot(cx.psb[0:4])
    siB3 = lambda h, cols: B1.ap("a", B1.h[:, 4 + h:5 + h, cols].to_broadcast([128, 3, 128]))
    saB3 = lambda h, cols: B2.ap("a", B2.h[:, h:h + 1, cols].to_broadcast([128, 3, 128]))
    yv = lambda c: B1("a", A, c, F)

    for t in range(NTW):
        t0 = t * TW
        for c in range(8):
            fw.dma("sp", xT("a", A, c, F), cx.x2((t, c), sl(c * 128, 128), sl(t0, TW)))
        fw.copy(xTb("a"), xT("a"), eng="act")

        def inproj(c0):
            ps = cx.ps.get()
            for kc in range(8):
                fw.mm(ps("a", A, F), Wb("w", A, kc, sl(c0, 128)), xTb("a", A, kc, F), start=(kc == 0), stop=(kc == 7))
            return ps
        def gate_gen():
            psi_ = cx.ps.get(); psf_ = cx.ps.get()
            for kc in range(8):
                fw.mm(psi_("a", G4, F), Wg32("a", A, kc, slice(0, 4)), xT("a", A, kc, F), start=(kc == 0), stop=(kc == 7))
            for kc in range(8):
                fw.mm(psf_("a", G4, F), Wg32("a", A, kc, slice(4, 8)), xT("a", A, kc, F), start=(kc == 0), stop=(kc == 7))
            gf = lambda q: GF("a", G4, q, F)
            g3 = lambda q: GF.ap("a", GF.h[0:4, q, :].rearrange("p (c l) -> p c l", l=64))
            smv = lambda q, a=0, n=None: SM("a", G4, q, slice(a, NCH if n is None else a + n))
            smb = lambda q: SM.ap("a", SM.h[0:4, q, :].rearrange("p (c o) -> p c o", o=1).to_broadcast([4, NCH, 64]))
            fw.act(gf(0), psi_("a", G4, F), AF.Tanh, scale=1.0 / 15.0, bias=hp("a", G4, slice(2, 3)))
            fw.ts(gf(0), gf(0), 15.0, ALU.mult)
            fw.act(gf(1), psf_("a", G4, F), AF.Tanh, scale=1.0 / 15.0, bias=hp("a", G4, slice(3, 4)))
            fw.act(gf(1), gf(1), AF.Exp, scale=-15.0)
            yield
            fw.ts(gf(1), gf(1), 1.0, ALU.add)
            fw.act(gf(1), gf(1), AF.Ln)
            fw.ts(gf(1), gf(1), -1.0, ALU.mult)
            rm = C(cx, C_RM, TW, 4); rb = C(cx, C_RB, TW, 4)
            fw.op("dve", lambda: nc.vector.tensor_tensor_scan(gf(2).ap, rm.ap, gf(1).ap, 0.0, ALU.mult, ALU.add), [gf(2)], [rm, gf(1)])
            yield
            fw.tt(gf(3), gf(0), gf(2), ALU.subtract)
            fw.op("dve", lambda: nc.vector.tensor_tensor_scan(gf(4).ap, rb.ap, gf(3).ap, 0.0, ALU.add, ALU.max), [gf(4)], [rb, gf(3)])
            fw.tt(gf(5), gf(2), gf(4), ALU.add)
            fw.copy(smv(0), GF.ap("a", GF.h[0:4, 2, 63::64]))
            fw.tt(smv(1), smv(0), GF.ap("a", GF.h[0:4, 4, 63::64]), ALU.add)
            fw.op("dve", lambda: nc.vector.tensor_tensor_scan(smv(2).ap, smv(0).ap, smv(1).ap, mcar("a").ap, ALU.add, ALU.max),
                  [smv(2)], [smv(0), smv(1), mcar("a")])
            fw.copy(smv(3, 0, 1), mcar("a"))
            if NCH > 1:
                fw.copy(smv(3, 1, NCH - 1), smv(2, 0, NCH - 1))
            fw.copy(mcar("a"), smv(2, NCH - 1, 1))
            yield
            fw.tt(g3(6), g3(2), smb(3), ALU.add)
            fw.tt(gf(7), gf(6), gf(5), ALU.max)
            fw.tt(gf(8), gf(6), gf(7), ALU.subtract); fw.act(gf(8), gf(8), AF.Exp)
            fw.tt(gf(9), gf(5), gf(7), ALU.subtract); fw.act(gf(9), gf(9), AF.Exp)
            fw.act(gf(10), gf(7), AF.Exp, scale=-1.0)
            fw.tt(smv(5), smv(0), smv(2), ALU.subtract)
            fw.tt(g3(11), g3(3), smb(5), ALU.add); fw.act(gf(11), gf(11), AF.Exp)
            fw.tt(smv(4), smv(5), smv(3), ALU.add); fw.act(smv(4), smv(4), AF.Exp)
            yield
            for g in range(NG):
                ps = cx.ps.get()
                for qi, q in enumerate((3, 11)):
                    fw.mm(ps("a", A, sl(qi * 4, 4)), GF("a", G4, q, sl(g * 128, 128)), C(cx, C_ID, 4, 4))
                fw.copy(TMc[g]("a"), ps.ap("a", ps.h[:, 0:8].rearrange("p (q h) -> p q h", h=4)))
            for h in range(4):
                bcast_rows(cx, cmB(h, F), gf(4), C_SEL4, h, 4, TW)
                bcast_rows(cx, siB(h, F), gf(8), C_SEL4, h, 4, TW)
                bcast_rows(cx, saB(h, F), gf(9), C_SEL4, h, 4, TW)
                bcast_rows(cx, emB(h, F), gf(10), C_SEL4, h, 4, TW)
                yield
            ps = cx.ps.get()
            for h in range(4):
                fw.mm(ps("a", A, sl(h * NCH, NCH)), C(cx, C_SEL4 + h * 128, 128, 4), smv(4))
            fw.copy(decB("a"), ps.ap("a", ps.h[:, 0:4 * NCH].rearrange("p (h c) -> p h c", c=NCH)))
            yield
        gg = gate_gen()
        for h in range(4):
            next(gg, None); next(gg, None)
            ps = inproj(h * 128)
            fw.act(qT("a", A, h, F), ps("a", A, F), AF.Copy, scale=128.0 ** -0.5)
            ps = inproj(512 + h * 128)
            fw.copy(kT("a", A, h, F), ps("a", A, F), eng="act")
        for c in range(8):
            next(gg, None); next(gg, None)
            ps = inproj(2048 + c * 128)
            fw.act(og("a", A, c, F), ps("a", A, F), AF.Sigmoid)
        for g in range(NG):
            gs = sl(g * 128, 128)
            next(gg, None); next(gg, None)
            ps = cx.ps.get()
            for kc in range(8):
                fw.mm(ps("a"), xTb("a", A, kc, gs), Wb("w", A, kc, slice(512, 1024)), start=(kc == 0), stop=(kc == 7))
            fw.copy(ktok("a", A, g, slice(0, 512)), ps("a"), eng="act")
            for half in range(2):
                ps = cx.ps.get()
                for kc in range(8):
                    fw.mm(ps("a"), xTb("a", A, kc, gs), Wb("w", A, kc, sl(1024 + half * 512, 512)), start=(kc == 0), stop=(kc == 7))
                fw.copy(vaug.ap("a", vaug.h[:, g, 2 * half:2 * half + 2, 0:256]),
                        ps.ap("a", ps.h[:, :].rearrange("p (h d) -> p h d", d=256)), eng="act")
        for _ in gg:
            pass
        H4 = range(4)
        for g in range(NG):
            gs = sl(g * 128, 128)
            aT = [TMc[g]("a", A, 0, slice(h, h + 1)) for h in H4]; ekT = [TMc[g]("a", A, 1, slice(h, h + 1)) for h in H4]
            for h in H4:
                fw.stt(U_E[h]("a"), cmB(h, gs), aT[h], C(cx, C_NBTI, 128), ALU.subtract, ALU.add)
            for h in H4:
                fw.act(U_E[h]("a"), U_E[h]("a"), AF.Exp, scale=-1.0)
                fw.act(U_kw[h]("a"), ktok("a", A, g, sl(h * 128, 128)), AF.Identity, scale=ekT[h])
            psQ = [None] * 4
            for h in H4:
                psQ[h] = cx.ps.get()
                fw.mm(psQ[h]("a", A, slice(0, 128)), kT("a", A, h, gs), qT("a", A, h, gs))
            for h in H4:
                fw.tt(U_pm[h]("a"), psQ[h]("a", A, slice(0, 128)), U_E[h]("a"), ALU.mult)
            for c in range(2):
                cs = sl(c * 64, 64)
                ci = g * 2 + c
                for h in H4:
                    for j in range(3):
                        fw.mm(psR[h]("a", A, sl(j * 128 + c * 64, 64)), Caug[h]("a", A, sl(j * 128, 128)),
                              qT("a", A, h, sl(g * 128 + c * 64, 64)))
                psS = [None] * 4
                for h in H4:
                    psS[h] = cx.ps.get()
                    fw.mm(psS[h]("a", A, slice(0, 384)), U_kw[h]("a", cs, slice(0, 128)), vaug("a", cs, g, h, slice(0, 384)))
                for h in H4:
                    fw.stt(Caug[h]("a"), Caug[h]("a"), decB("a", A, h, slice(ci, ci + 1)), psS[h]("a", A, slice(0, 384)), ALU.mult, ALU.add)
            for h in H4:
                psI = cx.ps.get()
                for j in range(3):
                    fw.mm(psI("a", A, sl(j * 128, 128)), vaug("a", A, g, h, sl(j * 128, 128)), U_pm[h]("a"))
                t1 = U_t1[h]; t2 = U_t2[h]
                fw.tt(t1("a"), psR[h]("a", A, slice(0, 384)), siB3(h, gs), ALU.mult)
                fw.tt(t2("a"), psI("a", A, slice(0, 384)), saB3(h, gs), ALU.mult)
                fw.tt(t1("a"), t1("a"), t2("a"), ALU.add, eng="pool")
            for h in H4:
                t1 = U_t1[h]; dn = U_dn[h]
                c2 = t1("a", A, slice(256, 384))
                fw.ts(dn("a"), c2, -1.0, ALU.mult, eng="pool")
                fw.tt(dn("a"), dn("a"), c2, ALU.max)
                fw.tt(dn("a"), dn("a"), emB(h, gs), ALU.max)
                fw.op("dve", lambda: nc.vector.reciprocal(dn("a").ap, dn("a").ap), [dn("a")], [dn("a")])
            for h in H4:
                t1 = U_t1[h]; dn = U_dn[h]
                for j in range(2):
                    fw.tt(hT("a", A, h * 2 + j, gs), t1("a", A, sl(j * 128, 128)), dn("a"), ALU.mult)
        for h in range(4):
            ps = cx.ps.get()
            for j in range(2):
                sq = tmpr.get()
                fw.act(sq("a"), hT("a", A, h * 2 + j, F), AF.Square)
                fw.mm(ps("a", A, F), cx.ones, sq("a"), start=(j == 0), stop=(j == 1))
            rn = tmpr.get(); tq = tmpr.get()
            rstd_from(cx, rn("a"), ps("a", A, F), 1.0 / 256.0, 1e-6, tq("a"))
            for j in range(2):
                c = h * 2 + j
                t1 = tmpr.get()
                fw.tt(t1("a"), hT("a", A, c, F), rn("a"), ALU.mult)
                fw.tt(t1("a"), t1("a"), og("a", A, c, F), ALU.mult, eng="pool")
                fw.act(hb("a", A, c, F), t1("a"), AF.Identity, scale=ng("a", A, slice(c, c + 1)))
        for dc in range(8):
            ps = cx.ps.get()
            for kc in range(8):
                fw.mm(ps("a", A, F), Wo("w", A, kc, sl(dc * 128, 128)), hb("a", A, kc, F), start=(kc == 0), stop=(kc == 7))
            fw.stt(yv(dc), xT("a", A, dc, F), ALPHA, ps("a", A, F), ALU.mult, ALU.add)
        ln_feature_major(cx, ph, [yv(c) for c in range(8)], 8,
                         [lng("a", A, slice(c, c + 1)) for c in range(8)], [lnb("a", A, slice(c, c + 1)) for c in range(8)],
                         None, sq_rot=tmpr, stat=stat, TW=TW, out_rot=x3o,
                         out_cb=lambda c, v: fw.dma("sp", cx.x3((t, c), sl(c * 128, 128), sl(t0, TW)), v))
    ph.close()
    cx.ps = Rot(cx.psb[0:6])


def phaseF(cx, layer, xin, xout_scratch):
    fw, nc, di = cx.fw, cx.nc, cx.di
    ph = Phase(cx)
    TW = 512
    NTW = cx.S // TW
    NTS = 2 if NTW % 2 == 0 else 1
    SW = NTS * TW
    A = slice(0, 128); F = slice(0, TW)
    moe = (layer == 1)
    NE = 8 if moe else 1
    if moe:
        wg_ap = lambda e: di["moe_w_gate"].h[e]; wu_ap = lambda e: di["moe_w_up"].h[e]; wd_ap = lambda e: di["moe_w_down"].h[e]
    else:
        wg_ap = lambda e: di["ffn_w_gate"].h; wu_ap = lambda e: di["ffn_w_up"].h; wd_ap = lambda e: di["ffn_w_down"].h
    lng = load_cols(cx, ph, di["ln_ffn_g"].h[layer, :].rearrange("(c p) -> p c", p=128), [128, 8])
    lnb = load_cols(cx, ph, di["ln_ffn_b"].h[layer, :].rearrange("(c p) -> p c", p=128), [128, 8])
    Wpp = load_w_bf16(cx, ph, di["ple_w_proj"].h[layer], 2, 1024)
    if moe:
        Wr = ph.sb([128, 8, 8])
        for kc in range(8):
            fw.dma("sp", Wr("a", A, kc, slice(0, 8)), View(di["moe_w_router"].h[kc * 128:(kc + 1) * 128, :], []))
        brB = ph.sb([128, 8])
        fw.dma("sp", brB("a"), View(di["moe_b_router"].h[:].partition_broadcast(128), []), allow_slow_non_contiguous=True)
        sel8 = ph.sb([8, 1024])
        fw.dma("sp", sel8("a"), di["sel8"]("a"))
        combT = [ph.sb([8, TW]) for _ in range(NTS)]
        combB = [ph.sb([128, TW]) for _ in range(NTS)]
        rt = ph.rot(2, [128, 176])
    xf = [ph.sb([128, 8, TW]) for _ in range(NTS)]
    xb = [ph.sb([128, 8, TW], BF16) for _ in range(NTS)]
    y = [ph.sb([128, 8, TW]) for _ in range(NTS)]
    wgb = ph.rot(2, [128, 8, 512], BF16); wub = ph.rot(2, [128, 8, 512], BF16); wdb = ph.rot(2, [128, 4, 1024], BF16)
    hb = ph.rot(8 * NTS, [128, TW], BF16)
    tmpr = ph.rot(4, [128, TW])
    stat = [ph.sb([128, TW]) for _ in range(4)]
    xin_rot = ph.rot(2, [128, 256])
    pTb = ph.sb([128, 2, TW], BF16)
    otok = ph.rot(2, [128, 1024])
    oT = ph.rot(3, [128, TW]) if xout_scratch is not None else None

    for st in range(NTW // NTS):
        for ti in range(NTS):
            t = st * NTS + ti; t0 = t * TW
            for c in range(8):
                fw.dma("sp", xf[ti]("a", A, c, F), xin((t, c), sl(c * 128, 128), sl(t0, TW)))
            fw.copy(xb[ti]("a"), xf[ti]("a"), eng="act")
            if moe:
                ps = cx.ps.get()
                for g in range(4):
                    gs = sl(g * 128, 128)
                    for kc in range(8):
                        fw.mm(ps("a", A, sl(g * 8, 8)), xf[ti]("a", A, kc, gs), Wr("a", A, kc, slice(0, 8)), start=(kc == 0), stop=(kc == 7))
                r = rt.get()
                R3 = lambda a: r.ap("a", r.h[:, a:a + 32].rearrange("p (g e) -> p g e", e=8))
                R1 = lambda a: r("a", A, sl(a, 4))
                RB = lambda a: r.ap("a", r.h[:, a:a + 4].rearrange("p (g o) -> p g o", o=1).to_broadcast([128, 4, 8]))
                lg = R3(0); eq = R3(32); l2 = R3(64); ex = R3(96); cb = R3(128)
                m1 = R1(160); m2 = R1(164); dd = R1(168)
                fw.tt(lg, ps.ap("a", ps.h[:, 0:32].rearrange("p (g e) -> p g e", e=8)),
                      brB.ap("a", brB.h[:, :].rearrange("p (o e) -> p o e", o=1).to_broadcast([128, 4, 8])), ALU.add)
                fw.reduce(m1, lg, ALU.max)
                fw.tt(eq, lg, RB(160), ALU.is_equal)
                fw.stt(l2, eq, -1.0e30, lg, ALU.mult, ALU.add)
                fw.reduce(m2, l2, ALU.max)
                fw.tt(eq, lg, RB(164), ALU.is_ge)
                fw.tt(ex, lg, RB(160), ALU.subtract)
                fw.act(ex, ex, AF.Exp)
                fw.tt(dd, m2, m1, ALU.subtract)
                fw.act(dd, dd, AF.Exp)
                fw.ts(dd, dd, 1.0, ALU.add)
                fw.op("dve", lambda: nc.vector.reciprocal(dd.ap, dd.ap), [dd], [dd])
                fw.tt(cb, ex, RB(168), ALU.mult)
                fw.tt(cb, cb, eq, ALU.mult)
                ps2 = cx.ps.get()
                for g in range(4):
                    fw.mm(ps2("a", slice(0, 8), sl(g * 128, 128)), r("a", A, sl(128 + g * 8, 8)), cx.ident)
                fw.copy(combT[ti]("a"), ps2("a", slice(0, 8), F), eng="act")
        for e in range(NE):
            if moe:
                for ti in range(NTS):
                    ps = cx.ps.get()
                    fw.mm(ps("a", A, F), sel8("a", slice(0, 8), sl(e * 128, 128)), combT[ti]("a"))
                    fw.copy(combB[ti]("a"), ps("a", A, F), eng="act")
            for f in range(7):
                wg = wgb.get(); wu = wub.get(); wd = wdb.get()
                fw.dma("pool", wg("a"), View(wg_ap(e)[:, f * 512:(f + 1) * 512].rearrange("(k p) c -> p k c", p=128), []))
                fw.dma("pool", wu("a"), View(wu_ap(e)[:, f * 512:(f + 1) * 512].rearrange("(k p) c -> p k c", p=128), []))
                fw.dma("pool", wd("a"), View(wd_ap(e)[f * 512:(f + 1) * 512, :].rearrange("(k p) c -> p k c", p=128), []))
                hs = [[None] * 4 for _ in range(NTS)]
                for fc in range(4):
                    for ti in range(NTS):
                        pg = cx.ps.get(); pu = cx.ps.get()
                        for kc in range(8):
                            fw.mm(pg("a", A, F), wg("a", A, kc, sl(fc * 128, 128)), xb[ti]("a", A, kc, F), start=(kc == 0), stop=(kc == 7))
                        for kc in range(8):
                            fw.mm(pu("a", A, F), wu("a", A, kc, sl(fc * 128, 128)), xb[ti]("a", A, kc, F), start=(kc == 0), stop=(kc == 7))
                        sg = tmpr.get()
                        fw.act(sg("a"), pg("a", A, F), AF.Silu)
                        h = hb.get()
                        if moe:
                            fw.tt(sg("a"), sg("a"), pu("a", A, F), ALU.mult)
                            fw.tt(h("a"), sg("a"), combB[ti]("a"), ALU.mult)
                        else:
                            fw.tt(h("a"), sg("a"), pu("a", A, F), ALU.mult)
                        hs[ti][fc] = h
                for dc in range(8):
                    for ti in range(NTS):
                        pd = cx.ps.get()
                        for fc in range(4):
                            fw.mm(pd("a", A, F), wd("a", A, fc, sl(dc * 128, 128)), hs[ti][fc]("a"), start=(fc == 0), stop=(fc == 3))
                        if e == 0 and f == 0:
                            fw.stt(y[ti]("a", A, dc, F), xf[ti]("a", A, dc, F), ALPHA, pd("a", A, F), ALU.mult, ALU.add)
                        else:
                            fw.tt(y[ti]("a", A, dc, F), pd("a", A, F), y[ti]("a", A, dc, F), ALU.add)
        for ti in range(NTS):
            t = st * NTS + ti; t0 = t * TW
            yv = y[ti]; xx = xf[ti]; xxb = xb[ti]
            ln_feature_major(cx, ph, [yv("a", A, c, F) for c in range(8)], 8,
                             [lng("a", A, slice(c, c + 1)) for c in range(8)], [lnb("a", A, slice(c, c + 1)) for c in range(8)],
                             [xx("a", A, c, F) for c in range(8)], sq_rot=tmpr, stat=stat, TW=TW)
            fw.copy(xxb("a"), xx("a"), eng="act")
            load_x_tile_T(cx, di["p"].h[layer], t0, TW, None, pTb, xin_rot, nchunks=2)
            for half in range(2):
                wpg = wgb.get()
                fw.dma("pool", wpg("a"), View(di["ple_w_gate"].h[layer][:, half * 512:(half + 1) * 512].rearrange("(k p) c -> p k c", p=128), []))
                for d4 in range(4):
                    dc = half * 4 + d4
                    pg = cx.ps.get(); pp = cx.ps.get()
                    for kc in range(8):
                        fw.mm(pg("a", A, F), wpg("a", A, kc, sl(d4 * 128, 128)), xxb("a", A, kc, F), start=(kc == 0), stop=(kc == 7))
                    for kc in range(2):
                        fw.mm(pp("a", A, F), Wpp("w", A, kc, sl(dc * 128, 128)), pTb("a", A, kc, F), start=(kc == 0), stop=(kc == 1))
                    sg = tmpr.get()
                    fw.act(sg("a"), pg("a", A, F), AF.Sigmoid)
                    fw.tt(sg("a"), sg("a"), pp("a", A, F), ALU.mult)
                    if xout_scratch is not None:
                        o = oT.get()
                        fw.tt(o("a"), sg("a"), xx("a", A, dc, F), ALU.add, eng="pool")
                        fw.dma("sp", xout_scratch((t, dc), sl(dc * 128, 128), sl(t0, TW)), o("a"))
                    else:
                        fw.tt(yv("a", A, dc, F), sg("a"), xx("a", A, dc, F), ALU.add, eng="pool")
            if xout_scratch is None:
                for g in range(4):
                    ot = otok.get()
                    for d0 in (0, 4):
                        ps = cx.ps.get()
                        for d4 in range(4):
                            fw.transpose(ps("a", A, sl(d4 * 128, 128)), yv("a", A, d0 + d4, sl(g * 128, 128)), cx.ident)
                        fw.copy(ot("a", A, sl(d0 * 128, 512)), ps("a"), eng="act")
                    fw.dma("sp", cx.out((t, g), sl(t0 + g * 128, 128), slice(0, 1024)), ot("a"))
    ph.close()


from concourse.bass_utils import run_bass_kernel_spmd

SEQ = 4096
NCORES = 8
_SQUEEZE = ("ab_w_in", "ab_conv_qkv", "ab_a_log", "ab_dt_bias", "ab_o_norm_g", "ab_dw_w", "ab_dw_b", "ab_cn_g",
            "ab_cn_b", "ab_w_out", "ffn_w_gate", "ffn_w_up", "ffn_w_down", "c_w_in", "c_b_i", "c_b_f", "c_norm_g",
            "c_w_out", "moe_w_router", "moe_b_router", "moe_w_gate", "moe_w_up", "moe_w_down")


def build_program(S):
    nc = bass.Bass("TRN2", target_bir_lowering=False)
    cx = setup(nc, S)
    phaseA(cx)
    phaseF(cx, 0, cx.x1, cx.x2)
    phaseC(cx)
    phaseF(cx, 1, cx.x3, None)
    cx.fw.barrier()
    return nc


def kernel(**inputs):
    f32 = lambda a: np.ascontiguousarray(np.asarray(a), dtype=np.float32)
    x = f32(inputs["x"]); p = f32(inputs["p"])
    B, S, _ = x.shape
    shared = {}
    for k, v in inputs.items():
        if k in ("x", "p"):
            continue
        a = f32(v)
        if k in _SQUEEZE:
            a = np.ascontiguousarray(a[0])
        shared[k] = a
    shared["cst"] = make_consts()
    shared["sel8"] = make_sel8()
    nc = build_program(S)
    in_maps = []
    for b in range(B):
        m = dict(shared)
        m["x"] = np.ascontiguousarray(x[b])
        m["p"] = np.ascontiguousarray(p[:, b])
        in_maps.append(m)
    res = run_bass_kernel_spmd(nc, in_maps, core_ids=list(range(B)))
    return np.stack([np.asarray(r["out"], dtype=np.float32) for r in res.results], axis=0)
```

```python
import numpy as np
import concourse.bass as bass
import concourse.mybir as mybir

F32 = mybir.dt.float32
BF16 = mybir.dt.bfloat16
AF = mybir.ActivationFunctionType
ALU = mybir.AluOpType
AX = mybir.AxisListType


class Trk:
    __slots__ = ("writer", "readers")

    def __init__(self):
        self.writer = None
        self.readers = {}


class View:
    __slots__ = ("ap", "trks")

    def __init__(self, ap, trks):
        self.ap = ap
        self.trks = trks


class TB:
    def __init__(self, handle):
        self.h = handle
        self.trk = {}

    def __call__(self, key, *idx):
        t = self.trk.get(key)
        if t is None:
            t = self.trk[key] = Trk()
        ap = self.h[idx] if idx else self.h[:]
        return View(ap, [t])

    def ap(self, key, ap):
        t = self.trk.get(key)
        if t is None:
            t = self.trk[key] = Trk()
        return View(ap, [t])

    def multi(self, keys, *idx):
        ts = []
        for key in keys:
            t = self.trk.get(key)
            if t is None:
                t = self.trk[key] = Trk()
            ts.append(t)
        ap = self.h[idx] if idx else self.h[:]
        return View(ap, ts)


class FW:
    NDMA = 48

    def __init__(self, nc):
        self.nc = nc
        self.eng = {"pe": nc.tensor, "dve": nc.vector, "act": nc.scalar,
                    "pool": nc.gpsimd, "sp": nc.sync}
        self.sem = {}
        self.cnt = {}
        for k in self.eng:
            self.sem[k] = nc.alloc_semaphore("s_" + k)
            self.cnt[k] = 0
        self.dsem = [nc.alloc_semaphore("d%d" % i) for i in range(self.NDMA)]
        self.dcnt = [0] * self.NDMA
        self.dnext = 0
        self.dnext_sw = 0
        self.waited = {k: {} for k in self.eng}
        self.ninst = 0
        self.nwait = 0

    def _sem(self, key):
        return self.sem[key] if isinstance(key, str) else self.dsem[key]

    def _need(self, eng, ev, needs):
        if ev is None:
            return
        key, val = ev
        if key == eng and eng == "pe":
            return
        if self.waited[eng].get(key, 0) >= val:
            return
        if needs.get(key, 0) < val:
            needs[key] = val

    def _collect(self, eng, outs, ins):
        needs = {}
        for v in ins:
            for t in v.trks:
                self._need(eng, t.writer, needs)
        for v in outs:
            for t in v.trks:
                self._need(eng, t.writer, needs)
                for key, ev in t.readers.items():
                    self._need(eng, ev, needs)
        return needs

    def _emit_waits(self, eng, items):
        e = self.eng[eng]
        for key, val in items:
            e.wait_ge(self._sem(key), val)
            self.waited[eng][key] = val
            self.nwait += 1

    def _deps(self, eng, outs, ins):
        self._emit_waits(eng, list(self._collect(eng, outs, ins).items()))

    def _record(self, ev, outs, ins):
        for v in ins:
            for t in v.trks:
                old = t.readers.get(ev[0])
                if old is None or old[1] < ev[1]:
                    t.readers[ev[0]] = ev
        for v in outs:
            for t in v.trks:
                t.writer = ev
                t.readers = {}

    def op(self, eng, fn, outs, ins, inc=True):
        items = list(self._collect(eng, outs, ins).items())
        self._emit_waits(eng, items[:-1])
        inst = fn()
        if items:
            key, val = items[-1]
            inst._wait_ge(self._sem(key), val)
            self.waited[eng][key] = val
        if inc:
            self.cnt[eng] += 1
            inst.then_inc(self.sem[eng], 1)
            ev = (eng, self.cnt[eng])
        else:
            assert eng == "pe"
            ev = (eng, self.cnt[eng] + 1)
        self.ninst += 1
        self._record(ev, outs, ins)
        return inst

    def dma(self, q, out, in_, **kw):
        half = self.NDMA // 2
        if q == "pool":
            i = half + self.dnext_sw
            self.dnext_sw = (self.dnext_sw + 1) % half
        else:
            i = self.dnext
            self.dnext = (self.dnext + 1) % half
        needs = {}
        if self.dcnt[i] > 0:
            self._need(q, (i, self.dcnt[i]), needs)
        for key, val in needs.items():
            self.eng[q].wait_ge(self._sem(key), val)
            self.waited[q][key] = val
        self._deps(q, [out], [in_])
        inst = self.eng[q].dma_start(out=out.ap, in_=in_.ap, **kw)
        self.dcnt[i] += 16
        inst.then_inc(self.dsem[i], 16)
        self.ninst += 1
        self._record((i, self.dcnt[i]), [out], [in_])
        return inst

    def barrier(self):
        for e in self.eng:
            for o in self.eng:
                if o != e and self.cnt[o] > self.waited[e].get(o, 0):
                    self.eng[e].wait_ge(self.sem[o], self.cnt[o])
                    self.waited[e][o] = self.cnt[o]
            for i in range(self.NDMA):
                if self.dcnt[i] > self.waited[e].get(i, 0):
                    self.eng[e].wait_ge(self.dsem[i], self.dcnt[i])
                    self.waited[e][i] = self.dcnt[i]

    def wait_all(self, eng, views):
        needs = {}
        for v in views:
            for t in v.trks:
                self._need(eng, t.writer, needs)
        for key, val in needs.items():
            self.eng[eng].wait_ge(self._sem(key), val)
            self.waited[eng][key] = val

    def mm(self, out, lhsT, rhs, start=True, stop=True, lazy=False, **kw):
        return self.op("pe", lambda: self.nc.tensor.matmul(out.ap, lhsT.ap, rhs.ap, start=start, stop=stop, **kw),
                       [out], [lhsT, rhs], inc=(bool(stop) or not lazy))

    def transpose(self, out, in_, ident):
        return self.op("pe", lambda: self.nc.tensor.matmul(out.ap, in_.ap, ident.ap, start=True, stop=True), [out], [in_, ident])

    def act(self, out, in_, func, bias=None, scale=None, eng="act", accum_out=None):
        ins = [in_]
        kw = {}
        if bias is not None:
            if isinstance(bias, View):
                ins.append(bias); kw["bias"] = bias.ap
            else:
                kw["bias"] = bias
        if scale is not None:
            if isinstance(scale, View):
                ins.append(scale); kw["scale"] = scale.ap
            else:
                kw["scale"] = scale
        outs = [out]
        if accum_out is not None:
            outs.append(accum_out); kw["accum_out"] = accum_out.ap
        return self.op("act", lambda: self.nc.scalar.activation(out.ap, in_.ap, func, **kw), outs, ins)

    def _ve(self, eng):
        return self.eng[eng]

    def tt(self, out, a, b, op, eng="dve"):
        return self.op(eng, lambda: self._ve(eng).tensor_tensor(out.ap, a.ap, b.ap, op), [out], [a, b])

    def ts(self, out, a, s1, op0, s2=None, op1=None, eng="dve", accum_out=None):
        ins = [a]
        s1a = s1
        if isinstance(s1, View):
            ins.append(s1); s1a = s1.ap
        s2a = s2
        if isinstance(s2, View):
            ins.append(s2); s2a = s2.ap
        kw = {}
        outs = [out]
        if op1 is not None:
            kw["op1"] = op1
        if accum_out is not None:
            kw["accum_out"] = accum_out.ap; outs.append(accum_out)
        return self.op(eng, lambda: self._ve(eng).tensor_scalar(out.ap, a.ap, s1a, s2a, op0, **kw), outs, ins)

    def stt(self, out, a, s, b, op0, op1, eng="dve"):
        ins = [a, b]
        sa = s
        if isinstance(s, View):
            ins.append(s); sa = s.ap
        return self.op(eng, lambda: self._ve(eng).scalar_tensor_tensor(out.ap, a.ap, sa, b.ap, op0, op1), [out], ins)

    def copy(self, out, in_, eng="dve"):
        if eng == "act":
            return self.op("act", lambda: self.nc.scalar.copy(out.ap, in_.ap), [out], [in_])
        return self.op(eng, lambda: self._ve(eng).tensor_copy(out.ap, in_.ap), [out], [in_])

    def memset(self, out, val, eng="dve"):
        return self.op(eng, lambda: self._ve(eng).memset(out.ap, val), [out], [])

    def reduce(self, out, in_, op, axis=AX.X, eng="dve"):
        return self.op(eng, lambda: self._ve(eng).tensor_reduce(out.ap, in_.ap, axis, op), [out], [in_])

import contextlib
import numpy as np
import concourse.bass as bass
import concourse.mybir as mybir

D = 1024
TT = 512
NEG = 1.0e4
ALPHA = 4.0 ** 0.25
C_ID, C_ONES, C_NBS, C_NBTI, C_SM01T, C_SEL4, C_RM, C_RB, C_END = 0, 128, 256, 384, 512, 640, 1152, 1664, 2176
C_SEL8 = 0


def make_consts():
    c = np.zeros((128, C_END), np.float32)
    idx = np.arange(128)
    same = (idx[:, None] // 64) == (idx[None, :] // 64)
    c[:, C_ID:C_ID + 128] = np.eye(128)
    c[:, C_ONES:C_ONES + 128] = 1.0
    c[:, C_NBS:C_NBS + 128] = np.where(same & (idx[None, :] < idx[:, None]), 0.0, NEG)
    c[:, C_NBTI:C_NBTI + 128] = np.where(same & (idx[None, :] >= idx[:, None]), 0.0, NEG)
    c[:, C_SM01T:C_SM01T + 128] = np.where(same & (idx[None, :] > idx[:, None]), 1.0, 0.0)
    for h in range(4):
        c[h, C_SEL4 + h * 128:C_SEL4 + (h + 1) * 128] = 1.0
    t = np.arange(512)
    c[0:4, C_RM:C_RM + 512] = np.where(t % 64 == 0, 0.0, 1.0)[None, :]
    c[0:4, C_RB:C_RB + 512] = np.where(t % 64 == 0, -1.0e30, 0.0)[None, :]
    return c


def make_sel8():
    c = np.zeros((8, 1024), np.float32)
    for e in range(8):
        c[e, e * 128:(e + 1) * 128] = 1.0
    return c


class Ctx:
    pass


class Rot:
    def __init__(self, bufs):
        self.bufs = bufs
        self.i = 0

    def get(self):
        b = self.bufs[self.i]
        self.i = (self.i + 1) % len(self.bufs)
        return b


def sl(a, n):
    return slice(a, a + n)


def setup(nc, S, dbg=False):
    cx = Ctx()
    cx.nc = nc
    cx.S = S
    cx.NT = S // TT
    fw = cx.fw = FW(nc)
    di = {}

    def din(name, shape):
        di[name] = TB(nc.dram_tensor(name, list(shape), F32, kind="ExternalInput"))

    din("x", (S, D)); din("p", (2, S, 256))
    din("ab_w_in", (D, 3080)); din("ab_conv_qkv", (4, 1536)); din("ab_a_log", (4,)); din("ab_dt_bias", (4,))
    din("ab_o_norm_g", (128,)); din("ab_dw_w", (31, 512)); din("ab_dw_b", (512,)); din("ab_cn_g", (512,))
    din("ab_cn_b", (512,)); din("ab_w_out", (D, D)); din("ffn_w_gate", (D, 3584)); din("ffn_w_up", (D, 3584))
    din("ffn_w_down", (3584, D)); din("c_w_in", (D, 3080)); din("c_b_i", (4,)); din("c_b_f", (4,))
    din("c_norm_g", (1024,)); din("c_w_out", (D, D)); din("moe_w_router", (D, 8)); din("moe_b_router", (8,))
    din("moe_w_gate", (8, D, 3584)); din("moe_w_up", (8, D, 3584)); din("moe_w_down", (8, 3584, D))
    din("ln_mix_g", (2, D)); din("ln_mix_b", (2, D)); din("ln_ffn_g", (2, D)); din("ln_ffn_b", (2, D))
    din("ple_w_proj", (2, 256, D)); din("ple_w_gate", (2, D, D)); din("cst", (128, C_END)); din("sel8", (8, 1024))
    cx.di = di
    cx.out = TB(nc.dram_tensor("out", [S, D], F32, kind="ExternalOutput"))
    kd = "ExternalOutput" if dbg else "Internal"
    cx.x1 = TB(nc.dram_tensor("x1s", [D, S], F32, kind=kd))
    cx.x2 = TB(nc.dram_tensor("x2s", [D, S], F32, kind=kd))
    cx.x3 = TB(nc.dram_tensor("x3s", [D, S], F32, kind=kd))
    cx.psb = [TB(nc.alloc_psum_tensor("ps%d" % i, [128, 512], F32)) for i in range(8)]
    cx.ps = Rot(cx.psb[0:6])
    cx.psl = Rot(cx.psb[6:8])
    cx.cst = TB(nc.alloc_sbuf_tensor("cst_sb", [128, C_END], F32))
    fw.dma("sp", cx.cst("c"), di["cst"]("c"))
    cx.ident = cx.cst("c", slice(0, 128), sl(C_ID, 128))
    cx.ones = cx.cst("c", slice(0, 128), sl(C_ONES, 128))
    return cx


def C(cx, off, n, rows=128):
    return cx.cst("c", slice(0, rows), sl(off, n))


class Phase:
    uid = 0

    def __init__(self, cx):
        self.cx = cx
        self.es = contextlib.ExitStack()
        self.n = 0

    def sb(self, shape, dtype=F32, name=None):
        self.n += 1
        Phase.uid += 1
        nm = name or ("t%d" % Phase.uid)
        return TB(self.es.enter_context(self.cx.nc.sbuf_tensor(nm, list(shape), dtype)))

    def rot(self, n, shape, dtype=F32):
        return Rot([self.sb(shape, dtype) for _ in range(n)])

    def close(self):
        self.cx.fw.barrier()
        self.es.close()


def bcast_rows(cx, dst, src_rows, sel_off, h, nrows, n=512):
    fw = cx.fw
    ps = cx.ps.get()
    fw.mm(ps("a", slice(0, 128), slice(0, n)), C(cx, sel_off + h * 128, 128, nrows), src_rows)
    fw.copy(dst, ps("a", slice(0, 128), slice(0, n)), eng="act")


def rstd_from(cx, out, in_, scale, eps, tmp):
    fw = cx.fw
    fw.ts(tmp, in_, scale, ALU.mult, eps, ALU.add)
    fw.act(tmp, tmp, AF.Ln)
    fw.act(out, tmp, AF.Exp, scale=-0.5)


def ln_feature_major(cx, ph, y, nchunk, gcol, bcol, outs, eps=1e-5, func=AF.Identity, sq_rot=None, stat=None,
                     TW=512, out_rot=None, out_cb=None):
    fw = cx.fw
    n = float(nchunk * 128)
    A = slice(0, 128); F = slice(0, TW)
    ps1 = cx.ps.get(); ps2 = cx.ps.get()
    for c in range(nchunk):
        fw.mm(ps1("a", A, F), cx.ones, y[c], start=(c == 0), stop=(c == nchunk - 1))
    for c in range(nchunk):
        sq = sq_rot.get()
        fw.act(sq("a"), y[c], AF.Square)
        fw.mm(ps2("a", A, F), cx.ones, sq("a"), start=(c == 0), stop=(c == nchunk - 1))
    mean, msq, rstd, tmp = stat
    fw.act(mean("a"), ps1("a", A, F), AF.Copy, scale=1.0 / n)
    fw.tt(msq("a"), mean("a"), mean("a"), ALU.mult)
    fw.stt(tmp("a"), ps2("a", A, F), 1.0 / n, msq("a"), ALU.mult, ALU.subtract)
    fw.ts(tmp("a"), tmp("a"), eps, ALU.add)
    fw.act(tmp("a"), tmp("a"), AF.Ln)
    fw.act(rstd("a"), tmp("a"), AF.Exp, scale=-0.5)
    for c in range(nchunk):
        t = sq_rot.get()
        fw.tt(t("a"), y[c], mean("a"), ALU.subtract)
        fw.tt(t("a"), t("a"), rstd("a"), ALU.mult, eng="pool")
        if outs is not None:
            fw.act(outs[c], t("a"), func, scale=gcol[c], bias=bcol[c])
        else:
            o = out_rot.get()
            fw.act(o("a"), t("a"), func, scale=gcol[c], bias=bcol[c])
            out_cb(c, o("a"))


def load_cols(cx, ph, dram_ap_rearranged, shape, q="sp"):
    t = ph.sb(shape)
    cx.fw.dma(q, t("a"), View(dram_ap_rearranged, []), allow_slow_non_contiguous=True)
    return t


def load_kcp(cx, ph, w2d, K, Cn):
    t = ph.sb([128, Cn, K])
    for kk in range(K):
        cx.fw.dma("sp", t.ap("a", t.h[:, :, kk]), View(w2d[kk, :].rearrange("(c p) -> p c", p=128), []),
                  allow_slow_non_contiguous=True)
    return t


class StopBuild(Exception):
    pass


def ck(cx, name):
    if getattr(cx, "stop_at", None) == name:
        raise StopBuild(name)


def mark(cx, name, t=None):
    pt = getattr(cx, "prof_tile", None)
    cur = getattr(cx, "_scope", None)
    if cur is not None:
        cx.nc.leave_named_scope(cur) if hasattr(cx.nc, "leave_named_scope") else None
        cx._scope = None
    if pt is not None and t == pt and name is not None:
        cx.nc.enter_named_scope(name)
        cx._scope = name


def load_w_bf16(cx, ph, wap, kchunks, ncols, q="pool"):
    t = ph.sb([128, kchunks, ncols], BF16)
    for kc in range(kchunks):
        cx.fw.dma(q, t("w", slice(0, 128), kc, slice(0, ncols)), View(wap[kc * 128:(kc + 1) * 128, :], []))
    return t


def load_x_tile_T(cx, src_ap, t0, TW, xT, xTb, xin_rot, nchunks=8):
    fw = cx.fw
    for g in range(TW // 128):
        b = xin_rot.get()
        fw.dma("sp", b("a", slice(0, 128), slice(0, nchunks * 128)), View(src_ap[t0 + g * 128:t0 + (g + 1) * 128, :], []))
        ck(cx, "xdma")
        for k0 in range(0, nchunks, 4):
            nk = min(4, nchunks - k0)
            ps = cx.ps.get()
            for k in range(nk):
                fw.transpose(ps("a", slice(0, 128), sl(k * 128, 128)), b("a", slice(0, 128), sl((k0 + k) * 128, 128)), cx.ident)
            ck(cx, "xtr")
            src = ps.ap("a", ps.h[:, 0:nk * 128].rearrange("p (k t) -> p k t", t=128))
            if xT is not None:
                fw.copy(xT.ap("a", xT.h[:, k0:k0 + nk, g * 128:(g + 1) * 128]), src, eng="act")
            ck(cx, "xcp1")
            if xTb is not None and xT is not None:
                fw.copy(xTb.ap("a", xTb.h[:, k0:k0 + nk, g * 128:(g + 1) * 128]), xT.ap("a", xT.h[:, k0:k0 + nk, g * 128:(g + 1) * 128]), eng="dve")
            elif xTb is not None:
                fw.copy(xTb.ap("a", xTb.h[:, k0:k0 + nk, g * 128:(g + 1) * 128]), src, eng="act")


def phaseA(cx, TW=256):
    fw, nc, di = cx.fw, cx.nc, cx.di
    ph = Phase(cx)
    NTW = cx.S // TW
    NG = TW // 128
    NCH = TW // 64
    Wb = load_w_bf16(cx, ph, di["ab_w_in"].h, 8, 3080)
    Wg32 = ph.sb([128, 8, 8])
    for kc in range(8):
        fw.dma("sp", Wg32("a", slice(0, 128), kc, slice(0, 8)), View(di["ab_w_in"].h[kc * 128:(kc + 1) * 128, 2048:2056], []))
    cw = load_kcp(cx, ph, di["ab_conv_qkv"].h, 4, 12)
    dww = load_kcp(cx, ph, di["ab_dw_w"].h, 31, 4)
    dwb = load_cols(cx, ph, di["ab_dw_b"].h[:].rearrange("(c p) -> p c", p=128), [128, 4])
    cng = load_cols(cx, ph, di["ab_cn_g"].h[:].rearrange("(c p) -> p c", p=128), [128, 4])
    cnb = load_cols(cx, ph, di["ab_cn_b"].h[:].rearrange("(c p) -> p c", p=128), [128, 4])
    ong = load_cols(cx, ph, di["ab_o_norm_g"].h[:].rearrange("(c p) -> p c", p=128), [128, 1])
    lng = load_cols(cx, ph, di["ln_mix_g"].h[0, :].rearrange("(c p) -> p c", p=128), [128, 8])
    lnb = load_cols(cx, ph, di["ln_mix_b"].h[0, :].rearrange("(c p) -> p c", p=128), [128, 8])
    hp = ph.sb([4, 4])
    fw.dma("sp", hp("a", slice(0, 4), slice(0, 1)), View(di["ab_a_log"].h[:].rearrange("(p o) -> p o", o=1), []), allow_slow_non_contiguous=True)
    fw.dma("sp", hp("a", slice(0, 4), slice(1, 2)), View(di["ab_dt_bias"].h[:].rearrange("(p o) -> p o", o=1), []), allow_slow_non_contiguous=True)
    fw.act(hp("a", slice(0, 4), slice(2, 3)), hp("a", slice(0, 4), slice(0, 1)), AF.Exp)
    fw.ts(hp("a", slice(0, 4), slice(2, 3)), hp("a", slice(0, 4), slice(2, 3)), -1.0, ALU.mult)
    ck(cx, "w")
    halo_q = ph.sb([128, 12, 3]); fw.memset(halo_q("a"), 0.0)
    ucv = [ph.sb([128, 30 + TW]) for _ in range(4)]
    for c in range(4):
        fw.memset(ucv[c]("a", slice(0, 128), slice(0, 30)), 0.0)
    Sst = [ph.sb([128, 128]) for _ in range(4)]
    for h in range(4):
        fw.memset(Sst[h]("a"), 0.0)
    ck(cx, "mem")
    xin_rot = ph.rot(2, [128, 1024])
    xT = ph.sb([128, 8, TW]); xTb = ph.sb([128, 8, TW], BF16)
    cvr = ph.rot(2, [128, 3 + TW])
    qkv = ph.sb([128, 12, TW])
    zs = ph.sb([128, 4, TW], BF16)
    tmpr = ph.rot(6, [128, TW])
    GF = ph.sb([4, 8, TW])
    TM = [ph.sb([128, 5, 4]) for _ in range(NG)]
    BIG1 = ph.sb([128, 8, TW]); BIG2 = ph.sb([128, 8, TW])
    gcB = lambda h, cols: BIG1(("c", h), slice(0, 128), h, cols)
    nbB = lambda h, cols: BIG1(("c", 4 + h), slice(0, 128), 4 + h, cols)
    egB = lambda h, cols: BIG2(("c", h), slice(0, 128), h, cols)
    acc = lambda c: BIG2(("c", 4 + c), slice(0, 128), 4 + c, slice(0, TW))
    yT = lambda c: BIG1(("c", c), slice(0, 128), c, slice(0, TW))
    cdec = ph.sb([128, 4, NCH])
    usb = ph.sb([128, 4, TW], BF16)
    oab = ph.sb([128, 4, TW], BF16)
    oT = ph.sb([128, 4, TW])
    stat = [ph.sb([128, TW]) for _ in range(4)]
    x1o = ph.rot(3, [128, TW])
    mk4 = lambda shape=(128, 128): [ph.sb(list(shape)) for _ in range(4)]
    U_nk = mk4(); U_kd = mk4(); U_vb = mk4(); U_ds = mk4(); U_dt = mk4(); U_t1 = mk4(); U_at = mk4()
    U_nw = mk4(); U_qd = mk4(); U_vn = mk4()
    U_PQ = [[ph.sb([128, 256]) for _ in range(2)] for _ in range(4)]
    U_X = [[ph.sb([128, 128]) for _ in range(2)] for _ in range(4)]
    worot = ph.rot(2, [128, 8, 128], BF16)
    A = slice(0, 128)
    F = slice(0, TW)
    G4 = slice(0, 4)

    for t in range(NTW):
        t0 = t * TW
        load_x_tile_T(cx, di["x"].h, t0, TW, xT, xTb, xin_rot)
        ck(cx, "x")
        def inproj(c0):
            ps = cx.ps.get()
            for kc in range(8):
                fw.mm(ps("a", A, F), Wb("w", A, kc, sl(c0, 128)), xTb("a", A, kc, F), start=(kc == 0), stop=(kc == 7))
            return ps
        def gate_gen():
            psb_ = cx.ps.get(); psd_ = cx.ps.get()
            for kc in range(8):
                fw.mm(psb_("a", G4, F), Wg32("a", A, kc, slice(0, 4)), xT("a", A, kc, F), start=(kc == 0), stop=(kc == 7))
            for kc in range(8):
                fw.mm(psd_("a", G4, F), Wg32("a", A, kc, slice(4, 8)), xT("a", A, kc, F), start=(kc == 0), stop=(kc == 7))
            gf = lambda q: GF("a", G4, q, F)
            fw.act(gf(0), psb_("a", G4, F), AF.Sigmoid)
            fw.ts(gf(3), gf(0), -1.0, ALU.mult)
            fw.act(gf(7), psd_("a", G4, F), AF.Identity, bias=hp("a", G4, slice(1, 2)))
            yield
            fw.ts(gf(6), gf(7), 0.0, ALU.max)
            fw.stt(gf(5), gf(6), -2.0, gf(7), ALU.mult, ALU.add)
            fw.act(gf(5), gf(5), AF.Exp)
            fw.ts(gf(5), gf(5), 1.0, ALU.add)
            fw.act(gf(5), gf(5), AF.Ln)
            yield
            fw.tt(gf(5), gf(5), gf(6), ALU.add)
            fw.ts(gf(1), gf(5), hp("a", G4, slice(2, 3)), ALU.mult)
            rm = C(cx, C_RM, TW, 4)
            fw.op("dve", lambda: nc.vector.tensor_tensor_scan(gf(2).ap, rm.ap, gf(1).ap, 0.0, ALU.mult, ALU.add),
                  [gf(2)], [rm, gf(1)])
            fw.act(gf(4), gf(2), AF.Exp)
            fw.tt(gf(5), gf(3), gf(4), ALU.mult)
            gc3 = GF.ap("a", GF.h[0:4, 2, :].rearrange("p (c l) -> p c l", l=64))
            gcl = GF.ap("a", GF.h[0:4, 2, :].rearrange("p (c l) -> p c l", l=64)[:, :, 63:64].to_broadcast([4, NCH, 64]))
            kds3 = GF.ap("a", GF.h[0:4, 6, :].rearrange("p (c l) -> p c l", l=64))
            fw.tt(kds3, gcl, gc3, ALU.subtract)
            yield
            fw.act(gf(6), gf(6), AF.Exp)
            for g in range(NG):
                ps = cx.ps.get()
                for qi, q in enumerate((2, 3, 5, 0, 6)):
                    fw.mm(ps("a", A, sl(qi * 4, 4)), GF("a", G4, q, sl(g * 128, 128)), C(cx, C_ID, 4, 4))
                fw.copy(TM[g]("a"), ps.ap("a", ps.h[:, 0:20].rearrange("p (q h) -> p q h", h=4)))
                yield
            for h in range(4):
                bcast_rows(cx, gcB(h, F), gf(2), C_SEL4, h, 4, TW)
                bcast_rows(cx, nbB(h, F), gf(3), C_SEL4, h, 4, TW)
                bcast_rows(cx, egB(h, F), gf(4), C_SEL4, h, 4, TW)
                yield
            ps = cx.ps.get()
            for h in range(4):
                fw.mm(ps("a", A, sl(h * NCH, NCH)), C(cx, C_SEL4 + h * 128, 128, 4), GF.ap("a", GF.h[0:4, 4, 63::64]))
            fw.copy(cdec("a"), ps.ap("a", ps.h[:, 0:4 * NCH].rearrange("p (h c) -> p h c", c=NCH)))
            yield
        gg = gate_gen()
        for c in range(12):
            next(gg, None); next(gg, None)
            ps = inproj(c * 128)
            cv = cvr.get()
            fw.copy(cv("a", A, slice(0, 3)), halo_q("a", A, c, slice(0, 3)), eng="pool")
            fw.copy(cv("a", A, slice(3, 3 + TW)), ps("a", A, F), eng="act")
            fw.copy(halo_q("a", A, c, slice(0, 3)), cv("a", A, slice(TW, TW + 3)), eng="pool")
            tq = tmpr.get()
            fw.ts(tq("a"), cv("a", A, slice(0, TW)), cw("a", A, c, slice(0, 1)), ALU.mult)
            for k in range(1, 4):
                fw.stt(tq("a"), cv("a", A, slice(k, k + TW)), cw("a", A, c, slice(k, k + 1)), tq("a"), ALU.mult, ALU.add)
            fw.act(qkv("a", A, c, F), tq("a"), AF.Silu)
        for _ in gg:
            pass
        ck(cx, "conv")
        for c in range(8):
            sq = tmpr.get()
            fw.act(sq("a"), qkv("a", A, c, F), AF.Square)
            ps = cx.ps.get()
            fw.mm(ps("a", A, F), cx.ones, sq("a"))
            rn = tmpr.get()
            rstd_from(cx, rn("a"), ps("a", A, F), 1.0, 1e-6, sq("a"))
            scale = (128.0 ** -0.5) if c < 4 else 1.0
            fw.stt(qkv("a", A, c, F), qkv("a", A, c, F), scale, rn("a"), ALU.mult, ALU.mult)
        for c in range(4):
            ps = inproj(1536 + c * 128)
            fw.act(zs("a", A, c, F), ps("a", A, F), AF.Silu)
        ck(cx, "l2")
        ck(cx, "tm")
        def conf_gen():
            for c in range(4):
                psa = inproj(2056 + c * 128)
                psg = inproj(2568 + c * 128)
                sg = tmpr.get()
                fw.act(sg("a"), psg("a", A, F), AF.Sigmoid)
                fw.tt(ucv[c]("a", A, slice(30, 30 + TW)), psa("a", A, F), sg("a"), ALU.mult)
                yield
                fw.ts(acc(c), ucv[c]("a", A, slice(0, TW)), dww("a", A, c, slice(0, 1)), ALU.mult, dwb("a", A, slice(c, c + 1)), ALU.add)
                for k in range(1, 31):
                    fw.stt(acc(c), ucv[c]("a", A, slice(k, k + TW)), dww("a", A, c, slice(k, k + 1)), acc(c), ALU.mult, ALU.add)
                    if k % 3 == 0:
                        yield
                fw.copy(ucv[c]("a", A, slice(0, 30)), ucv[c]("a", A, slice(TW, TW + 30)), eng="pool")
                yield
            ln_feature_major(cx, ph, [acc(c) for c in range(4)], 4,
                             [cng("a", A, slice(c, c + 1)) for c in range(4)], [cnb("a", A, slice(c, c + 1)) for c in range(4)],
                             [usb("a", A, c, F) for c in range(4)], func=AF.Silu, sq_rot=tmpr, stat=stat, TW=TW)
            yield
        cg = conf_gen()
        pump = lambda n=1: [next(cg, None) for _ in range(n)]
        ck(cx, "conf")
        H4 = range(4)
        for g in range(NG):
            gs = sl(g * 128, 128)
            pso = cx.psl.get()
            qn = [qkv("a", A, h, gs) for h in H4]; kn = [qkv("a", A, 4 + h, gs) for h in H4]; vv = [qkv("a", A, 8 + h, gs) for h in H4]
            gcT = [TM[g]("a", A, 0, slice(h, h + 1)) for h in H4]; nbT = [TM[g]("a", A, 1, slice(h, h + 1)) for h in H4]
            nbegT = [TM[g]("a", A, 2, slice(h, h + 1)) for h in H4]; betaT = [TM[g]("a", A, 3, slice(h, h + 1)) for h in H4]
            kdsT = [TM[g]("a", A, 4, slice(h, h + 1)) for h in H4]
            Pv = lambda bb: bb("a", A, slice(0, 128))
            Qv = lambda bb: bb("a", A, slice(128, 256))
            pst = [None] * 4
            for h in H4:
                ps = pst[h] = cx.ps.get()
                fw.transpose(ps("a", A, slice(0, 128)), kn[h], cx.ident)
                fw.transpose(ps("a", A, slice(128, 256)), vv[h], cx.ident)
            for h in H4:
                ps = pst[h]
                fw.act(U_nk[h]("a"), ps("a", A, slice(0, 128)), AF.Identity, scale=nbegT[h])
                fw.act(U_kd[h]("a"), ps("a", A, slice(0, 128)), AF.Identity, scale=kdsT[h])
                fw.act(U_vb[h]("a"), ps("a", A, slice(128, 256)), AF.Identity, scale=betaT[h])
            for h in H4:
                fw.stt(U_ds[h]("a"), gcB(h, gs), gcT[h], C(cx, C_NBS, 128), ALU.subtract, ALU.add)
                fw.stt(U_dt[h]("a"), gcB(h, gs), gcT[h], C(cx, C_NBTI, 128), ALU.subtract, ALU.subtract)
            pump(2)
            for h in H4:
                fw.act(U_ds[h]("a"), U_ds[h]("a"), AF.Exp, scale=-1.0)
                fw.act(U_dt[h]("a"), U_dt[h]("a"), AF.Exp)
            pump(2)
            for h in H4:
                fw.tt(U_t1[h]("a"), U_dt[h]("a"), C(cx, C_SM01T, 128), ALU.mult, eng="pool")
                fw.tt(U_t1[h]("a"), U_t1[h]("a"), nbB(h, gs), ALU.mult, eng="pool")
                fw.tt(U_qd[h]("a"), qn[h], egB(h, gs), ALU.mult, eng="pool")
            psA = [None] * 4
            for h in H4:
                psA[h] = cx.ps.get()
                fw.mm(psA[h]("a", A, slice(0, 128)), kn[h], kn[h])
                fw.mm(psA[h]("a", A, slice(128, 256)), kn[h], qn[h])
            cur = [0] * 4
            for h in H4:
                PQ = U_PQ[h][0]
                fw.stt(Pv(PQ), psA[h]("a", A, slice(0, 128)), nbT[h], U_ds[h]("a"), ALU.mult, ALU.mult)
                fw.tt(Qv(PQ), psA[h]("a", A, slice(0, 128)), U_t1[h]("a"), ALU.mult)
                fw.tt(U_at[h]("a"), psA[h]("a", A, slice(128, 256)), U_dt[h]("a"), ALU.mult)
            for h in H4:
                fw.tt(U_X[h][0]("a"), Qv(U_PQ[h][0]), cx.ident, ALU.add, eng="pool")
            pump(2)
            for lvl in range(1, 6):
                ps1 = [None] * 4; ps2 = [None] * 4
                for h in H4:
                    PQ = U_PQ[h][cur[h]]
                    ps1[h] = cx.ps.get()
                    fw.mm(ps1[h]("a", A, slice(0, 128)), Qv(PQ), Pv(PQ))
                    if lvl < 5:
                        fw.mm(ps1[h]("a", A, slice(128, 256)), Pv(PQ), Qv(PQ))
                for h in H4:
                    PQn = U_PQ[h][1 - cur[h]]
                    if lvl < 5:
                        fw.copy(PQn("a"), ps1[h]("a", A, slice(0, 256)), eng="act")
                    else:
                        fw.copy(Pv(PQn), ps1[h]("a", A, slice(0, 128)), eng="act")
                for h in H4:
                    PQn = U_PQ[h][1 - cur[h]]
                    ps2[h] = cx.ps.get()
                    fw.mm(ps2[h]("a", A, slice(0, 128)), Pv(PQn), U_X[h][cur[h]]("a"))
                for h in H4:
                    fw.tt(U_X[h][1 - cur[h]]("a"), ps2[h]("a", A, slice(0, 128)), U_X[h][cur[h]]("a"), ALU.add)
                    cur[h] = 1 - cur[h]
                pump(2)
            Xt = [U_X[h][cur[h]]("a") for h in H4]
            psn = [None] * 4
            for h in H4:
                psn[h] = cx.ps.get()
                fw.mm(psn[h]("a", A, slice(0, 128)), U_nk[h]("a"), Xt[h])
            for h in H4:
                fw.copy(U_nw[h]("a"), psn[h]("a", A, slice(0, 128)), eng="act")
            pump(2)
            for c in range(2):
                cs = sl(c * 64, 64)
                ci = g * 2 + c
                psC = [None] * 4; psS = [None] * 4
                for h in H4:
                    psC[h] = cx.ps.get()
                    fw.mm(psC[h]("a", A, slice(0, 128)), Xt[h], U_vb[h]("a"), start=True, stop=False)
                    fw.mm(psC[h]("a", A, slice(0, 128)), U_nw[h]("a"), Sst[h]("a"), start=False, stop=True)
                for h in H4:
                    fw.copy(U_vn[h]("a", cs, slice(0, 128)), psC[h]("a", cs, slice(0, 128)), eng="act")
                for h in H4:
                    ocols = sl(h * 128 + c * 64, 64)
                    fw.mm(pso("a", A, ocols), Sst[h]("a"), U_qd[h]("a", A, cs), start=True, stop=False)
                    fw.mm(pso("a", A, ocols), U_vn[h]("a", cs, slice(0, 128)), U_at[h]("a", cs, cs), start=False, stop=True)
                for h in H4:
                    psS[h] = cx.ps.get()
                    fw.mm(psS[h]("a", A, slice(0, 128)), U_kd[h]("a", cs, slice(0, 128)), U_vn[h]("a", cs, slice(0, 128)))
                for h in H4:
                    fw.stt(Sst[h]("a"), Sst[h]("a"), cdec("a", A, h, slice(ci, ci + 1)), psS[h]("a", A, slice(0, 128)), ALU.mult, ALU.add)
                pump(2)
            fw.copy(oT.ap("a", oT.h[:, :, g * 128:(g + 1) * 128]), pso.ap("a", pso.h[:, :].rearrange("p (h t) -> p h t", t=128)), eng="act")
        for _ in cg:
            pass
        ck(cx, "dn")
        for h in range(4):
            sq = tmpr.get()
            fw.act(sq("a"), oT("a", A, h, F), AF.Square)
            ps = cx.ps.get()
            fw.mm(ps("a", A, F), cx.ones, sq("a"))
            rn = tmpr.get()
            rstd_from(cx, rn("a"), ps("a", A, F), 1.0 / 128.0, 1e-6, sq("a"))
            fw.tt(rn("a"), rn("a"), oT("a", A, h, F), ALU.mult)
            fw.tt(rn("a"), rn("a"), zs("a", A, h, F), ALU.mult, eng="pool")
            fw.act(oab("a", A, h, F), rn("a"), AF.Identity, scale=ong("a", A, slice(0, 1)))
        ck(cx, "rms")
        for dc in range(8):
            wo = worot.get()
            fw.dma("pool", wo("a"), View(di["ab_w_out"].h[:, dc * 128:(dc + 1) * 128].rearrange("(k p) c -> p k c", p=128), []))
            ps = cx.ps.get()
            for kc in range(8):
                rhs = oab("a", A, kc, F) if kc < 4 else usb("a", A, kc - 4, F)
                fw.mm(ps("a", A, F), wo("a", A, kc, slice(0, 128)), rhs, start=(kc == 0), stop=(kc == 7))
            fw.stt(yT(dc), xT("a", A, dc, F), ALPHA, ps("a", A, F), ALU.mult, ALU.add)
        xo = [x1o.get() if c < 0 else None for c in range(8)]
        outv = []
        def ln_out_cb(c):
            b = x1o.get()
            return b
        bufs = []
        ln_feature_major(cx, ph, [yT(c) for c in range(8)], 8,
                         [lng("a", A, slice(c, c + 1)) for c in range(8)], [lnb("a", A, slice(c, c + 1)) for c in range(8)],
                         None, sq_rot=tmpr, stat=stat, TW=TW, out_rot=x1o,
                         out_cb=lambda c, v: fw.dma("sp", cx.x1((t, c), sl(c * 128, 128), sl(t0, TW)), v))
    ph.close()


def phaseC(cx, TW=256):
    fw, nc, di = cx.fw, cx.nc, cx.di
    ph = Phase(cx)
    NTW = cx.S // TW; NG = TW // 128; NCH = TW // 64
    A = slice(0, 128); F = slice(0, TW); G4 = slice(0, 4)
    Wb = load_w_bf16(cx, ph, di["c_w_in"].h, 8, 3080)
    Wo = load_w_bf16(cx, ph, di["c_w_out"].h, 8, 1024)
    Wg32 = ph.sb([128, 8, 8])
    for kc in range(8):
        fw.dma("sp", Wg32("a", A, kc, slice(0, 8)), View(di["c_w_in"].h[kc * 128:(kc + 1) * 128, 3072:3080], []))
    ng = load_cols(cx, ph, di["c_norm_g"].h[0:1024].rearrange("(c p) -> p c", p=128), [128, 8])
    lng = load_cols(cx, ph, di["ln_mix_g"].h[1, :].rearrange("(c p) -> p c", p=128), [128, 8])
    lnb = load_cols(cx, ph, di["ln_mix_b"].h[1, :].rearrange("(c p) -> p c", p=128), [128, 8])
    hp = ph.sb([4, 4])
    fw.dma("sp", hp("a", G4, slice(0, 1)), View(di["c_b_i"].h[:].rearrange("(p o) -> p o", o=1), []), allow_slow_non_contiguous=True)
    fw.dma("sp", hp("a", G4, slice(1, 2)), View(di["c_b_f"].h[:].rearrange("(p o) -> p o", o=1), []), allow_slow_non_contiguous=True)
    fw.ts(hp("a", G4, slice(2, 4)), hp("a", G4, slice(0, 2)), 1.0 / 15.0, ALU.mult)
    Caug = [ph.sb([128, 384]) for _ in range(4)]
    for h in range(4):
        fw.memset(Caug[h]("a"), 0.0)
    mcar = ph.sb([4, 1]); fw.memset(mcar("a"), 0.0)
    vaug = ph.sb([128, NG, 4, 384])
    fw.memset(vaug("a"), 1.0)
    xT = ph.sb([128, 8, TW]); xTb = ph.sb([128, 8, TW], BF16)
    qT = ph.sb([128, 4, TW]); kT = ph.sb([128, 4, TW])
    ktok = ph.sb([128, NG, 512])
    og = ph.sb([128, 8, TW], BF16)
    GF = ph.sb([4, 12, TW])
    SM = ph.sb([4, 8, NCH])
    TMc = [ph.sb([128, 2, 4]) for _ in range(NG)]
    B1 = ph.sb([128, 8, TW]); B2 = ph.sb([128, 8, TW])
    cmB = lambda h, cols: B1("a", A, h, cols)
    siB = lambda h, cols: B1("a", A, 4 + h, cols)
    saB = lambda h, cols: B2("a", A, h, cols)
    emB = lambda h, cols: B2("a", A, 4 + h, cols)
    decB = ph.sb([128, 4, NCH])
    hT = ph.sb([128, 8, TW])
    hb = ph.sb([128, 8, TW], BF16)
    tmpr = ph.rot(6, [128, TW])
    stat = [ph.sb([128, TW]) for _ in range(4)]
    x3o = ph.rot(3, [128, TW])
    mk4 = lambda shape=(128, 128): [ph.sb(list(shape)) for _ in range(4)]
    U_E = mk4(); U_kw = mk4(); U_pm = mk4(); U_dn = mk4()
    U_t1 = mk4((128, 384)); U_t2 = mk4((128, 384))
    psR = [cx.psb[6], cx.psb[7], cx.psb[4], cx.psb[5]]
    cx.ps = Rot(cx.psb[0:4])
    siB3 = lambda h, cols: B1.ap("a", B1.h[:, 4 + h:5 + h, cols].to_broadcast([128, 3, 128]))
    saB3 = lambda h, cols: B2.ap("a", B2.h[:, h:h + 1, cols].to_broadcast([128, 3, 128]))
    yv = lambda c: B1("a", A, c, F)

    for t in range(NTW):
        t0 = t * TW
        for c in range(8):
            fw.dma("sp", xT("a", A, c, F), cx.x2((t, c), sl(c * 128, 128), sl(t0, TW)))
        fw.copy(xTb("a"), xT("a"), eng="act")

        def inproj(c0):
            ps = cx.ps.get()
            for kc in range(8):
                fw.mm(ps("a", A, F), Wb("w", A, kc, sl(c0, 128)), xTb("a", A, kc, F), start=(kc == 0), stop=(kc == 7))
            return ps
        def gate_gen():
            psi_ = cx.ps.get(); psf_ = cx.ps.get()
            for kc in range(8):
                fw.mm(psi_("a", G4, F), Wg32("a", A, kc, slice(0, 4)), xT("a", A, kc, F), start=(kc == 0), stop=(kc == 7))
            for kc in range(8):
                fw.mm(psf_("a", G4, F), Wg32("a", A, kc, slice(4, 8)), xT("a", A, kc, F), start=(kc == 0), stop=(kc == 7))
            gf = lambda q: GF("a", G4, q, F)
            g3 = lambda q: GF.ap("a", GF.h[0:4, q, :].rearrange("p (c l) -> p c l", l=64))
            smv = lambda q, a=0, n=None: SM("a", G4, q, slice(a, NCH if n is None else a + n))
            smb = lambda q: SM.ap("a", SM.h[0:4, q, :].rearrange("p (c o) -> p c o", o=1).to_broadcast([4, NCH, 64]))
            fw.act(gf(0), psi_("a", G4, F), AF.Tanh, scale=1.0 / 15.0, bias=hp("a", G4, slice(2, 3)))
            fw.ts(gf(0), gf(0), 15.0, ALU.mult)
            fw.act(gf(1), psf_("a", G4, F), AF.Tanh, scale=1.0 / 15.0, bias=hp("a", G4, slice(3, 4)))
            fw.act(gf(1), gf(1), AF.Exp, scale=-15.0)
            yield
            fw.ts(gf(1), gf(1), 1.0, ALU.add)
            fw.act(gf(1), gf(1), AF.Ln)
            fw.ts(gf(1), gf(1), -1.0, ALU.mult)
            rm = C(cx, C_RM, TW, 4); rb = C(cx, C_RB, TW, 4)
            fw.op("dve", lambda: nc.vector.tensor_tensor_scan(gf(2).ap, rm.ap, gf(1).ap, 0.0, ALU.mult, ALU.add), [gf(2)], [rm, gf(1)])
            yield
            fw.tt(gf(3), gf(0), gf(2), ALU.subtract)
            fw.op("dve", lambda: nc.vector.tensor_tensor_scan(gf(4).ap, rb.ap, gf(3).ap, 0.0, ALU.add, ALU.max), [gf(4)], [rb, gf(3)])
            fw.tt(gf(5), gf(2), gf(4), ALU.add)
            fw.copy(smv(0), GF.ap("a", GF.h[0:4, 2, 63::64]))
            fw.tt(smv(1), smv(0), GF.ap("a", GF.h[0:4, 4, 63::64]), ALU.add)
            fw.op("dve", lambda: nc.vector.tensor_tensor_scan(smv(2).ap, smv(0).ap, smv(1).ap, mcar("a").ap, ALU.add, ALU.max),
                  [smv(2)], [smv(0), smv(1), mcar("a")])
            fw.copy(smv(3, 0, 1), mcar("a"))
            if NCH > 1:
                fw.copy(smv(3, 1, NCH - 1), smv(2, 0, NCH - 1))
            fw.copy(mcar("a"), smv(2, NCH - 1, 1))
            yield
            fw.tt(g3(6), g3(2), smb(3), ALU.add)
            fw.tt(gf(7), gf(6), gf(5), ALU.max)
            fw.tt(gf(8), gf(6), gf(7), ALU.subtract); fw.act(gf(8), gf(8), AF.Exp)
            fw.tt(gf(9), gf(5), gf(7), ALU.subtract); fw.act(gf(9), gf(9), AF.Exp)
            fw.act(gf(10), gf(7), AF.Exp, scale=-1.0)
            fw.tt(smv(5), smv(0), smv(2), ALU.subtract)
            fw.tt(g3(11), g3(3), smb(5), ALU.add); fw.act(gf(11), gf(11), AF.Exp)
            fw.tt(smv(4), smv(5), smv(3), ALU.add); fw.act(smv(4), smv(4), AF.Exp)
            yield
            for g in range(NG):
                ps = cx.ps.get()
                for qi, q in enumerate((3, 11)):
                    fw.mm(ps("a", A, sl(qi * 4, 4)), GF("a", G4, q, sl(g * 128, 128)), C(cx, C_ID, 4, 4))
                fw.copy(TMc[g]("a"), ps.ap("a", ps.h[:, 0:8].rearrange("p (q h) -> p q h", h=4)))
            for h in range(4):
                bcast_rows(cx, cmB(h, F), gf(4), C_SEL4, h, 4, TW)
                bcast_rows(cx, siB(h, F), gf(8), C_SEL4, h, 4, TW)
                bcast_rows(cx, saB(h, F), gf(9), C_SEL4, h, 4, TW)
                bcast_rows(cx, emB(h, F), gf(10), C_SEL4, h, 4, TW)
                yield
            ps = cx.ps.get()
            for h in range(4):
                fw.mm(ps("a", A, sl(h * NCH, NCH)), C(cx, C_SEL4 + h * 128, 128, 4), smv(4))
            fw.copy(decB("a"), ps.ap("a", ps.h[:, 0:4 * NCH].rearrange("p (h c) -> p h c", c=NCH)))
            yield
        gg = gate_gen()
        for h in range(4):
            next(gg, None); next(gg, None)
            ps = inproj(h * 128)
            fw.act(qT("a", A, h, F), ps("a", A, F), AF.Copy, scale=128.0 ** -0.5)
            ps = inproj(512 + h * 128)
            fw.copy(kT("a", A, h, F), ps("a", A, F), eng="act")
        for c in range(8):
            next(gg, None); next(gg, None)
            ps = inproj(2048 + c * 128)
            fw.act(og("a", A, c, F), ps("a", A, F), AF.Sigmoid)
        for g in range(NG):
            gs = sl(g * 128, 128)
            next(gg, None); next(gg, None)
            ps = cx.ps.get()
            for kc in range(8):
                fw.mm(ps("a"), xTb("a", A, kc, gs), Wb("w", A, kc, slice(512, 1024)), start=(kc == 0), stop=(kc == 7))
            fw.copy(ktok("a", A, g, slice(0, 512)), ps("a"), eng="act")
            for half in range(2):
                ps = cx.ps.get()
                for kc in range(8):
                    fw.mm(ps("a"), xTb("a", A, kc, gs), Wb("w", A, kc, sl(1024 + half * 512, 512)), start=(kc == 0), stop=(kc == 7))
                fw.copy(vaug.ap("a", vaug.h[:, g, 2 * half:2 * half + 2, 0:256]),
                        ps.ap("a", ps.h[:, :].rearrange("p (h d) -> p h d", d=256)), eng="act")
        for _ in gg:
            pass
        H4 = range(4)
        for g in range(NG):
            gs = sl(g * 128, 128)
            aT = [TMc[g]("a", A, 0, slice(h, h + 1)) for h in H4]; ekT = [TMc[g]("a", A, 1, slice(h, h + 1)) for h in H4]
            for h in H4:
                fw.stt(U_E[h]("a"), cmB(h, gs), aT[h], C(cx, C_NBTI, 128), ALU.subtract, ALU.add)
            for h in H4:
                fw.act(U_E[h]("a"), U_E[h]("a"), AF.Exp, scale=-1.0)
                fw.act(U_kw[h]("a"), ktok("a", A, g, sl(h * 128, 128)), AF.Identity, scale=ekT[h])
            psQ = [None] * 4
            for h in H4:
                psQ[h] = cx.ps.get()
                fw.mm(psQ[h]("a", A, slice(0, 128)), kT("a", A, h, gs), qT("a", A, h, gs))
            for h in H4:
                fw.tt(U_pm[h]("a"), psQ[h]("a", A, slice(0, 128)), U_E[h]("a"), ALU.mult)
            for c in range(2):
                cs = sl(c * 64, 64)
                ci = g * 2 + c
                for h in H4:
                    for j in range(3):
                        fw.mm(psR[h]("a", A, sl(j * 128 + c * 64, 64)), Caug[h]("a", A, sl(j * 128, 128)),
                              qT("a", A, h, sl(g * 128 + c * 64, 64)))
                psS = [None] * 4
                for h in H4:
                    psS[h] = cx.ps.get()
                    fw.mm(psS[h]("a", A, slice(0, 384)), U_kw[h]("a", cs, slice(0, 128)), vaug("a", cs, g, h, slice(0, 384)))
                for h in H4:
                    fw.stt(Caug[h]("a"), Caug[h]("a"), decB("a", A, h, slice(ci, ci + 1)), psS[h]("a", A, slice(0, 384)), ALU.mult, ALU.add)
            for h in H4:
                psI = cx.ps.get()
                for j in range(3):
                    fw.mm(psI("a", A, sl(j * 128, 128)), vaug("a", A, g, h, sl(j * 128, 128)), U_pm[h]("a"))
                t1 = U_t1[h]; t2 = U_t2[h]
                fw.tt(t1("a"), psR[h]("a", A, slice(0, 384)), siB3(h, gs), ALU.mult)
                fw.tt(t2("a"), psI("a", A, slice(0, 384)), saB3(h, gs), ALU.mult)
                fw.tt(t1("a"), t1("a"), t2("a"), ALU.add, eng="pool")
            for h in H4:
                t1 = U_t1[h]; dn = U_dn[h]
                c2 = t1("a", A, slice(256, 384))
                fw.ts(dn("a"), c2, -1.0, ALU.mult, eng="pool")
                fw.tt(dn("a"), dn("a"), c2, ALU.max)
                fw.tt(dn("a"), dn("a"), emB(h, gs), ALU.max)
                fw.op("dve", lambda: nc.vector.reciprocal(dn("a").ap, dn("a").ap), [dn("a")], [dn("a")])
            for h in H4:
                t1 = U_t1[h]; dn = U_dn[h]
                for j in range(2):
                    fw.tt(hT("a", A, h * 2 + j, gs), t1("a", A, sl(j * 128, 128)), dn("a"), ALU.mult)
        for h in range(4):
            ps = cx.ps.get()
            for j in range(2):
                sq = tmpr.get()
                fw.act(sq("a"), hT("a", A, h * 2 + j, F), AF.Square)
                fw.mm(ps("a", A, F), cx.ones, sq("a"), start=(j == 0), stop=(j == 1))
            rn = tmpr.get(); tq = tmpr.get()
            rstd_from(cx, rn("a"), ps("a", A, F), 1.0 / 256.0, 1e-6, tq("a"))
            for j in range(2):
                c = h * 2 + j
                t1 = tmpr.get()
                fw.tt(t1("a"), hT("a", A, c, F), rn("a"), ALU.mult)
                fw.tt(t1("a"), t1("a"), og("a", A, c, F), ALU.mult, eng="pool")
                fw.act(hb("a", A, c, F), t1("a"), AF.Identity, scale=ng("a", A, slice(c, c + 1)))
        for dc in range(8):
            ps = cx.ps.get()
            for kc in range(8):
                fw.mm(ps("a", A, F), Wo("w", A, kc, sl(dc * 128, 128)), hb("a", A, kc, F), start=(kc == 0), stop=(kc == 7))
            fw.stt(yv(dc), xT("a", A, dc, F), ALPHA, ps("a", A, F), ALU.mult, ALU.add)
        ln_feature_major(cx, ph, [yv(c) for c in range(8)], 8,
                         [lng("a", A, slice(c, c + 1)) for c in range(8)], [lnb("a", A, slice(c, c + 1)) for c in range(8)],
                         None, sq_rot=tmpr, stat=stat, TW=TW, out_rot=x3o,
                         out_cb=lambda c, v: fw.dma("sp", cx.x3((t, c), sl(c * 128, 128), sl(t0, TW)), v))
    ph.close()
    cx.ps = Rot(cx.psb[0:6])


def phaseF(cx, layer, xin, xout_scratch):
    fw, nc, di = cx.fw, cx.nc, cx.di
    ph = Phase(cx)
    TW = 512
    NTW = cx.S // TW
    NTS = 2 if NTW % 2 == 0 else 1
    SW = NTS * TW
    A = slice(0, 128); F = slice(0, TW)
    moe = (layer == 1)
    NE = 8 if moe else 1
    if moe:
        wg_ap = lambda e: di["moe_w_gate"].h[e]; wu_ap = lambda e: di["moe_w_up"].h[e]; wd_ap = lambda e: di["moe_w_down"].h[e]
    else:
        wg_ap = lambda e: di["ffn_w_gate"].h; wu_ap = lambda e: di["ffn_w_up"].h; wd_ap = lambda e: di["ffn_w_down"].h
    lng = load_cols(cx, ph, di["ln_ffn_g"].h[layer, :].rearrange("(c p) -> p c", p=128), [128, 8])
    lnb = load_cols(cx, ph, di["ln_ffn_b"].h[layer, :].rearrange("(c p) -> p c", p=128), [128, 8])
    Wpp = load_w_bf16(cx, ph, di["ple_w_proj"].h[layer], 2, 1024)
    if moe:
        Wr = ph.sb([128, 8, 8])
        for kc in range(8):
            fw.dma("sp", Wr("a", A, kc, slice(0, 8)), View(di["moe_w_router"].h[kc * 128:(kc + 1) * 128, :], []))
        brB = ph.sb([128, 8])
        fw.dma("sp", brB("a"), View(di["moe_b_router"].h[:].partition_broadcast(128), []), allow_slow_non_contiguous=True)
        sel8 = ph.sb([8, 1024])
        fw.dma("sp", sel8("a"), di["sel8"]("a"))
        combT = [ph.sb([8, TW]) for _ in range(NTS)]
        combB = [ph.sb([128, TW]) for _ in range(NTS)]
        rt = ph.rot(2, [128, 176])
    xf = [ph.sb([128, 8, TW]) for _ in range(NTS)]
    xb = [ph.sb([128, 8, TW], BF16) for _ in range(NTS)]
    y = [ph.sb([128, 8, TW]) for _ in range(NTS)]
    wgb = ph.rot(2, [128, 8, 512], BF16); wub = ph.rot(2, [128, 8, 512], BF16); wdb = ph.rot(2, [128, 4, 1024], BF16)
    hb = ph.rot(8 * NTS, [128, TW], BF16)
    tmpr = ph.rot(4, [128, TW])
    stat = [ph.sb([128, TW]) for _ in range(4)]
    xin_rot = ph.rot(2, [128, 256])
    pTb = ph.sb([128, 2, TW], BF16)
    otok = ph.rot(2, [128, 1024])
    oT = ph.rot(3, [128, TW]) if xout_scratch is not None else None

    for st in range(NTW // NTS):
        for ti in range(NTS):
            t = st * NTS + ti; t0 = t * TW
            for c in range(8):
                fw.dma("sp", xf[ti]("a", A, c, F), xin((t, c), sl(c * 128, 128), sl(t0, TW)))
            fw.copy(xb[ti]("a"), xf[ti]("a"), eng="act")
            if moe:
                ps = cx.ps.get()
                for g in range(4):
                    gs = sl(g * 128, 128)
                    for kc in range(8):
                        fw.mm(ps("a", A, sl(g * 8, 8)), xf[ti]("a", A, kc, gs), Wr("a", A, kc, slice(0, 8)), start=(kc == 0), stop=(kc == 7))
                r = rt.get()
                R3 = lambda a: r.ap("a", r.h[:, a:a + 32].rearrange("p (g e) -> p g e", e=8))
                R1 = lambda a: r("a", A, sl(a, 4))
                RB = lambda a: r.ap("a", r.h[:, a:a + 4].rearrange("p (g o) -> p g o", o=1).to_broadcast([128, 4, 8]))
                lg = R3(0); eq = R3(32); l2 = R3(64); ex = R3(96); cb = R3(128)
                m1 = R1(160); m2 = R1(164); dd = R1(168)
                fw.tt(lg, ps.ap("a", ps.h[:, 0:32].rearrange("p (g e) -> p g e", e=8)),
                      brB.ap("a", brB.h[:, :].rearrange("p (o e) -> p o e", o=1).to_broadcast([128, 4, 8])), ALU.add)
                fw.reduce(m1, lg, ALU.max)
                fw.tt(eq, lg, RB(160), ALU.is_equal)
                fw.stt(l2, eq, -1.0e30, lg, ALU.mult, ALU.add)
                fw.reduce(m2, l2, ALU.max)
                fw.tt(eq, lg, RB(164), ALU.is_ge)
                fw.tt(ex, lg, RB(160), ALU.subtract)
                fw.act(ex, ex, AF.Exp)
                fw.tt(dd, m2, m1, ALU.subtract)
                fw.act(dd, dd, AF.Exp)
                fw.ts(dd, dd, 1.0, ALU.add)
                fw.op("dve", lambda: nc.vector.reciprocal(dd.ap, dd.ap), [dd], [dd])
                fw.tt(cb, ex, RB(168), ALU.mult)
                fw.tt(cb, cb, eq, ALU.mult)
                ps2 = cx.ps.get()
                for g in range(4):
                    fw.mm(ps2("a", slice(0, 8), sl(g * 128, 128)), r("a", A, sl(128 + g * 8, 8)), cx.ident)
                fw.copy(combT[ti]("a"), ps2("a", slice(0, 8), F), eng="act")
        for e in range(NE):
            if moe:
                for ti in range(NTS):
                    ps = cx.ps.get()
                    fw.mm(ps("a", A, F), sel8("a", slice(0, 8), sl(e * 128, 128)), combT[ti]("a"))
                    fw.copy(combB[ti]("a"), ps("a", A, F), eng="act")
            for f in range(7):
                wg = wgb.get(); wu = wub.get(); wd = wdb.get()
                fw.dma("pool", wg("a"), View(wg_ap(e)[:, f * 512:(f + 1) * 512].rearrange("(k p) c -> p k c", p=128), []))
                fw.dma("pool", wu("a"), View(wu_ap(e)[:, f * 512:(f + 1) * 512].rearrange("(k p) c -> p k c", p=128), []))
                fw.dma("pool", wd("a"), View(wd_ap(e)[f * 512:(f + 1) * 512, :].rearrange("(k p) c -> p k c", p=128), []))
                hs = [[None] * 4 for _ in range(NTS)]
                for fc in range(4):
                    for ti in range(NTS):
                        pg = cx.ps.get(); pu = cx.ps.get()
                        for kc in range(8):
                            fw.mm(pg("a", A, F), wg("a", A, kc, sl(fc * 128, 128)), xb[ti]("a", A, kc, F), start=(kc == 0), stop=(kc == 7), lazy=True)
                        for kc in range(8):
                            fw.mm(pu("a", A, F), wu("a", A, kc, sl(fc * 128, 128)), xb[ti]("a", A, kc, F), start=(kc == 0), stop=(kc == 7), lazy=True)
                        sg = tmpr.get()
                        fw.act(sg("a"), pg("a", A, F), AF.Silu)
                        h = hb.get()
                        if moe:
                            fw.tt(sg("a"), sg("a"), pu("a", A, F), ALU.mult)
                            fw.tt(h("a"), sg("a"), combB[ti]("a"), ALU.mult)
                        else:
                            fw.tt(h("a"), sg("a"), pu("a", A, F), ALU.mult)
                        hs[ti][fc] = h
                for dc in range(8):
                    for ti in range(NTS):
                        pd = cx.ps.get()
                        for fc in range(4):
                            fw.mm(pd("a", A, F), wd("a", A, fc, sl(dc * 128, 128)), hs[ti][fc]("a"), start=(fc == 0), stop=(fc == 3), lazy=True)
                        if e == 0 and f == 0:
                            fw.stt(y[ti]("a", A, dc, F), xf[ti]("a", A, dc, F), ALPHA, pd("a", A, F), ALU.mult, ALU.add)
                        else:
                            fw.tt(y[ti]("a", A, dc, F), pd("a", A, F), y[ti]("a", A, dc, F), ALU.add)
        for ti in range(NTS):
            t = st * NTS + ti; t0 = t * TW
            yv = y[ti]; xx = xf[ti]; xxb = xb[ti]
            ln_feature_major(cx, ph, [yv("a", A, c, F) for c in range(8)], 8,
                             [lng("a", A, slice(c, c + 1)) for c in range(8)], [lnb("a", A, slice(c, c + 1)) for c in range(8)],
                             [xx("a", A, c, F) for c in range(8)], sq_rot=tmpr, stat=stat, TW=TW)
            fw.copy(xxb("a"), xx("a"), eng="act")
            load_x_tile_T(cx, di["p"].h[layer], t0, TW, None, pTb, xin_rot, nchunks=2)
            for half in range(2):
                wpg = wgb.get()
                fw.dma("pool", wpg("a"), View(di["ple_w_gate"].h[layer][:, half * 512:(half + 1) * 512].rearrange("(k p) c -> p k c", p=128), []))
                for d4 in range(4):
                    dc = half * 4 + d4
                    pg = cx.ps.get(); pp = cx.ps.get()
                    for kc in range(8):
                        fw.mm(pg("a", A, F), wpg("a", A, kc, sl(d4 * 128, 128)), xxb("a", A, kc, F), start=(kc == 0), stop=(kc == 7))
                    for kc in range(2):
                        fw.mm(pp("a", A, F), Wpp("w", A, kc, sl(dc * 128, 128)), pTb("a", A, kc, F), start=(kc == 0), stop=(kc == 1))
                    sg = tmpr.get()
                    fw.act(sg("a"), pg("a", A, F), AF.Sigmoid)
                    fw.tt(sg("a"), sg("a"), pp("a", A, F), ALU.mult)
                    if xout_scratch is not None:
                        o = oT.get()
                        fw.tt(o("a"), sg("a"), xx("a", A, dc, F), ALU.add, eng="pool")
                        fw.dma("sp", xout_scratch((t, dc), sl(dc * 128, 128), sl(t0, TW)), o("a"))
                    else:
                        fw.tt(yv("a", A, dc, F), sg("a"), xx("a", A, dc, F), ALU.add, eng="pool")
            if xout_scratch is None:
                for g in range(4):
                    ot = otok.get()
                    for d0 in (0, 4):
                        ps = cx.ps.get()
                        for d4 in range(4):
                            fw.transpose(ps("a", A, sl(d4 * 128, 128)), yv("a", A, d0 + d4, sl(g * 128, 128)), cx.ident)
                        fw.copy(ot("a", A, sl(d0 * 128, 512)), ps("a"), eng="act")
                    fw.dma("sp", cx.out((t, g), sl(t0 + g * 128, 128), slice(0, 1024)), ot("a"))
    ph.close()


from concourse.bass_utils import run_bass_kernel_spmd

SEQ = 4096
NCORES = 8
_SQUEEZE = ("ab_w_in", "ab_conv_qkv", "ab_a_log", "ab_dt_bias", "ab_o_norm_g", "ab_dw_w", "ab_dw_b", "ab_cn_g",
            "ab_cn_b", "ab_w_out", "ffn_w_gate", "ffn_w_up", "ffn_w_down", "c_w_in", "c_b_i", "c_b_f", "c_norm_g",
            "c_w_out", "moe_w_router", "moe_b_router", "moe_w_gate", "moe_w_up", "moe_w_down")


def build_program(S):
    nc = bass.Bass("TRN2", target_bir_lowering=False)
    cx = setup(nc, S)
    phaseA(cx)
    phaseF(cx, 0, cx.x1, cx.x2)
    phaseC(cx)
    phaseF(cx, 1, cx.x3, None)
    cx.fw.barrier()
    return nc


def kernel(**inputs):
    f32 = lambda a: np.ascontiguousarray(np.asarray(a), dtype=np.float32)
    x = f32(inputs["x"]); p = f32(inputs["p"])
    B, S, _ = x.shape
    shared = {}
    for k, v in inputs.items():
        if k in ("x", "p"):
            continue
        a = f32(v)
        if k in _SQUEEZE:
            a = np.ascontiguousarray(a[0])
        shared[k] = a
    shared["cst"] = make_consts()
    shared["sel8"] = make_sel8()
    nc = build_program(S)
    in_maps = []
    for b in range(B):
        m = dict(shared)
        m["x"] = np.ascontiguousarray(x[b])
        m["p"] = np.ascontiguousarray(p[:, b])
        in_maps.append(m)
    res = run_bass_kernel_spmd(nc, in_maps, core_ids=list(range(B)))
    return np.stack([np.asarray(r["out"], dtype=np.float32) for r in res.results], axis=0)
```
